# Optimizing a Trainium2 kernel written in Bass

```python
import jax
import jax.numpy as jnp
from jax import lax
import numpy as np

D_MODEL = 1024
BATCH = 2
SEQ = 16384
DEPTH = 2

GRID_W = 64
CTX_LEN = 256

ML_HEADS = 4
ML_DH = 64
ML_W = ML_HEADS * ML_DH
ML_CHUNK = 64
FT_GROUPS = 4
FT_GC = 64
FT_W = FT_GROUPS * FT_GC
GQ_KV = 2
GQ_G = 4
GQ_HEADS = GQ_KV * GQ_G
GQ_DH = 64
GQ_W = GQ_HEADS * GQ_DH
GQ_BLOCK = 128
ROPE_PAIRS = GQ_DH // 4
ROPE_BASE = 10000.0
GL_HEADS = 4
GL_DK = 64
GL_DV = 64
GL_W = GL_HEADS * GL_DV
GL_RANK = 16
GL_TAU = 16.0
GL_CHUNK = 64
FF_DENSE = 2816
N_EXPERTS = 8
TOP_K = 2
FF_EXPERT = 1408

N_BRANCH = 4
BRANCH_WIDTHS = (ML_W, FT_W, GQ_W, GL_W)
MIX_W = ML_W + FT_W + GQ_W + GL_W
N_ADA = 6
LN_EPS = 1e-6
DEEPNORM_ALPHA = (2.0 * DEPTH) ** 0.25
DEEPNORM_BETA = (8.0 * DEPTH) ** -0.25

IN_COLS = (
    ('ml_q', ML_W), ('ml_k', ML_W), ('ml_v', ML_W), ('ml_o', ML_W),
    ('ml_if', ML_HEADS), ('ml_ff', ML_HEADS), ('ml_ib', ML_HEADS), ('ml_fb', ML_HEADS),
    ('ft', FT_W),
    ('gq_q', GQ_W), ('gq_k', GQ_KV * GQ_DH), ('gq_v', GQ_KV * GQ_DH),
    ('gl_q', GL_HEADS * GL_DK), ('gl_k', GL_HEADS * GL_DK), ('gl_v', GL_HEADS * GL_DV), ('gl_r', GL_W),
    ('gl_af', GL_RANK), ('gl_ab', GL_RANK),
)
N_IN = sum(w for _, w in IN_COLS)

kernel_name = 'hybrid_diffusion_mlstm_fnet_gqa_gla_moe'

F32 = jnp.float32


def _layernorm(x):
    xf = x.astype(F32)
    xc = xf - jnp.mean(xf, axis=-1, keepdims=True)
    var = jnp.mean(xc * xc, axis=-1, keepdims=True)
    return (xc * lax.rsqrt(var + LN_EPS)).astype(x.dtype)


def _rms(x):
    xf = x.astype(F32)
    return (xf * lax.rsqrt(jnp.mean(xf * xf, axis=-1, keepdims=True) + LN_EPS)).astype(x.dtype)


def _modulate(x, shift, scale):
    return _layernorm(x) * (1.0 + scale) + shift


def _post_ln(x, g, b):
    return _layernorm(x) * g + b


def _split_cols(p):
    out = {}
    off = 0
    for name, w in IN_COLS:
        out[name] = p[..., off:off + w]
        off += w
    return out


def _heads(a, n):
    b_, t, w = a.shape
    return a.reshape(b_, t, n, w // n).transpose(0, 2, 1, 3)


def _merge_heads(a):
    b_, n, t, d = a.shape
    return a.transpose(0, 2, 1, 3).reshape(b_, t, n * d)


def _to_chunks(a, size):
    nc = a.shape[2] // size
    a = a.reshape(a.shape[:2] + (nc, size) + a.shape[3:])
    return jnp.moveaxis(a, 2, 0)


def _from_chunks(a):
    a = jnp.moveaxis(a, 0, 2)
    return a.reshape(a.shape[:2] + (a.shape[2] * a.shape[3],) + a.shape[4:])


def _flip(a):
    return jnp.flip(a, axis=2)


def _mlstm_scan(q, k, v, li, lf, state):
    size = ML_CHUNK
    causal = jnp.tril(jnp.ones((size, size), dtype=bool))

    def step(carry, inp):
        cmat, nvec, m = carry
        qc, kc, vc, ic, fc = inp
        b = jnp.cumsum(fc, axis=-1)
        d = jnp.where(causal, b[..., :, None] - b[..., None, :] + ic[..., None, :], -jnp.inf)
        inter = b + m[..., None]
        m_t = jnp.maximum(inter, jnp.max(d, axis=-1))
        w_intra = jnp.exp(d - m_t[..., None]) * jnp.einsum('bhtd,bhsd->bhts', qc, kc)
        w_inter = jnp.exp(inter - m_t)
        num = (jnp.einsum('bhts,bhsd->bhtd', w_intra, vc)
               + w_inter[..., None] * jnp.einsum('bhvk,bhtk->bhtv', cmat, qc))
        den = jnp.sum(w_intra, axis=-1) + w_inter * jnp.einsum('bhk,bhtk->bht', nvec, qc)
        h = num / jnp.maximum(jnp.abs(den), jnp.exp(-m_t))[..., None]
        b_last = b[..., -1]
        g = b_last[..., None] - b + ic
        m_new = jnp.maximum(b_last + m, jnp.max(g, axis=-1))
        ws = jnp.exp(g - m_new[..., None])
        wc = jnp.exp(b_last + m - m_new)
        cmat = wc[..., None, None] * cmat + jnp.einsum('bhs,bhsv,bhsk->bhvk', ws, vc, kc)
        nvec = wc[..., None] * nvec + jnp.einsum('bhs,bhsk->bhk', ws, kc)
        return (cmat, nvec, m_new), h

    xs = (_to_chunks(q, size), _to_chunks(k, size), _to_chunks(v, size),
          _to_chunks(li, size), _to_chunks(lf, size))
    state, h = lax.scan(step, state, xs)
    return _from_chunks(h), state


def _mlstm_zero(b_):
    return (jnp.zeros((b_, ML_HEADS, ML_DH, ML_DH), F32), jnp.zeros((b_, ML_HEADS, ML_DH), F32),
            jnp.zeros((b_, ML_HEADS), F32))


def _mlstm_stream(cols, gate_b, init_f, init_b):
    q = _heads(cols['ml_q'], ML_HEADS).astype(F32)
    k = _heads(cols['ml_k'], ML_HEADS).astype(F32) * (ML_DH ** -0.5)
    v = _heads(cols['ml_v'], ML_HEADS).astype(F32)

    def gate(name, j):
        return jnp.swapaxes(cols[name].astype(F32) + gate_b[j].astype(F32), 1, 2)

    li_f, lf_f = gate('ml_if', 0), jax.nn.log_sigmoid(gate('ml_ff', 1))
    li_b, lf_b = gate('ml_ib', 2), jax.nn.log_sigmoid(gate('ml_fb', 3))
    h_f, st_f = _mlstm_scan(q, k, v, li_f, lf_f, init_f)
    h_b, st_b = _mlstm_scan(_flip(q), _flip(k), _flip(v), _flip(li_b), _flip(lf_b), init_b)
    return h_f + _flip(h_b), st_f, st_b


def _mlstm_finish(h, o_pre, norm_g):
    hn = _merge_heads(_layernorm(h)) * norm_g.astype(F32)
    return hn.astype(o_pre.dtype) * jax.nn.sigmoid(o_pre)


def _fourier(u):
    b_, t, _ = u.shape
    ug = u.astype(F32).reshape(b_, t, FT_GROUPS, FT_GC)
    y = jnp.fft.fft2(ug, axes=(1, 3), norm='ortho').real
    return y.reshape(b_, t, FT_W).astype(u.dtype)


def _rope_tables(rows, dtype):
    row = jnp.repeat(jnp.arange(rows, dtype=F32), GRID_W)
    col = jnp.tile(jnp.arange(GRID_W, dtype=F32), rows)
    inv = jnp.power(ROPE_BASE, -jnp.arange(ROPE_PAIRS, dtype=F32) / ROPE_PAIRS)
    ang = jnp.stack([row[:, None] * inv, col[:, None] * inv], axis=1)
    return jnp.cos(ang).astype(dtype), jnp.sin(ang).astype(dtype)


def _rope(x, cos, sin):
    xs = x.reshape(x.shape[:-1] + (2, 2, ROPE_PAIRS))
    x1, x2 = xs[..., 0, :], xs[..., 1, :]
    out = jnp.stack([x1 * cos - x2 * sin, x1 * sin + x2 * cos], axis=-2)
    return out.reshape(x.shape)


def _gqa_q(cols, g):
    b_, t, _ = cols['gq_q'].shape
    q = cols['gq_q'].reshape(b_, t, GQ_KV, GQ_G, GQ_DH).transpose(0, 2, 3, 1, 4)
    return _rms(q) * g


def _gqa_kv(cols, g):
    return _rms(_heads(cols['gq_k'], GQ_KV)) * g, _heads(cols['gq_v'], GQ_KV)


def _gqa_attend(q, k, v):
    b_, kv, g, t, dh = q.shape
    nb = t // GQ_BLOCK
    qb = jnp.moveaxis(q.reshape(b_, kv, g, nb, GQ_BLOCK, dh), 3, 0)

    def attend(qblk):
        s = jnp.einsum('bkgqd,bksd->bkgqs', qblk, k).astype(F32) * (GQ_DH ** -0.5)
        p = jax.nn.softmax(s, axis=-1).astype(v.dtype)
        return jnp.einsum('bkgqs,bksd->bkgqd', p, v)

    o = jnp.moveaxis(lax.map(attend, qb), 0, 3).reshape(b_, kv, g, t, dh)
    return o.transpose(0, 3, 1, 2, 4).reshape(b_, t, kv * g * dh)


def _gla_scan(q, k, v, la, s0):
    size = GL_CHUNK
    causal = jnp.tril(jnp.ones((size, size), dtype=bool))[:, :, None]

    def step(s, inp):
        qc, kc, vc, ac = inp
        g = jnp.cumsum(ac, axis=2)
        diff = jnp.where(causal, g[:, :, :, None, :] - g[:, :, None, :, :], -jnp.inf)
        a = jnp.einsum('bhtk,bhsk,bhtsk->bhts', qc, kc, jnp.exp(diff))
        o = (jnp.einsum('bhts,bhsv->bhtv', a, vc)
             + jnp.einsum('bhtk,bhkv->bhtv', qc * jnp.exp(g), s))
        g_last = g[:, :, -1:, :]
        s = (jnp.exp(g_last[:, :, 0, :])[..., None] * s
             + jnp.einsum('bhsk,bhsv->bhkv', kc * jnp.exp(g_last - g), vc))
        return s, o

    xs = (_to_chunks(q, size), _to_chunks(k, size), _to_chunks(v, size), _to_chunks(la, size))
    s, o = lax.scan(step, s0, xs)
    return _from_chunks(o), s


def _gla_zero(b_):
    return jnp.zeros((b_, GL_HEADS, GL_DK, GL_DV), F32)


def _gla_stream(cols, w2, b2, init_f, init_b):
    q = _heads(cols['gl_q'], GL_HEADS).astype(F32) * (GL_DK ** -0.5)
    k = _heads(cols['gl_k'], GL_HEADS).astype(F32)
    v = _heads(cols['gl_v'], GL_HEADS).astype(F32)

    def log_decay(name, j):
        a = (cols[name] @ w2[j] + b2[j]).astype(F32)
        return _heads(jax.nn.log_sigmoid(a) / GL_TAU, GL_HEADS)

    o_f, s_f = _gla_scan(q, k, v, log_decay('gl_af', 0), init_f)
    o_b, s_b = _gla_scan(_flip(q), _flip(k), _flip(v), _flip(log_decay('gl_ab', 1)), init_b)
    return o_f + _flip(o_b), s_f, s_b


def _gla_finish(o, r_pre, norm_g):
    on = _merge_heads(_rms(o)) * norm_g.astype(F32)
    return on.astype(r_pre.dtype) * jax.nn.silu(r_pre)


def _merge_branches(h, branches, w_branch, w_gate, b_gate, w_out):
    gates = jax.nn.sigmoid(h @ w_gate + b_gate)
    mixed = None
    off = 0
    for j, br in enumerate(branches):
        wd = br.shape[-1]
        u = gates[..., j * D_MODEL:(j + 1) * D_MODEL] * (br @ w_branch[off:off + wd])
        mixed = u if mixed is None else mixed + u
        off += wd
    return mixed @ w_out


def _swiglu(x, wg, wu, wd):
    return (jax.nn.silu(x @ wg) * (x @ wu)) @ wd


def _moe(x, wr, br, wg, wu, wd):
    logits = (x @ wr).astype(F32)
    _, idx = lax.top_k(logits + br.astype(F32), TOP_K)
    w = jax.nn.softmax(jnp.take_along_axis(logits, idx, axis=-1), axis=-1)
    dense = jnp.sum(jax.nn.one_hot(idx, N_EXPERTS, dtype=F32) * w[..., None], axis=-2).astype(x.dtype)
    out = dense[..., 0:1] * _swiglu(x, wg[0], wu[0], wd[0])
    for e in range(1, N_EXPERTS):
        out = out + dense[..., e:e + 1] * _swiglu(x, wg[e], wu[e], wd[e])
    return out


def setup_inputs(seed: int = 0) -> dict:
    key = jax.random.key(seed)
    keys = iter(jax.random.split(key, 48))

    def nrm(shape, scale):
        return jax.random.normal(next(keys), shape, jnp.float32) * scale

    d = D_MODEL
    nl = DEPTH
    n_dense = (DEPTH + 1) // 2
    n_moe = DEPTH // 2
    f_bias = jnp.linspace(3.0, 6.0, ML_HEADS, dtype=jnp.float32)
    z_bias = jnp.zeros((ML_HEADS,), jnp.float32)
    gate_base = jnp.stack([z_bias, f_bias, z_bias, f_bias])
    w_branch = jnp.concatenate([nrm((nl, w, d), w ** -0.5) for w in BRANCH_WIDTHS], axis=1)
    return {
        'x': nrm((BATCH, SEQ, d), 1.0),
        'c': nrm((BATCH, d), 1.0),
        'ctx': nrm((BATCH, CTX_LEN, d), 1.0),
        'c_ctx': nrm((d,), 1.0),
        'w_ada': nrm((nl, d, N_ADA * d), 0.5 * d ** -0.5),
        'b_ada': nrm((nl, N_ADA * d), 0.01),
        'w_in': nrm((nl, d, N_IN), d ** -0.5),
        'ml_gate_b': gate_base + nrm((nl, 4, ML_HEADS), 0.1),
        'ml_norm_g': 1.0 + nrm((nl, ML_W), 0.02),
        'gq_qnorm_g': 1.0 + nrm((nl, GQ_DH), 0.02),
        'gq_knorm_g': 1.0 + nrm((nl, GQ_DH), 0.02),
        'gl_w2': nrm((nl, 2, GL_RANK, GL_HEADS * GL_DK), GL_RANK ** -0.5),
        'gl_b2': nrm((nl, 2, GL_HEADS * GL_DK), 0.1),
        'gl_norm_g': 1.0 + nrm((nl, GL_W), 0.02),
        'w_branch': w_branch,
        'w_gate': nrm((nl, d, N_BRANCH * d), d ** -0.5),
        'b_gate': nrm((nl, N_BRANCH * d), 0.01),
        'w_out': nrm((nl, d, d), DEEPNORM_BETA * d ** -0.5),
        'ln1_g': 1.0 + nrm((nl, d), 0.02),
        'ln1_b': nrm((nl, d), 0.01),
        'ln2_g': 1.0 + nrm((nl, d), 0.02),
        'ln2_b': nrm((nl, d), 0.01),
        'ffd_wg': nrm((n_dense, d, FF_DENSE), d ** -0.5),
        'ffd_wu': nrm((n_dense, d, FF_DENSE), d ** -0.5),
        'ffd_wd': nrm((n_dense, FF_DENSE, d), DEEPNORM_BETA * FF_DENSE ** -0.5),
        'moe_wr': nrm((n_moe, d, N_EXPERTS), d ** -0.5),
        'moe_br': nrm((n_moe, N_EXPERTS), 0.01),
        'moe_wg': nrm((n_moe, N_EXPERTS, d, FF_EXPERT), d ** -0.5),
        'moe_wu': nrm((n_moe, N_EXPERTS, d, FF_EXPERT), d ** -0.5),
        'moe_wd': nrm((n_moe, N_EXPERTS, FF_EXPERT, d), DEEPNORM_BETA * FF_EXPERT ** -0.5),
    }


def reference(x, c, ctx, c_ctx, w_ada, b_ada, w_in, ml_gate_b, ml_norm_g, gq_qnorm_g, gq_knorm_g,
              gl_w2, gl_b2, gl_norm_g, w_branch, w_gate, b_gate, w_out, ln1_g, ln1_b, ln2_g, ln2_b,
              ffd_wg, ffd_wu, ffd_wd, moe_wr, moe_br, moe_wg, moe_wu, moe_wd):
    b_ = x.shape[0]
    rows = x.shape[1] // GRID_W
    cos, sin = _rope_tables(rows, x.dtype)
    alpha = DEEPNORM_ALPHA
    xc = ctx
    for l in range(DEPTH):
        last = l == DEPTH - 1
        ada = (jax.nn.silu(c) @ w_ada[l] + b_ada[l]).reshape(b_, N_ADA, 1, D_MODEL)
        ada_c = (jax.nn.silu(c_ctx) @ w_ada[l] + b_ada[l]).reshape(N_ADA, 1, D_MODEL)

        h = _modulate(x, ada[:, 0], ada[:, 1])
        hc = _modulate(xc, ada_c[0], ada_c[1])
        pl = _split_cols(h @ w_in[l])
        pc = _split_cols(hc @ w_in[l])

        ml_c, ml_sf, ml_sb = _mlstm_stream(pc, ml_gate_b[l], _mlstm_zero(b_), _mlstm_zero(b_))
        gl_c, gl_sf, gl_sb = _gla_stream(pc, gl_w2[l], gl_b2[l], _gla_zero(b_), _gla_zero(b_))
        k_c, v_c = _gqa_kv(pc, gq_knorm_g[l])

        ml_l, _, _ = _mlstm_stream(pl, ml_gate_b[l], ml_sf, ml_sb)
        gl_l, _, _ = _gla_stream(pl, gl_w2[l], gl_b2[l], gl_sf, gl_sb)
        k_l, v_l = _gqa_kv(pl, gq_knorm_g[l])
        q_l = _rope(_gqa_q(pl, gq_qnorm_g[l]), cos, sin)
        att_l = _gqa_attend(q_l, jnp.concatenate([_rope(k_l, cos, sin), k_c], axis=2),
                            jnp.concatenate([v_l, v_c], axis=2))
        br_l = [_mlstm_finish(ml_l, pl['ml_o'], ml_norm_g[l]), _fourier(pl['ft']), att_l,
                _gla_finish(gl_l, pl['gl_r'], gl_norm_g[l])]
        y = _merge_branches(h, br_l, w_branch[l], w_gate[l], b_gate[l], w_out[l])
        x_mid = _post_ln(alpha * x + ada[:, 2] * y, ln1_g[l], ln1_b[l])

        if not last:
            att_c = _gqa_attend(_gqa_q(pc, gq_qnorm_g[l]), k_c, v_c)
            br_c = [_mlstm_finish(ml_c, pc['ml_o'], ml_norm_g[l]), _fourier(pc['ft']), att_c,
                    _gla_finish(gl_c, pc['gl_r'], gl_norm_g[l])]
            yc = _merge_branches(hc, br_c, w_branch[l], w_gate[l], b_gate[l], w_out[l])
            xc = _post_ln(alpha * xc + ada_c[2] * yc, ln1_g[l], ln1_b[l])

        h2 = _modulate(x_mid, ada[:, 3], ada[:, 4])
        if l % 2 == 0:
            j = l // 2
            f = _swiglu(h2, ffd_wg[j], ffd_wu[j], ffd_wd[j])
            if not last:
                h2c = _modulate(xc, ada_c[3], ada_c[4])
                fc = _swiglu(h2c, ffd_wg[j], ffd_wu[j], ffd_wd[j])
                xc = _post_ln(alpha * xc + ada_c[5] * fc, ln2_g[l], ln2_b[l])
        else:
            j = l // 2
            f = _moe(h2, moe_wr[j], moe_br[j], moe_wg[j], moe_wu[j], moe_wd[j])
            if not last:
                h2c = _modulate(xc, ada_c[3], ada_c[4])
                fc = _moe(h2c, moe_wr[j], moe_br[j], moe_wg[j], moe_wu[j], moe_wd[j])
                xc = _post_ln(alpha * xc + ada_c[5] * fc, ln2_g[l], ln2_b[l])
        x = _post_ln(alpha * x_mid + ada[:, 5] * f, ln2_g[l], ln2_b[l])
    return x
```

```python
import numpy as np
from contextlib import ExitStack
import concourse.bass as bass
import concourse.mybir as mybir
from concourse.bass_utils import run_bass_kernel_spmd

F32 = mybir.dt.float32
BF16 = mybir.dt.bfloat16
AF = mybir.ActivationFunctionType
ALU = mybir.AluOpType
AX = mybir.AxisListType

ENG = ('pe', 'act', 'dve', 'pool', 'sp')
SAME_SYNC = {'pe': False, 'act': True, 'dve': True, 'pool': True, 'sp': False}


class Dep:
    __slots__ = ('w', 'r')

    def __init__(self):
        self.w = {}
        self.r = {}


class Sched:
    def __init__(self, nc, ndma=8):
        self.nc = nc
        self.ndma = ndma
        self.prog = {e: [] for e in ENG}
        self.cnt = {e: 0 for e in ENG}
        self.seen = {e: {} for e in ENG}
        self.dcnt = {e: 0 for e in ENG}
        self.stack = ExitStack()
        self.esem = {e: self.stack.enter_context(nc.semaphore("s_" + e)) for e in ('pe', 'act', 'dve', 'pool')}
        self.dsem = {q: [self.stack.enter_context(nc.semaphore("d_%s%d" % (q, i))) for i in range(ndma)]
                     for q in ('sp', 'pool', 'act')}
        self.csem = self.stack.enter_context(nc.semaphore("c_cc"))
        self.ccnt = 0

    def sb(self, shape, dt, name=None):
        t = self.stack.enter_context(self.nc.sbuf_tensor(name, list(shape), dt) if name else self.nc.sbuf_tensor(list(shape), dt))
        return t

    def ps(self, shape, dt=F32, name=None):
        full = 512 if dt == F32 else 1024
        t = self.stack.enter_context(self.nc.psum_tensor([128, full], dt))
        shape = list(shape)
        n = 1
        for d in shape[1:]:
            n *= d
        assert n <= full
        v = t[0:shape[0], 0:n]
        if len(shape) == 3:
            v = v.rearrange("p (a b) -> p a b", a=shape[1])
        return v

    def _need(self, reads, writes):
        need = {}
        for d in reads:
            for k, v in d.w.items():
                if need.get(k, 0) < v:
                    need[k] = v
        for d in writes:
            for k, v in d.w.items():
                if need.get(k, 0) < v:
                    need[k] = v
            for k, v in d.r.items():
                if need.get(k, 0) < v:
                    need[k] = v
        return need

    def op(self, eng, fn, reads=(), writes=()):
        need = self._need(reads, writes)
        mykey = ('e', eng)
        waits = []
        seen = self.seen[eng]
        for k, v in need.items():
            if k == mykey and not SAME_SYNC[eng]:
                continue
            if seen.get(k, 0) < v:
                seen[k] = v
                waits.append((k, v))
        self.cnt[eng] += 1
        n = self.cnt[eng]
        for d in writes:
            d.w = {mykey: n}
            d.r = {}
        for d in reads:
            if d not in writes:
                d.r[mykey] = n
        self.prog[eng].append((waits, fn, mykey))

    def dma(self, q, out, in_, reads=(), writes=(), **kw):
        need = self._need(reads, writes)
        i = self.dcnt[q] % self.ndma
        gen = self.dcnt[q] // self.ndma
        self.dcnt[q] += 1
        key = ('d', q, i)
        val = 16 * (gen + 1)
        if gen > 0:
            need[key] = max(need.get(key, 0), 16 * gen)
        waits = []
        seen = self.seen[q]
        for k, v in need.items():
            if seen.get(k, 0) < v:
                seen[k] = v
                waits.append((k, v))
        for d in writes:
            d.w = {key: val}
            d.r = {}
        for d in reads:
            if d not in writes:
                d.r[key] = val
        self.prog[q].append((waits, (lambda e, out=out, in_=in_, kw=kw: e.dma_start(out=out, in_=in_, **kw)), key))

    def cc(self, kind, out, in_, groups, reads=(), writes=()):
        q = 'pool'
        need = self._need(reads, writes)
        self.ccnt += 1
        key = ('c',)
        val = self.ccnt
        waits = []
        seen = self.seen[q]
        for k, v in need.items():
            if seen.get(k, 0) < v:
                seen[k] = v
                waits.append((k, v))
        for d in writes:
            d.w = {key: val}
            d.r = {}
        for d in reads:
            if d not in writes:
                d.r[key] = val
        self.prog[q].append((waits, (lambda e: e.collective_compute(kind, ALU.bypass, replica_groups=groups, ins=[in_.opt()], outs=[out.opt()])), key))

    def barrier(self):
        allk = {}
        for e in ('pe', 'act', 'dve', 'pool'):
            if self.cnt[e]:
                allk[('e', e)] = self.cnt[e]
        for q in ('sp', 'pool', 'act'):
            for j in range(min(self.dcnt[q], self.ndma)):
                ngen = (self.dcnt[q] - 1 - j) // self.ndma + 1
                allk[('d', q, j)] = 16 * ngen
        if self.ccnt:
            allk[('c',)] = self.ccnt
        for e in ENG:
            waits = []
            for k, v in allk.items():
                if self.seen[e].get(k, 0) < v:
                    self.seen[e][k] = v
                    waits.append((k, v))
            if waits:
                self.prog[e].append((waits, None, None))

    def _sem(self, k):
        if k[0] == 'e':
            return self.esem[k[1]]
        if k[0] == 'c':
            return self.csem
        return self.dsem[k[1]][k[2]]

    def emit(self):
        self.barrier()
        nc = self.nc
        prog = self.prog
        sched = self

        def replay(e, name):
            for waits, fn, key in prog[name]:
                for k, v in waits:
                    e.wait_ge(sched._sem(k), v)
                if fn is None:
                    continue
                ins = fn(e)
                if key[0] == 'e':
                    ins.then_inc(sched.esem[name], 1)
                elif key[0] == 'c':
                    ins.then_inc(sched.csem, 1)
                else:
                    ins.then_inc(sched.dsem[key[1]][key[2]], 16)

        with nc.Block() as block:
            @block.tensor
            def _(e):
                replay(e, 'pe')

            @block.scalar
            def _(e):
                replay(e, 'act')

            @block.vector
            def _(e):
                replay(e, 'dve')

            @block.gpsimd
            def _(e):
                replay(e, 'pool')

            @block.sync
            def _(e):
                replay(e, 'sp')
        self.stack.close()


TL = 16384
TC = 256
TA = TL + TC
NCH = TA // 128
D = 1024
NTM = 644
NFM = 352
MLK, MLV, MLO, GAT, GQQ, GQK, GQV, GLK, GLV, GLR = 0, 64, 128, 192, 196, 324, 388, 452, 516, 580
LN_EPS = 1e-6


class Scope:
    def __init__(self, S):
        self.S = S

    def __enter__(self):
        self.saved = self.S.stack
        self.S.stack = ExitStack()
        return self

    def __exit__(self, *a):
        self.S.barrier()
        self.S.stack.close()
        self.S.stack = self.saved
        return False


def emit_A1(S, hT, wtm, wfm, PTM, PFM, hT_deps=()):
    with Scope(S):
        Wtm = S.sb([128, 8, NTM], BF16)
        Wfm = S.sb([128, 8, NFM], BF16)
        dWt, dWf = Dep(), Dep()
        S.dma('pool', Wtm[:], wtm.rearrange("(kc p) n -> p kc n", p=128), writes=[dWt])
        S.dma('pool', Wfm[:], wfm.rearrange("(kc p) n -> p kc n", p=128), writes=[dWf])
        hb = [S.sb([128, 8, 512], BF16) for _ in range(2)]
        dh = [Dep(), Dep()]
        psA = [S.ps([128, 512]) for _ in range(2)]
        psB = [S.ps([128, 512]) for _ in range(2)]
        dpA = [Dep(), Dep()]
        dpB = [Dep(), Dep()]
        psF = [S.ps([128, 512]) for _ in range(3)]
        dpF = [Dep() for _ in range(3)]
        stg = [S.sb([128, NTM], F32) for _ in range(2)]
        dstg = [Dep(), Dep()]
        stf = [S.sb([128, 512], F32) for _ in range(3)]
        dstf = [Dep() for _ in range(3)]
        hT_fn = hT if callable(hT) else (lambda t0, W, hTv=hT.rearrange("(kc p) t -> p kc t", p=128): hTv[:, :, t0:t0 + W])
        frows = [(0, 128), (128, 128), (256, 96)]
        tiles = [(0, 256)] + [(256 + 512 * j, 512) for j in range(32)]
        nsub = 0
        for st, (t0, W) in enumerate(tiles):
            b = st % 2
            src = hT_fn(t0, W)
            if isinstance(src, list):
                for hf in range(2):
                    S.dma('sp', hb[b][hf * 64:(hf + 1) * 64, :, 0:W], src[hf], writes=[dh[b]], reads=list(hT_deps))
            else:
                S.dma('sp', hb[b][:, :, 0:W], src, writes=[dh[b]], reads=list(hT_deps))
            for sub in range(W // 128):
                pb = nsub % 2
                nsub += 1
                for kc in range(8):
                    S.op('pe', lambda e, pb=pb, b=b, kc=kc, sub=sub: e.matmul(
                        psA[pb][:, :], hb[b][:, kc, sub * 128:(sub + 1) * 128], Wtm[:, kc, 0:512],
                        start=(kc == 0), stop=(kc == 7)), reads=[dh[b], dWt], writes=[dpA[pb]])
                for kc in range(8):
                    S.op('pe', lambda e, pb=pb, b=b, kc=kc, sub=sub: e.matmul(
                        psB[pb][:, 0:NTM - 512], hb[b][:, kc, sub * 128:(sub + 1) * 128], Wtm[:, kc, 512:NTM],
                        start=(kc == 0), stop=(kc == 7)), reads=[dh[b], dWt], writes=[dpB[pb]])
                S.op('act', lambda e, pb=pb: e.activation(out=stg[pb][:, 0:512], in_=psA[pb][:, :], func=AF.Copy),
                     reads=[dpA[pb]], writes=[dstg[pb]])
                S.op('dve', lambda e, pb=pb: e.tensor_copy(out=stg[pb][:, 512:NTM], in_=psB[pb][:, 0:NTM - 512]),
                     reads=[dpB[pb]], writes=[dstg[pb]])
                r0 = t0 + sub * 128
                S.dma('sp', PTM[r0:r0 + 128, :], stg[pb][:, :], reads=[dstg[pb]])
            for g, (f0, fn) in enumerate(frows):
                for kc in range(8):
                    S.op('pe', lambda e, g=g, b=b, kc=kc, f0=f0, fn=fn, W=W: e.matmul(
                        psF[g][0:fn, 0:W], Wfm[:, kc, f0:f0 + fn], hb[b][:, kc, 0:W],
                        start=(kc == 0), stop=(kc == 7)), reads=[dh[b], dWf], writes=[dpF[g]])
                eng = 'act' if g == 1 else 'dve'
                if eng == 'act':
                    S.op('act', lambda e, g=g, fn=fn, W=W: e.activation(out=stf[g][0:fn, 0:W], in_=psF[g][0:fn, 0:W], func=AF.Copy),
                         reads=[dpF[g]], writes=[dstf[g]])
                else:
                    S.op('dve', lambda e, g=g, fn=fn, W=W: e.tensor_copy(out=stf[g][0:fn, 0:W], in_=psF[g][0:fn, 0:W]),
                         reads=[dpF[g]], writes=[dstf[g]])
                S.dma('pool', PFM[f0:f0 + fn, t0:t0 + W], stf[g][0:fn, 0:W], reads=[dstf[g]])


def emit_A4(S, PTM, BR, cos3, sin3, gq, gk, ident, with_ctx_q):
    with Scope(S):
        QK = S.sb([128, 2, TA], BF16)
        VP = S.sb([128, NCH, 65], BF16)
        identb = S.sb([128, 128], BF16)
        identf = S.sb([128, 128], F32)
        Gq = S.sb([128, 64], F32)
        Gk = S.sb([128, 64], F32)
        dC = Dep()
        S.dma('pool', identb[:], ident, writes=[dC])
        S.dma('sp', identf[:], ident, writes=[dC])
        S.dma('sp', Gq[:], gq.partition_broadcast(128), writes=[dC])
        S.dma('sp', Gk[:], gk.partition_broadcast(128), writes=[dC])
        dQK, dVP = Dep(), Dep()
        S.op('pool', lambda e: e.memset(VP[:, :, 64:65], 1.0), writes=[dVP])
        with Scope(S):
            xin = [S.sb([128, 256], F32) for _ in range(2)]
            cs = [S.sb([128, 192], F32) for _ in range(2)]
            sn = [S.sb([128, 192], F32) for _ in range(2)]
            dx = [Dep(), Dep()]
            sq_ = [S.sb([128, 192], F32) for _ in range(2)]
            ss_ = [S.sb([128, 3], F32) for _ in range(2)]
            rstd_ = [S.sb([128, 3], F32) for _ in range(2)]
            xn_ = [S.sb([128, 192], F32) for _ in range(2)]
            t1_ = [S.sb([128, 192], F32) for _ in range(2)]
            t2_ = [S.sb([128, 192], F32) for _ in range(2)]
            xr = [S.sb([128, 256], BF16) for _ in range(2)]
            dxr = [Dep(), Dep()]
            dsq_, dss_, drs_, dxn_, dt1_, dt2_ = [[Dep(), Dep()] for _ in range(6)]
            psT = [S.ps([128, 2, 128], BF16) for _ in range(2)]
            dpsT = [Dep(), Dep()]
            for c in range(NCH):
                b = c % 2
                r0 = c * 128
                sq, ss, rstd, xn, t1, t2 = sq_[b], ss_[b], rstd_[b], xn_[b], t1_[b], t2_[b]
                dsq, dss, drs, dxn, dt1, dt2 = dsq_[b], dss_[b], drs_[b], dxn_[b], dt1_[b], dt2_[b]
                lat = c >= 2
                S.dma('sp', xin[b][:], PTM[r0:r0 + 128, GQQ:GQQ + 256], writes=[dx[b]])
                if lat:
                    S.dma('sp', cs[b][:], cos3[r0 - TC:r0 - TC + 128, :], writes=[dx[b]])
                    S.dma('sp', sn[b][:], sin3[r0 - TC:r0 - TC + 128, :], writes=[dx[b]])
                S.op('pool', lambda e, sq=sq, ss=ss, rstd=rstd, xn=xn, t1=t1, t2=t2, b=b: e.tensor_tensor(out=sq[:], in0=xin[b][:, 0:192], in1=xin[b][:, 0:192], op=ALU.mult),
                     reads=[dx[b]], writes=[dsq])
                S.op('dve', lambda e, sq=sq, ss=ss, rstd=rstd, xn=xn, t1=t1, t2=t2: e.tensor_reduce(out=ss[:], in_=sq[:].rearrange("p (h d) -> p h d", h=3), axis=AX.X, op=ALU.add),
                     reads=[dsq], writes=[dss])
                S.op('act', lambda e, sq=sq, ss=ss, rstd=rstd, xn=xn, t1=t1, t2=t2: e.activation(out=rstd[:], in_=ss[:], func=AF.Sqrt, bias=LN_EPS, scale=1.0 / 64),
                     reads=[dss], writes=[drs])
                S.op('dve', lambda e, sq=sq, ss=ss, rstd=rstd, xn=xn, t1=t1, t2=t2: e.reciprocal(out=rstd[:], in_=rstd[:]),
                     reads=[drs], writes=[drs])
                for hh in range(3):
                    G = Gq if hh < 2 else Gk
                    dst = xn if lat else xr[b]
                    S.op('dve', lambda e, sq=sq, ss=ss, rstd=rstd, xn=xn, t1=t1, t2=t2, hh=hh, G=G, dst=dst, b=b: e.scalar_tensor_tensor(
                        out=dst[:, hh * 64:(hh + 1) * 64], in0=xin[b][:, hh * 64:(hh + 1) * 64], scalar=rstd[:, hh:hh + 1],
                        in1=G[:], op0=ALU.mult, op1=ALU.mult), reads=[dx[b], drs, dC], writes=[dxn if lat else dxr[b]])
                if lat:
                    S.op('pool', lambda e, sq=sq, ss=ss, rstd=rstd, xn=xn, t1=t1, t2=t2, b=b: e.tensor_tensor(out=t1[:], in0=xn[:], in1=cs[b][:], op=ALU.mult),
                         reads=[dxn, dx[b]], writes=[dt1])
                    xv = xn[:].rearrange("p (g f s) -> p g f s", g=6, f=2)
                    for f in range(2):
                        S.op('dve', lambda e, sq=sq, ss=ss, rstd=rstd, xn=xn, t1=t1, t2=t2, f=f, b=b, xv=xv: e.tensor_tensor(
                            out=t2[:].rearrange("p (g f s) -> p g f s", g=6, f=2)[:, :, f, :],
                            in0=xv[:, :, 1 - f, :],
                            in1=sn[b][:].rearrange("p (g f s) -> p g f s", g=6, f=2)[:, :, f, :], op=ALU.mult),
                            reads=[dxn, dx[b]], writes=[dt2])
                    S.op('dve', lambda e, sq=sq, ss=ss, rstd=rstd, xn=xn, t1=t1, t2=t2, b=b: e.tensor_tensor(out=xr[b][:, 0:192], in0=t1[:], in1=t2[:], op=ALU.add),
                         reads=[dt1, dt2], writes=[dxr[b]])
                S.op('act', lambda e, sq=sq, ss=ss, rstd=rstd, xn=xn, t1=t1, t2=t2, b=b: e.activation(out=xr[b][:, 192:256], in_=xr[b][:, 128:192], func=AF.Copy),
                     reads=[dxr[b]], writes=[dxr[b]])
                S.op('act', lambda e, sq=sq, ss=ss, rstd=rstd, xn=xn, t1=t1, t2=t2, b=b, c=c: e.activation(out=VP[:, c, 0:64], in_=xin[b][:, 192:256], func=AF.Copy),
                     reads=[dx[b]], writes=[dVP])
                for j in range(2):
                    S.op('pe', lambda e, sq=sq, ss=ss, rstd=rstd, xn=xn, t1=t1, t2=t2, b=b, j=j: e.transpose(psT[b][:, j, :], xr[b][:, j * 128:(j + 1) * 128], identb[:]),
                         reads=[dxr[b], dC], writes=[dpsT[b]])
                S.op('act', lambda e, sq=sq, ss=ss, rstd=rstd, xn=xn, t1=t1, t2=t2, b=b, r0=r0: e.activation(out=QK[:, :, r0:r0 + 128], in_=psT[b][:, :, :], func=AF.Copy),
                     reads=[dpsT[b]], writes=[dQK])
        with Scope(S):
            psS = [S.ps([128, 512]) for _ in range(4)]
            dS_ = [Dep() for _ in range(4)]
            Pb = [S.sb([128, 512], BF16) for _ in range(4)]
            dP = [Dep() for _ in range(4)]
            psO = [S.ps([128, 512]) for _ in range(2)]
            dO = [Dep(), Dep()]
            OTs = S.sb([128, 512], F32)
            dOT = Dep()
            psT2 = S.ps([128, 4, 65])
            dT2 = Dep()
            rec = S.sb([128, 4], F32)
            drec = Dep()
            ostg = [S.sb([128, 4, 64], BR.dtype) for _ in range(2)]
            dos = [Dep(), Dep()]
            blocks = []
            if with_ctx_q:
                blocks.append((0, 256, 2))
            for qb in range(32):
                blocks.append((TC + qb * 512, 512, NCH))
            pairs = []
            for bi, (t0, W, nk) in enumerate(blocks):
                for j in range(nk):
                    pairs.append((bi, t0, W, nk, j))

            def emit_S(p):
                bi, t0, W, nk, j = pairs[p]
                for hh in range(2):
                    s = (2 * p + hh) % 4
                    S.op('pe', lambda e, s=s, hh=hh, j=j, t0=t0, W=W: e.matmul(psS[s][:, 0:W], QK[hh * 64:(hh + 1) * 64, 1, j * 128:(j + 1) * 128],
                                                                              QK[hh * 64:(hh + 1) * 64, 0, t0:t0 + W], start=True, stop=True),
                         reads=[dQK], writes=[dS_[s]])

            def finish(hh, t0, W):
                S.op('dve', lambda e: e.tensor_copy(out=OTs[0:65, 0:W], in_=psO[hh][0:65, 0:W]), reads=[dO[hh]], writes=[dOT])
                ns = W // 128
                for s in range(ns):
                    S.op('pe', lambda e, s=s: e.transpose(psT2[:, s, :], OTs[0:65, s * 128:(s + 1) * 128], identf[0:65, 0:65]),
                         reads=[dOT, dC], writes=[dT2])
                S.op('dve', lambda e: e.reciprocal(out=rec[:, 0:ns], in_=psT2[:, 0:ns, 64]), reads=[dT2], writes=[drec])
                for s in range(ns):
                    S.op('dve', lambda e, s=s: e.tensor_scalar(out=ostg[hh][:, s, :], in0=psT2[:, s, 0:64], scalar1=rec[:, s:s + 1],
                                                               scalar2=None, op0=ALU.mult), reads=[dT2, drec], writes=[dos[hh]])
                S.dma('sp', BR[t0:t0 + W, 128 + hh * 64:128 + hh * 64 + 64].rearrange("(s p) n -> p s n", p=128),
                      ostg[hh][:, 0:ns, :], reads=[dos[hh]])

            emit_S(0)
            for p in range(len(pairs)):
                bi, t0, W, nk, j = pairs[p]
                if p + 1 < len(pairs):
                    emit_S(p + 1)
                for hh in range(2):
                    s = (2 * p + hh) % 4
                    S.op('act', lambda e, s=s, W=W: e.activation(out=Pb[s][:, 0:W], in_=psS[s][:, 0:W], func=AF.Exp, scale=0.125),
                         reads=[dS_[s]], writes=[dP[s]])
                for hh in range(2):
                    s = (2 * p + hh) % 4
                    S.op('pe', lambda e, s=s, W=W, j=j, nk=nk, hh=hh: e.matmul(psO[hh][0:65, 0:W], VP[:, j, 0:65], Pb[s][:, 0:W],
                                                                              start=(j == 0), stop=(j == nk - 1)),
                         reads=[dP[s], dVP], writes=[dO[hh]])
                if j == nk - 1:
                    for hh in range(2):
                        finish(hh, t0, W)


def emit_A2(S, PTM, PFM, BR, gate_b, norm_g, tri, ones):
    LN8 = float(np.log(8.0))
    with Scope(S):
        Hd = [S.sb([128, NCH, 64], F32) for _ in range(2)]
        dH = [Dep(), Dep()]
        with Scope(S):
            QT = S.sb([64, TA], BF16)
            KT = S.sb([64, TA], BF16)
            Ktm = S.sb([128, NCH, 64], BF16)
            V2 = [S.sb([128, NCH, 65], BF16) for _ in range(2)]
            G = S.sb([128, NCH, 4], F32)
            gb = S.sb([128, 4], F32)
            nb = S.sb([128, 4], F32)
            bse = S.sb([128, 2], F32)
            TR = S.sb([128, 2, 128], F32)
            ON = S.sb([128, 128], F32)
            E = [S.sb([128, NCH], F32) for _ in range(2)]
            IRS = [S.sb([128, NCH], F32) for _ in range(2)]
            DEC = [S.sb([128, NCH], F32) for _ in range(2)]
            dQT, dKT, dK, dG, dc, dV2 = Dep(), Dep(), Dep(), Dep(), Dep(), [Dep(), Dep()]
            dE, dIRS, dDEC = [Dep(), Dep()], [Dep(), Dep()], [Dep(), Dep()]
            for j in range(4):
                a, bnd = j * (TA // 4), (j + 1) * (TA // 4)
                S.dma('pool', QT[:, a:bnd], PFM[0:64, a:bnd], writes=[dQT])
                S.dma('pool', KT[:, a:bnd], PFM[64:128, a:bnd], writes=[dKT])
            PTMv = PTM.rearrange("(c p) n -> p c n", p=128)
            for j in range(5):
                S.dma('pool', Ktm[:, j * 26:(j + 1) * 26, :], PTMv[:, j * 26:(j + 1) * 26, MLK:MLK + 64], writes=[dK])
            S.dma('sp', G[:], PTMv[:, :, GAT:GAT + 4], writes=[dG])
            S.dma('sp', gb[:], gate_b.partition_broadcast(128), writes=[dc])
            S.dma('sp', TR[:], tri.rearrange("a p n -> p a n"), writes=[dc])
            S.dma('sp', ON[:], ones, writes=[dc])
            S.op('dve', lambda e: e.tensor_scalar(out=nb[:], in0=gb[:], scalar1=-1.0, scalar2=None, op0=ALU.mult), reads=[dc], writes=[dc])
            S.op('dve', lambda e: e.tensor_scalar(out=bse[:], in0=gb[:].rearrange("p (a b) -> p a b", b=2)[:, :, 0], scalar1=-LN8, scalar2=None, op0=ALU.add),
                 reads=[dc], writes=[dc])
            with Scope(S):
                V = Hd[1]
                dV = Dep()
                for j in range(5):
                    S.dma('sp', V[:, j * 26:(j + 1) * 26, :], PTMv[:, j * 26:(j + 1) * 26, MLV:MLV + 64], writes=[dV])
                L = [S.sb([128, NCH], F32) for _ in range(2)]
                e1 = S.sb([128, NCH], F32)
                tmp = S.sb([128, NCH], F32)
                psC = [S.ps([128, 2, NCH]) for _ in range(2)]
                dL, de1, dtmp, dpc = [Dep(), Dep()], Dep(), Dep(), [Dep(), Dep()]
                for d in range(2):
                    gi, gf = 2 * d, 2 * d + 1
                    S.op('act', lambda e, gf=gf: e.activation(out=e1[:], in_=G[:, :, gf], func=AF.Exp, scale=-1.0, bias=nb[:, gf:gf + 1]),
                         reads=[dG, dc], writes=[de1])
                    S.op('act', lambda e, d=d: e.activation(out=L[d][:], in_=e1[:], func=AF.Ln, bias=1.0), reads=[de1], writes=[dL[d]])
                    S.op('pe', lambda e, d=d: e.matmul(psC[d][:, 0, :], TR[:, d, :], L[d][:], start=True, stop=True), reads=[dL[d], dc], writes=[dpc[d]])
                    S.op('pe', lambda e, d=d: e.matmul(psC[d][:, 1, :], ON[:], L[d][:], start=True, stop=True), reads=[dL[d], dc], writes=[dpc[d]])
                    S.op('dve', lambda e, d=d, gi=gi: e.tensor_tensor(out=tmp[:], in0=G[:, :, gi], in1=psC[d][:, 0, :], op=ALU.add),
                         reads=[dG, dpc[d]], writes=[dtmp])
                    S.op('act', lambda e, d=d: e.activation(out=E[d][:], in_=tmp[:], func=AF.Exp, bias=bse[:, d:d + 1]), reads=[dtmp, dc], writes=[dE[d]])
                    S.op('act', lambda e, d=d: e.activation(out=IRS[d][:], in_=psC[d][:, 0, :], func=AF.Exp), reads=[dpc[d]], writes=[dIRS[d]])
                    S.op('act', lambda e, d=d: e.activation(out=DEC[d][:], in_=psC[d][:, 1, :], func=AF.Exp, scale=-1.0), reads=[dpc[d]], writes=[dDEC[d]])
                    S.op('dve', lambda e, d=d: e.tensor_tensor(out=V2[d][:, :, 0:64], in0=V[:], in1=E[d][:].unsqueeze(2).to_broadcast([128, NCH, 64]), op=ALU.mult),
                         reads=[dV, dE[d]], writes=[dV2[d]])
                    S.op('pool', lambda e, d=d: e.tensor_copy(out=V2[d][:, :, 64], in_=E[d][:]), reads=[dE[d]], writes=[dV2[d]])
            with Scope(S):
                C = [S.sb([64, 65], F32) for _ in range(2)]
                Cb = [S.sb([64, 65], BF16) for _ in range(2)]
                dC_, dCb = [Dep(), Dep()], [Dep(), Dep()]
                psS = [S.ps([128, 128]) for _ in range(2)]
                psH = [S.ps([128, 65]) for _ in range(2)]
                psU = [S.ps([64, 65]) for _ in range(2)]
                dpS, dpH, dpU = [Dep(), Dep()], [Dep(), Dep()], [Dep(), Dep()]
                Sm = [S.sb([128, 128], BF16) for _ in range(2)]
                dSm = [Dep(), Dep()]
                dab = [S.sb([128, 1], F32) for _ in range(2)]
                fac = [S.sb([128, 1], F32) for _ in range(2)]
                ddab, dfac = [Dep(), Dep()], [Dep(), Dep()]
                tmpC = [S.sb([64, 65], F32) for _ in range(2)]
                dtC = [Dep(), Dep()]
                for d in range(2):
                    S.op('pool', lambda e, d=d: e.memset(C[d][:], 0.0), writes=[dC_[d]])
                    S.op('pool', lambda e, d=d: e.memset(Cb[d][:], 0.0), writes=[dCb[d]])
                order = [list(range(NCH)), [1, 0] + list(range(NCH - 1, 1, -1))]
                for k in range(NCH):
                    for d in range(2):
                        c = order[d][k]
                        sl = slice(c * 128, (c + 1) * 128)
                        S.op('pe', lambda e, d=d, sl=sl: e.matmul(psS[d][:], KT[:, sl], QT[:, sl], start=True, stop=True), reads=[dKT, dQT], writes=[dpS[d]])
                        S.op('dve', lambda e, d=d: e.tensor_tensor(out=Sm[d][:], in0=psS[d][:], in1=TR[:, d, :], op=ALU.mult), reads=[dpS[d], dc], writes=[dSm[d]])
                        S.op('pe', lambda e, d=d, c=c: e.matmul(psH[d][:], Sm[d][:], V2[d][:, c, :], start=True, stop=False), reads=[dSm[d], dV2[d]], writes=[dpH[d]])
                        S.op('pe', lambda e, d=d, sl=sl: e.matmul(psH[d][:], QT[:, sl], Cb[d][:], start=False, stop=True), reads=[dQT, dCb[d]], writes=[dpH[d]])
                        S.op('pe', lambda e, d=d, c=c: e.matmul(psU[d][:], Ktm[:, c, :], V2[d][:, c, :], start=True, stop=True), reads=[dK, dV2[d]], writes=[dpU[d]])
                        S.op('act', lambda e, d=d: e.activation(out=dab[d][:], in_=psH[d][:, 64:65], func=AF.Abs), reads=[dpH[d]], writes=[ddab[d]])
                        S.op('dve', lambda e, d=d, c=c: e.tensor_scalar(out=fac[d][:], in0=dab[d][:], scalar1=IRS[d][:, c:c + 1], scalar2=None, op0=ALU.max),
                             reads=[ddab[d], dIRS[d]], writes=[dfac[d]])
                        S.op('dve', lambda e, d=d: e.reciprocal(out=fac[d][:], in_=fac[d][:]), reads=[dfac[d]], writes=[dfac[d]])
                        S.op('act', lambda e, d=d, c=c: e.activation(out=Hd[d][:, c, :], in_=psH[d][:, 0:64], func=AF.Copy, scale=fac[d][:, 0:1]),
                             reads=[dpH[d], dfac[d]], writes=[dH[d]])
                        S.op('dve', lambda e, d=d: e.tensor_tensor(out=tmpC[d][:], in0=psU[d][:], in1=C[d][:], op=ALU.add), reads=[dpU[d], dC_[d]], writes=[dtC[d]])
                        S.op('dve', lambda e, d=d, c=c: e.tensor_scalar(out=C[d][:], in0=tmpC[d][:], scalar1=DEC[d][0:64, c:c + 1], scalar2=None, op0=ALU.mult),
                             reads=[dtC[d], dDEC[d]], writes=[dC_[d]])
                        S.op('act', lambda e, d=d, c=c: e.activation(out=Cb[d][:], in_=tmpC[d][:], func=AF.Copy, scale=DEC[d][0:64, c:c + 1]),
                             reads=[dtC[d], dDEC[d]], writes=[dCb[d]])
        with Scope(S):
            PTMv = PTM.rearrange("(c p) n -> p c n", p=128)
            O = S.sb([128, NCH, 64], F32)
            NG = S.sb([128, 64], F32)
            sm = S.sb([128, NCH], F32)
            vr = S.sb([128, NCH], F32)
            dO, dn, dsm, dvr = Dep(), Dep(), Dep(), Dep()
            for j in range(5):
                S.dma('sp', O[:, j * 26:(j + 1) * 26, :], PTMv[:, j * 26:(j + 1) * 26, MLO:MLO + 64], writes=[dO])
            S.dma('sp', NG[:], norm_g.partition_broadcast(128), writes=[dn])
            H, H2 = Hd[0], Hd[1]
            S.op('dve', lambda e: e.tensor_tensor(out=H[:], in0=H[:], in1=H2[:], op=ALU.add), reads=[dH[0], dH[1]], writes=[dH[0]])
            _finish_norm(S, H, H2, dH[0], dH[1], sm, vr, dsm, dvr, True)
            S.op('pool', lambda e: e.tensor_tensor(out=H[:], in0=H[:], in1=NG[:].unsqueeze(1).to_broadcast([128, NCH, 64]), op=ALU.mult), reads=[dH[0], dn], writes=[dH[0]])
            S.op('act', lambda e: e.activation(out=O[:], in_=O[:], func=AF.Sigmoid), reads=[dO], writes=[dO])
            Hout = H if BR.dtype == F32 else S.sb([128, NCH, 64], BR.dtype)
            S.op('dve', lambda e: e.tensor_tensor(out=Hout[:], in0=H[:], in1=O[:], op=ALU.mult), reads=[dH[0], dO], writes=[dH[0]])
            for j in range(5):
                S.dma('sp', BR.rearrange("(c p) n -> p c n", p=128)[:, j * 26:(j + 1) * 26, 0:64], Hout[:, j * 26:(j + 1) * 26, :], reads=[dH[0]])


def _finish_norm(S, H, T, dH, dT, sm, vr, dsm, dvr, center):
    bc = lambda a: a[:].unsqueeze(2).to_broadcast([128, NCH, 64])
    if center:
        S.op('dve', lambda e: e.tensor_reduce(out=sm[:], in_=H[:], axis=AX.X, op=ALU.add), reads=[dH], writes=[dsm])
        S.op('dve', lambda e: e.tensor_scalar(out=sm[:], in0=sm[:], scalar1=1.0 / 64, scalar2=None, op0=ALU.mult), reads=[dsm], writes=[dsm])
        S.op('dve', lambda e: e.tensor_tensor(out=H[:], in0=H[:], in1=bc(sm), op=ALU.subtract), reads=[dH, dsm], writes=[dH])
    S.op('pool', lambda e: e.tensor_tensor(out=T[:], in0=H[:], in1=H[:], op=ALU.mult), reads=[dH], writes=[dT])
    S.op('dve', lambda e: e.tensor_reduce(out=vr[:], in_=T[:], axis=AX.X, op=ALU.add), reads=[dT], writes=[dvr])
    S.op('act', lambda e: e.activation(out=vr[:], in_=vr[:], func=AF.Sqrt, bias=LN_EPS, scale=1.0 / 64), reads=[dvr], writes=[dvr])
    S.op('dve', lambda e: e.reciprocal(out=vr[:], in_=vr[:]), reads=[dvr], writes=[dvr])
    S.op('dve', lambda e: e.tensor_tensor(out=H[:], in0=H[:], in1=bc(vr), op=ALU.mult), reads=[dH, dvr], writes=[dH])


def emit_A3(S, PTM, PFM, BR, w2, b2, norm_g, tri):
    GW = 8
    groups = [(0, 2)] + [(2 + 8 * j, 8) for j in range(16)]
    with Scope(S):
        Hd = [S.sb([128, NCH, 64], F32) for _ in range(2)]
        dH = [Dep(), Dep()]
        PTMv = PTM.rearrange("(c p) n -> p c n", p=128)
        with Scope(S):
            TR = S.sb([128, 2, 128], F32)
            W2 = S.sb([16, 2, 64], BF16)
            B2 = S.sb([128, 2, 64], F32)
            dc = Dep()
            S.dma('sp', TR[:], tri.rearrange("a p n -> p a n"), writes=[dc])
            S.dma('pool', W2[:], w2.rearrange("a k n -> k a n"), writes=[dc])
            S.dma('sp', B2[:], b2.rearrange("a n -> (a n)").partition_broadcast(128), writes=[dc])
            St = [S.sb([64, 64], F32) for _ in range(2)]
            Sb = [S.sb([64, 64], BF16) for _ in range(2)]
            dSt, dSb = [Dep(), Dep()], [Dep(), Dep()]
            for d in range(2):
                S.op('pool', lambda e, d=d: e.memset(St[d][:], 0.0), writes=[dSt[d]])
                S.op('pool', lambda e, d=d: e.memset(Sb[d][:], 0.0), writes=[dSb[d]])
            AT = [S.sb([16, GW * 128], BF16) for _ in range(2)]
            QTf = [S.sb([64, GW * 128], F32) for _ in range(2)]
            KTf = [S.sb([64, GW * 128], F32) for _ in range(2)]
            Ktm = [S.sb([128, GW, 64], F32) for _ in range(2)]
            V = [S.sb([128, GW, 64], BF16) for _ in range(2)]
            dld = [Dep(), Dep()]
            A = [S.sb([128, GW, 64], F32) for _ in range(2)]
            dA = [Dep(), Dep()]
            EK = [S.sb([128, GW, 64], F32) for _ in range(2)]
            dEK = [Dep(), Dep()]
            Kt2 = [S.sb([128, GW, 64], BF16) for _ in range(2)]
            dKt2 = [Dep(), Dep()]
            EQ = [S.sb([64, GW * 128], F32) for _ in range(2)]
            EKT = [S.sb([64, GW * 128], F32) for _ in range(2)]
            dEQ, dEKT = [Dep(), Dep()], [Dep(), Dep()]
            QT2 = [S.sb([64, GW * 128], BF16) for _ in range(2)]
            KT2 = [S.sb([64, GW * 128], BF16) for _ in range(2)]
            dQT2, dKT2 = [Dep(), Dep()], [Dep(), Dep()]
            psA = S.ps([128, GW, 64])
            psG = S.ps([128, GW * 64])
            psGT = [S.ps([64, 4, 128]) for _ in range(2)]
            dpA, dpG, dpGT = Dep(), Dep(), [Dep(), Dep()]
            psS = S.ps([128, 128])
            psH = S.ps([128, 64])
            psU = S.ps([64, 64])
            dpS, dpH, dpU = Dep(), Dep(), Dep()
            Sm = S.sb([128, 128], BF16)
            dSm = Dep()
            tmpS = S.sb([64, 64], F32)
            dtS = Dep()
            gorder = [list(range(17)), [0] + list(range(16, 0, -1))]
            for gi in range(17):
                for d in range(2):
                    c0, gw = groups[gorder[d][gi]]
                    W = gw * 128
                    t0 = c0 * 128
                    arow = 320 + 16 * d
                    S.dma('pool', AT[d][:, 0:W], PFM[arow:arow + 16, t0:t0 + W], writes=[dld[d]])
                    S.dma('sp', QTf[d][:, 0:W], PFM[128:192, t0:t0 + W], writes=[dld[d]])
                    S.dma('sp', KTf[d][:, 0:W], PFM[192:256, t0:t0 + W], writes=[dld[d]])
                    S.dma('sp', Ktm[d][:, 0:gw, :], PTMv[:, c0:c0 + gw, GLK:GLK + 64], writes=[dld[d]])
                    S.dma('pool', V[d][:, 0:gw, :], PTMv[:, c0:c0 + gw, GLV:GLV + 64], writes=[dld[d]])
                    for j in range(gw):
                        S.op('pe', lambda e, d=d, j=j: e.matmul(psA[:, j, :], AT[d][:, j * 128:(j + 1) * 128], W2[:, d, :], start=True, stop=True),
                             reads=[dld[d], dc], writes=[dpA])
                    S.op('dve', lambda e, d=d, gw=gw: e.tensor_tensor(out=A[d][:, 0:gw, :], in0=psA[:, 0:gw, :],
                                                                       in1=B2[:, d, :].unsqueeze(1).to_broadcast([128, gw, 64]), op=ALU.add),
                         reads=[dpA, dc], writes=[dA[d]])
                    S.op('act', lambda e, d=d, gw=gw: e.activation(out=A[d][:, 0:gw, :], in_=A[d][:, 0:gw, :], func=AF.Exp, scale=-1.0), reads=[dA[d]], writes=[dA[d]])
                    S.op('act', lambda e, d=d, gw=gw: e.activation(out=A[d][:, 0:gw, :], in_=A[d][:, 0:gw, :], func=AF.Ln, bias=1.0), reads=[dA[d]], writes=[dA[d]])
                    S.op('pe', lambda e, d=d, gw=gw: e.matmul(psG[:, 0:gw * 64], TR[:, d, :], A[d][:, 0:gw, :].rearrange("p g n -> p (g n)"), start=True, stop=True),
                         reads=[dA[d], dc], writes=[dpG])
                    S.op('act', lambda e, d=d, gw=gw: e.activation(out=EK[d][:, 0:gw, :].rearrange("p g n -> p (g n)"), in_=psG[:, 0:gw * 64], func=AF.Exp, scale=1.0 / 16),
                         reads=[dpG], writes=[dEK[d]])
                    S.op('dve', lambda e, d=d, gw=gw: e.tensor_tensor(out=Kt2[d][:, 0:gw, :], in0=Ktm[d][:, 0:gw, :], in1=EK[d][:, 0:gw, :], op=ALU.mult),
                         reads=[dld[d], dEK[d]], writes=[dKt2[d]])
                    for j in range(gw):
                        S.op('pe', lambda e, d=d, j=j: e.matmul(psGT[j // 4][:, j % 4, :], A[d][:, j, :], TR[:, d, :], start=True, stop=True),
                             reads=[dA[d], dc], writes=[dpGT[j // 4]])
                    for hf in range((gw + 3) // 4):
                        n = min(4, gw - 4 * hf)
                        sl = slice(hf * 512, hf * 512 + n * 128)
                        S.op('act', lambda e, d=d, hf=hf, n=n, sl=sl: e.activation(out=EQ[d][:, sl], in_=psGT[hf][:, 0:n, :].rearrange("p g n -> p (g n)"), func=AF.Exp, scale=-1.0 / 16),
                             reads=[dpGT[hf]], writes=[dEQ[d]])
                        S.op('act', lambda e, d=d, hf=hf, n=n, sl=sl: e.activation(out=EKT[d][:, sl], in_=psGT[hf][:, 0:n, :].rearrange("p g n -> p (g n)"), func=AF.Exp, scale=1.0 / 16),
                             reads=[dpGT[hf]], writes=[dEKT[d]])
                    S.op('dve', lambda e, d=d, W=W: e.scalar_tensor_tensor(out=QT2[d][:, 0:W], in0=QTf[d][:, 0:W], scalar=0.125, in1=EQ[d][:, 0:W], op0=ALU.mult, op1=ALU.mult),
                         reads=[dld[d], dEQ[d]], writes=[dQT2[d]])
                    S.op('pool', lambda e, d=d, W=W: e.tensor_tensor(out=KT2[d][:, 0:W], in0=KTf[d][:, 0:W], in1=EKT[d][:, 0:W], op=ALU.mult),
                         reads=[dld[d], dEKT[d]], writes=[dKT2[d]])
                    corder = list(range(gw)) if d == 0 else list(range(gw - 1, -1, -1))
                    for j in corder:
                        c = c0 + j
                        sl = slice(j * 128, (j + 1) * 128)
                        dcol = j * 128 + (127 if d == 0 else 0)
                        S.op('pe', lambda e, d=d, sl=sl: e.matmul(psS[:], KT2[d][:, sl], QT2[d][:, sl], start=True, stop=True), reads=[dKT2[d], dQT2[d]], writes=[dpS])
                        S.op('dve', lambda e, d=d: e.tensor_tensor(out=Sm[:], in0=psS[:], in1=TR[:, d, :], op=ALU.mult), reads=[dpS, dc], writes=[dSm])
                        S.op('pe', lambda e, d=d, j=j: e.matmul(psH[:], Sm[:], V[d][:, j, :], start=True, stop=False), reads=[dSm, dld[d]], writes=[dpH])
                        S.op('pe', lambda e, d=d, sl=sl: e.matmul(psH[:], QT2[d][:, sl], Sb[d][:], start=False, stop=True), reads=[dQT2[d], dSb[d]], writes=[dpH])
                        S.op('pe', lambda e, d=d, j=j: e.matmul(psU[:], Kt2[d][:, j, :], V[d][:, j, :], start=True, stop=True), reads=[dKt2[d], dld[d]], writes=[dpU])
                        S.op('act', lambda e, d=d, c=c: e.activation(out=Hd[d][:, c, :], in_=psH[:], func=AF.Copy), reads=[dpH], writes=[dH[d]])
                        S.op('dve', lambda e, d=d: e.tensor_tensor(out=tmpS[:], in0=psU[:], in1=St[d][:], op=ALU.add), reads=[dpU, dSt[d]], writes=[dtS])
                        S.op('dve', lambda e, d=d, dcol=dcol: e.tensor_scalar(out=St[d][:], in0=tmpS[:], scalar1=EQ[d][:, dcol:dcol + 1], scalar2=None, op0=ALU.mult),
                             reads=[dtS, dEQ[d]], writes=[dSt[d]])
                        S.op('act', lambda e, d=d, dcol=dcol: e.activation(out=Sb[d][:], in_=tmpS[:], func=AF.Copy, scale=EQ[d][:, dcol:dcol + 1]),
                             reads=[dtS, dEQ[d]], writes=[dSb[d]])
        with Scope(S):
            Rg = S.sb([128, NCH, 64], F32)
            NG = S.sb([128, 64], F32)
            sm = S.sb([128, NCH], F32)
            vr = S.sb([128, NCH], F32)
            dR, dn, dsm, dvr = Dep(), Dep(), Dep(), Dep()
            for j in range(5):
                S.dma('sp', Rg[:, j * 26:(j + 1) * 26, :], PTMv[:, j * 26:(j + 1) * 26, GLR:GLR + 64], writes=[dR])
            S.dma('sp', NG[:], norm_g.partition_broadcast(128), writes=[dn])
            H, H2 = Hd[0], Hd[1]
            S.op('dve', lambda e: e.tensor_tensor(out=H[:], in0=H[:], in1=H2[:], op=ALU.add), reads=[dH[0], dH[1]], writes=[dH[0]])
            _finish_norm(S, H, H2, dH[0], dH[1], sm, vr, dsm, dvr, False)
            S.op('pool', lambda e: e.tensor_tensor(out=H[:], in0=H[:], in1=NG[:].unsqueeze(1).to_broadcast([128, NCH, 64]), op=ALU.mult), reads=[dH[0], dn], writes=[dH[0]])
            S.op('act', lambda e: e.activation(out=Rg[:], in_=Rg[:], func=AF.Silu), reads=[dR], writes=[dR])
            Hout = H if BR.dtype == F32 else S.sb([128, NCH, 64], BR.dtype)
            S.op('dve', lambda e: e.tensor_tensor(out=Hout[:], in0=H[:], in1=Rg[:], op=ALU.mult), reads=[dH[0], dR], writes=[dH[0]])
            for j in range(5):
                S.dma('sp', BR.rearrange("(c p) n -> p c n", p=128)[:, j * 26:(j + 1) * 26, 256:320], Hout[:, j * 26:(j + 1) * 26, :], reads=[dH[0]])


def emit_A5(S, PFM, BR, VD, f64, c256, f1, tw, c2, with_ctx):
    with Scope(S):
        F64 = S.sb([64, 128], F32)
        dc = Dep()
        S.dma('sp', F64[:], f64, writes=[dc])
        Vc = S.sb([128, 2, 128], F32)
        dVc = Dep()
        with Scope(S):
            UT = [S.sb([64, 2048], F32) for _ in range(2)]
            dU = [Dep(), Dep()]
            ps0 = [S.ps([128, 4, 128]) for _ in range(2)]
            dp0 = [Dep(), Dep()]
            Vt = [S.sb([128, 4, 128], F32) for _ in range(2)]
            dVt = [Dep(), Dep()]
            S.dma('sp', UT[0][:, 0:256], PFM[256:320, 0:256], writes=[dU[0]])
            for j in range(2):
                S.op('pe', lambda e, j=j: e.matmul(ps0[0][:, j, :], UT[0][:, j * 128:(j + 1) * 128], F64[:], start=True, stop=True), reads=[dU[0], dc], writes=[dp0[0]])
            S.op('dve', lambda e: e.tensor_copy(out=Vc[:], in_=ps0[0][:, 0:2, :]), reads=[dp0[0]], writes=[dVc])
            n4 = 0
            for g in range(8):
                b = (g + 1) % 2
                t0 = TC + g * 2048
                S.dma('sp', UT[b][:], PFM[256:320, t0:t0 + 2048], writes=[dU[b]])
                for q in range(4):
                    pb = n4 % 2
                    n4 += 1
                    for j in range(4):
                        col = (q * 4 + j) * 128
                        S.op('pe', lambda e, b=b, pb=pb, j=j, col=col: e.matmul(ps0[pb][:, j, :], UT[b][:, col:col + 128], F64[:], start=True, stop=True),
                             reads=[dU[b], dc], writes=[dp0[pb]])
                    if pb == 0:
                        S.op('dve', lambda e, pb=pb: e.tensor_copy(out=Vt[pb][:], in_=ps0[pb][:]), reads=[dp0[pb]], writes=[dVt[pb]])
                    else:
                        S.op('act', lambda e, pb=pb: e.activation(out=Vt[pb][:], in_=ps0[pb][:], func=AF.Copy), reads=[dp0[pb]], writes=[dVt[pb]])
                    r0 = g * 2048 + q * 512
                    S.dma('pool', VD[r0:r0 + 512, :].rearrange("(s p) n -> p s n", p=128), Vt[pb][:], reads=[dVt[pb]])
        if with_ctx:
            with Scope(S):
                CS = S.sb([128, 2, 2, 256], F32)
                dcs = Dep()
                S.dma('sp', CS[:], c256.rearrange("a (bt p) n -> p a bt n", p=128), writes=[dcs])
                psc = S.ps([128, 64])
                dpc = Dep()
                yc = S.sb([128, 64], BR.dtype)
                dyc = Dep()
                for a in range(2):
                    k = 0
                    for bt in range(2):
                        for cs_ in range(2):
                            S.op('pe', lambda e, a=a, bt=bt, cs_=cs_, k=k: e.matmul(psc[:], CS[:, cs_, bt, a * 128:(a + 1) * 128], Vc[:, bt, cs_ * 64:(cs_ + 1) * 64],
                                                                                  start=(k == 0), stop=(k == 3)), reads=[dcs, dVc], writes=[dpc])
                            k += 1
                    S.op('dve', lambda e: e.tensor_copy(out=yc[:], in_=psc[:]), reads=[dpc], writes=[dyc])
                    S.dma('sp', BR[a * 128:(a + 1) * 128, 64:128], yc[:], reads=[dyc])
        with Scope(S):
            X = S.sb([128, 128, 128], F32)
            dX = Dep()
            VDv = VD.rearrange("(t1 t2) n -> t1 t2 n", t2=128)
            for j in range(4):
                S.dma('sp' if j % 2 == 0 else 'pool', X[:, j * 32:(j + 1) * 32, :], VDv[:, j * 32:(j + 1) * 32, :], writes=[dX])
            F1 = S.sb([128, 2, 256], F32)
            TW = S.sb([128, 2, 128], F32)
            C2 = S.sb([128, 2, 128], F32)
            dt = Dep()
            S.dma('sp', F1[:], f1.rearrange("a p n -> p a n"), writes=[dt])
            S.dma('sp', TW[:], tw.rearrange("a p n -> p a n"), writes=[dt])
            S.dma('sp', C2[:], c2.rearrange("a p n -> p a n"), writes=[dt])
            ZP = [S.sb([128, 128, 64], F32) for _ in range(2)]
            dZP = [Dep(), Dep()]
            psZ = [S.ps([128, 256]) for _ in range(2)]
            dpZ = [Dep(), Dep()]
            tmp = [[S.sb([128, 128], F32) for _ in range(4)] for _ in range(2)]
            dtm = [[Dep() for _ in range(4)] for _ in range(2)]
            for c in range(64):
                b = c % 2
                S.op('pe', lambda e, b=b, c=c: e.matmul(psZ[b][:], X[:, :, c], F1[:, 0, :], start=True, stop=False), reads=[dX, dt], writes=[dpZ[b]])
                S.op('pe', lambda e, b=b, c=c: e.matmul(psZ[b][:], X[:, :, 64 + c], F1[:, 1, :], start=False, stop=True), reads=[dX, dt], writes=[dpZ[b]])
                combos = [(0, 0), (1, 1), (0, 1), (1, 0)]
                for k, (zp, tp) in enumerate(combos):
                    S.op('dve', lambda e, b=b, k=k, zp=zp, tp=tp: e.tensor_tensor(out=tmp[b][k][:], in0=psZ[b][:, zp * 128:(zp + 1) * 128], in1=TW[:, tp, :], op=ALU.mult),
                         reads=[dpZ[b], dt], writes=[dtm[b][k]])
                S.op('pool', lambda e, b=b, c=c: e.tensor_tensor(out=ZP[0][:, :, c], in0=tmp[b][0][:], in1=tmp[b][1][:], op=ALU.subtract),
                     reads=[dtm[b][0], dtm[b][1]], writes=[dZP[0]])
                S.op('pool', lambda e, b=b, c=c: e.tensor_tensor(out=ZP[1][:, :, c], in0=tmp[b][2][:], in1=tmp[b][3][:], op=ALU.add),
                     reads=[dtm[b][2], dtm[b][3]], writes=[dZP[1]])
            psY = [S.ps([128, 512]) for _ in range(2)]
            dpY = [Dep(), Dep()]
            Yst = [S.sb([128, 512], BR.dtype) for _ in range(2)]
            dY = [Dep(), Dep()]
            BRv = BR[TC:TA, 64:128].rearrange("(p t1) n -> p t1 n", t1=128)
            for k in range(16):
                b = k % 2
                for part in range(2):
                    S.op('pe', lambda e, b=b, k=k, part=part: e.matmul(psY[b][:], C2[:, part, :], ZP[part][:, 8 * k:8 * k + 8, :].rearrange("p a n -> p (a n)"),
                                                                       start=(part == 0), stop=(part == 1)), reads=[dZP[part], dt], writes=[dpY[b]])
                S.op('act', lambda e, b=b: e.activation(out=Yst[b][:], in_=psY[b][:], func=AF.Copy), reads=[dpY[b]], writes=[dY[b]])
                S.dma('sp', BRv[:, 8 * k:8 * k + 8, :], Yst[b][:].rearrange("p (a n) -> p a n", n=64), reads=[dY[b]])


def fourier_tables():
    f = np.float64
    c = np.arange(64, dtype=f)
    a64 = 2 * np.pi * np.outer(c, c) / 64
    f64 = np.concatenate([np.cos(a64), -np.sin(a64)], axis=1)
    t = np.arange(256, dtype=f)
    a256 = 2 * np.pi * np.outer(t, t) / 256
    c256 = np.stack([np.cos(a256), np.sin(a256)]) / 128.0
    n = np.arange(128, dtype=f)
    a128 = 2 * np.pi * np.outer(n, n) / 128
    C, Sn = np.cos(a128), np.sin(a128)
    f1 = np.stack([np.concatenate([C, -Sn], axis=1), np.concatenate([Sn, C], axis=1)])
    atw = 2 * np.pi * np.outer(n, n) / 16384
    tw = np.stack([np.cos(atw), -np.sin(atw)])
    c2 = np.stack([C, Sn]) / 1024.0
    g = lambda a: np.ascontiguousarray(a.astype(np.float32))
    return dict(f64=g(f64), c256=g(c256), f1=g(f1), tw=g(tw), c2=g(c2))


ALPHA = (2.0 * 2) ** 0.25
DBG = {}


def emit_ada(S, cvec, w, bvec, col0, ncols, out_tile, dout):
    with Scope(S):
        c8 = S.sb([128, 8], F32)
        sc = S.sb([128, 8, 128], F32)
        dc = Dep()
        S.dma('sp', c8[:], cvec.rearrange("(kc p) -> p kc", p=128), writes=[dc], allow_slow_non_contiguous=True)
        S.op('act', lambda e: e.activation(out=c8[:], in_=c8[:], func=AF.Silu), reads=[dc], writes=[dc])
        S.op('dve', lambda e: e.tensor_copy(out=sc[:], in_=c8[:].unsqueeze(2).to_broadcast([128, 8, 128])), reads=[dc], writes=[dc])
        S.dma('sp', out_tile[:, 0:ncols], bvec[col0:col0 + ncols].partition_broadcast(128), writes=[dout])
        wb = [S.sb([128, 8, 512], F32) for _ in range(2)]
        dw = [Dep(), Dep()]
        ps = [S.ps([128, 512]) for _ in range(2)]
        dp = [Dep(), Dep()]
        wv = w.rearrange("(kc p) n -> p kc n", p=128)
        for j in range(ncols // 512):
            b = j % 2
            S.dma('sp', wb[b][:], wv[:, :, col0 + j * 512:col0 + (j + 1) * 512], writes=[dw[b]])
            for kc in range(8):
                S.op('pe', lambda e, b=b, kc=kc: e.matmul(ps[b][:], sc[:, kc, :], wb[b][:, kc, :], start=(kc == 0), stop=(kc == 7)), reads=[dc, dw[b]], writes=[dp[b]])
            S.op('dve', lambda e, b=b, j=j: e.tensor_tensor(out=out_tile[:, j * 512:(j + 1) * 512], in0=ps[b][:], in1=out_tile[:, j * 512:(j + 1) * 512], op=ALU.add),
                 reads=[dp[b], dout], writes=[dout])


class LNK:
    def __init__(self, S):
        self.S = S
        self.st = S.sb([128, 2, 6], F32)
        self.mv = S.sb([128, 2], F32)
        self.rs = S.sb([128, 1], F32)
        self.d = Dep()

    def norm(self, out, dout, x, dx, A, B, dAB, tmp, dtmp):
        S = self.S
        st, mv, rs, d = self.st, self.mv, self.rs, self.d
        for j in range(2):
            S.op('dve', lambda e, j=j: e.bn_stats(out=st[:, j, :], in_=x[:, j * 512:(j + 1) * 512]), reads=[dx], writes=[d])
        S.op('dve', lambda e: e.bn_aggr(out=mv[:], in_=st[:]), reads=[d], writes=[d])
        S.op('act', lambda e: e.activation(out=rs[:], in_=mv[:, 1:2], func=AF.Sqrt, bias=LN_EPS), reads=[d], writes=[d])
        S.op('dve', lambda e: e.reciprocal(out=rs[:], in_=rs[:]), reads=[d], writes=[d])
        S.op('dve', lambda e: e.tensor_scalar(out=tmp[:], in0=x[:], scalar1=mv[:, 0:1], scalar2=rs[:, 0:1], op0=ALU.subtract, op1=ALU.mult),
             reads=[dx, d], writes=[dtmp])
        S.op('pool', lambda e: e.tensor_tensor(out=tmp[:], in0=tmp[:], in1=A, op=ALU.mult), reads=[dtmp, dAB], writes=[dtmp])
        S.op('dve', lambda e: e.tensor_tensor(out=out, in0=tmp[:], in1=B, op=ALU.add), reads=[dtmp, dAB], writes=[dout])


def emit_T(S, src, dsrc, dstT, ddst, ident, dident, ps, dps, dt_copy_eng='act'):
    for kc in range(8):
        S.op('pe', lambda e, kc=kc: e.transpose(ps[:, kc, :], src[:, kc * 128:(kc + 1) * 128], ident), reads=[dsrc, dident], writes=[dps])
    S.op(dt_copy_eng, (lambda e: e.activation(out=dstT, in_=ps[:], func=AF.Copy)) if dt_copy_eng == 'act' else (lambda e: e.tensor_copy(out=dstT, in_=ps[:])),
         reads=[dps], writes=[ddst])


def emit_PRE(S, xin, ntok, nctx, c_b, c_ctx, w_ada, b_ada, ident, HTN):
    with Scope(S):
        AD = [S.sb([128, 2048], F32) for _ in range(2)]
        dAD = [Dep(), Dep()]
        emit_ada(S, c_b, w_ada, b_ada, 0, 2048, AD[0], dAD[0])
        if nctx:
            emit_ada(S, c_ctx, w_ada, b_ada, 0, 2048, AD[1], dAD[1])
        for v in range(2 if nctx else 1):
            S.op('dve', lambda e, v=v: e.tensor_scalar(out=AD[v][:, 1024:2048], in0=AD[v][:, 1024:2048], scalar1=1.0, scalar2=None, op0=ALU.add), reads=[dAD[v]], writes=[dAD[v]])
        _lnmodT_loop(S, xin, ntok, nctx, AD, dAD, ident, HTN)


def _lnmodT_loop(S, xin, ntok, nctx, AD, dAD, ident, HTN):
    identb = S.sb([128, 128], BF16)
    did = Dep()
    S.dma('pool', identb[:], ident, writes=[did])
    ln = LNK(S)
    xt = [S.sb([128, 1024], F32) for _ in range(2)]
    dxt = [Dep(), Dep()]
    tmp = S.sb([128, 1024], F32)
    dtmp = Dep()
    hb = [S.sb([128, 1024], BF16) for _ in range(2)]
    dhb = [Dep(), Dep()]
    psT = [S.ps([128, 8, 128], BF16) for _ in range(2)]
    dpsT = [Dep(), Dep()]
    hT = [S.sb([128, 8, 128], BF16) for _ in range(2)]
    dhT = [Dep(), Dep()]
    HTv = HTN.rearrange("(kc p) t -> p kc t", p=128)
    for t in range(ntok // 128):
        b = t % 2
        v = 1 if t * 128 < nctx else 0
        S.dma('sp', xt[b][:], xin[t * 128:(t + 1) * 128, :], writes=[dxt[b]])
        ln.norm(hb[b][:], dhb[b], xt[b], dxt[b], AD[v][:, 1024:2048], AD[v][:, 0:1024], dAD[v], tmp, dtmp)
        emit_T(S, hb[b], dhb[b], hT[b][:], dhT[b], identb[:], did, psT[b], dpsT[b])
        S.dma('pool', HTv[:, :, t * 128:(t + 1) * 128], hT[b][:], reads=[dhT[b]])


def emit_B(S, last, moe, ntok, nctx, xin, hT, brT, c_b, c_ctx, w_ada, b_ada, w_ada_n, b_ada_n,
           w_gate, b_gate, w_branch, w_out, ln1g, ln1b, ln2g, ln2b, wg, wu, wd, wr, brr, ident,
           XMID, H2T, DENSE, XOUT, HTN):
    NE = 8 if moe else 2
    nv = 2 if nctx else 1
    cv = [c_b, c_ctx]
    with Scope(S):
        AD = [S.sb([128, 4096], F32) for _ in range(nv)]
        dAD = [Dep() for _ in range(nv)]
        for v in range(nv):
            emit_ada(S, cv[v], w_ada, b_ada, 2048, 4096, AD[v], dAD[v])
            S.op('dve', lambda e, v=v: e.tensor_scalar(out=AD[v][:, 2048:3072], in0=AD[v][:, 2048:3072], scalar1=1.0, scalar2=None, op0=ALU.add), reads=[dAD[v]], writes=[dAD[v]])
        identf = S.sb([128, 128], F32)
        did = Dep()
        S.dma('sp', identf[:], ident, writes=[did])
        with Scope(S):
            Wg = S.sb([128, 8, 4096], BF16)
            Wb = S.sb([128, 10, 1024], BF16)
            Wo = S.sb([128, 8, 1024], BF16)
            bg = S.sb([128, 32], F32)
            L1 = S.sb([128, 2, 1024], F32)
            dW = Dep()
            wgv = w_gate.rearrange("(kc p) n -> p kc n", p=128)
            for j in range(4):
                S.dma('pool', Wg[:, :, j * 1024:(j + 1) * 1024], wgv[:, :, j * 1024:(j + 1) * 1024], writes=[dW])
            S.dma('pool', Wb[:], w_branch.rearrange("(kc p) n -> p kc n", p=128), writes=[dW])
            S.dma('pool', Wo[:], w_out.rearrange("(kc p) n -> p kc n", p=128), writes=[dW])
            S.dma('sp', bg[:], b_gate, writes=[dW])
            S.dma('sp', L1[:, 0, :], ln1g.partition_broadcast(128), writes=[dW])
            S.dma('sp', L1[:, 1, :], ln1b.partition_broadcast(128), writes=[dW])
            if moe:
                WR = S.sb([128, 8, 8], F32)
                BRR = S.sb([128, 8], F32)
                S.dma('sp', WR[:], wr.rearrange("(kc p) n -> p kc n", p=128), writes=[dW])
                S.dma('sp', BRR[:], brr.partition_broadcast(128), writes=[dW])
            if isinstance(brT, tuple) and len(brT) > 3:
                BRG_fn0, selI0, cdt0, BRT0 = brT
                with Scope(S):
                    SelI0 = S.sb([128, 4, 128], BF16)
                    idb0 = S.sb([128, 128], BF16)
                    dsel0 = Dep()
                    S.dma('sp', SelI0[:], selI0.rearrange("r p n -> p r n"), writes=[dsel0]) if False else S.dma('pool', SelI0[:], selI0.rearrange("r p n -> p r n"), writes=[dsel0])
                    S.dma('pool', idb0[:], ident, writes=[dsel0])
                    cand0 = [[S.sb([128, 4, 320], cdt0) for _ in range(4)] for _ in range(2)]
                    dcand0 = [[Dep() for _ in range(4)] for _ in range(2)]
                    brtok0 = [[S.sb([128, 1280], BF16) for _ in range(4)] for _ in range(2)]
                    dbrtok0 = [[Dep() for _ in range(4)] for _ in range(2)]
                    psB0 = [[S.ps([128, 4, 128]) for _ in range(3)] for _ in range(2)]
                    dpsB0 = [[Dep() for _ in range(3)] for _ in range(2)]
                    bst0 = [S.sb([128, 10, 128], BF16) for _ in range(2)]
                    dbst0 = [Dep(), Dep()]
                    BRTv0 = BRT0.rearrange("(kc p) t -> p kc t", p=128)
                    segs0 = [(0, 0, 64), (256, 64, 64), (512, 128, 128), (1024, 256, 64)]
                    nops = 0
                    for ti0 in range(ntok // 128):
                        tt = ti0 * 128
                        tb = ti0 % 2
                        isctx = tt < nctx
                        ncand = 1 if isctx else 4
                        for r in range(ncand):
                            row = tt if isctx else TC + r * 4096 + (tt - nctx)
                            S.dma('sp', cand0[tb][r][:], BRG_fn0(row), writes=[dcand0[tb][r]])
                            for si, (fo, co, w) in enumerate(segs0):
                                eng = ('act', 'pool', 'dve')[nops % 3]
                                nops += 1
                                dst = brtok0[tb][r][:, fo:fo + 4 * w].rearrange("p (i w) -> p i w", i=4)
                                src = cand0[tb][r][:, :, co:co + w]
                                if eng == 'act':
                                    S.op('act', lambda e, dst=dst, src=src: e.activation(out=dst, in_=src, func=AF.Copy), reads=[dcand0[tb][r]], writes=[dbrtok0[tb][r]])
                                else:
                                    S.op(eng, lambda e, dst=dst, src=src: e.tensor_copy(out=dst, in_=src), reads=[dcand0[tb][r]], writes=[dbrtok0[tb][r]])
                        for kg in range(3):
                            kbs = list(range(kg * 4, min(10, kg * 4 + 4)))
                            for kb in kbs:
                                for r in range(ncand):
                                    rhs = idb0[:] if isctx else SelI0[:, r, :]
                                    S.op('pe', lambda e, kb=kb, r=r, kg=kg, ncand=ncand, rhs=rhs, tb=tb: e.matmul(
                                        psB0[tb][kg][:, kb - kg * 4, :], brtok0[tb][r][:, kb * 128:(kb + 1) * 128], rhs,
                                        start=(r == 0), stop=(r == ncand - 1)), reads=[dbrtok0[tb][r], dsel0], writes=[dpsB0[tb][kg]])
                            if kg == 1:
                                S.op('act', lambda e, kg=kg, kbs=kbs, tb=tb: e.activation(out=bst0[tb][:, kbs[0]:kbs[-1] + 1, :], in_=psB0[tb][kg][:, 0:len(kbs), :], func=AF.Copy),
                                     reads=[dpsB0[tb][kg]], writes=[dbst0[tb]])
                            else:
                                S.op('dve', lambda e, kg=kg, kbs=kbs, tb=tb: e.tensor_copy(out=bst0[tb][:, kbs[0]:kbs[-1] + 1, :], in_=psB0[tb][kg][:, 0:len(kbs), :]),
                                     reads=[dpsB0[tb][kg]], writes=[dbst0[tb]])
                        S.dma('sp', BRTv0[:, :, tt:tt + 128], bst0[tb][:], reads=[dbst0[tb]])
                brT = BRT0
            hs = S.sb([128, 8, 512], BF16)
            bs = S.sb([128, 10, 512], BF16)
            dhs, dbs = Dep(), Dep()
            dhs2, dbs2 = Dep(), Dep()
            fused_br = isinstance(brT, tuple)
            if fused_br:
                BRG, selI = brT[0], brT[1]
                SelI = S.sb([128, 4, 128], BF16)
                dsel = Dep()
                S.dma('pool', SelI[:], selI.rearrange("r p n -> p r n"), writes=[dsel])
                cand = S.sb([128, 4, 320], brT[2] if len(brT) > 2 else F32)
                dcand = Dep()
                brtok = [S.sb([128, 1280], BF16) for _ in range(4)]
                dbrtok = [Dep() for _ in range(4)]
                psB = S.ps([128, 4, 128])
                dpsB = Dep()
                BRG_fn = BRG if callable(BRG) else (lambda row, v=BRG.rearrange("i t n -> t i n"): v[row:row + 128, :, :])
            assert not fused_br
            mixT = [S.sb([128, 8, 512], BF16) for _ in range(2)]
            dmixT = [Dep(), Dep()]
            gate = [S.sb([128, 512], F32) for _ in range(2)]
            dgate = [Dep(), Dep()]
            mix = S.sb([128, 512], F32)
            dmix = Dep()
            tm = [S.sb([128, 512], F32) for _ in range(2)]
            dtm = [Dep(), Dep()]
            psg = [S.ps([128, 512]) for _ in range(2)]
            psp = [S.ps([128, 512]) for _ in range(2)]
            dpsg, dpsp = [Dep(), Dep()], [Dep(), Dep()]
            psy = [S.ps([128, 512]) for _ in range(2)]
            dpsy = [Dep(), Dep()]
            psT1 = S.ps([128, 4, 128])
            dps1 = Dep()
            psl = S.ps([128, 8])
            dpsl = Dep()
            xt = [S.sb([128, 1024], F32) for _ in range(2)]
            tmp = [S.sb([128, 1024], F32) for _ in range(2)]
            dxt, dtmp = [Dep(), Dep()], [Dep(), Dep()]
            xm = S.sb([128, 1024], F32)
            dxm = Dep()
            h2T = S.sb([128, 8, 128], BF16)
            h2Tf = S.sb([128, 8, 128], F32) if moe else None
            dh2T, dh2Tf = Dep(), Dep()
            lnk = [LNK(S), LNK(S)]
            rt = [S.sb([128, 8], F32) for _ in range(4)]
            r1 = [S.sb([128, 1], F32) for _ in range(4)]
            drt = Dep()
            hTv = hT.rearrange("(kc p) t -> p kc t", p=128)
            bTv = brT.rearrange("(kc p) t -> p kc t", p=128)
            H2Tv = H2T.rearrange("(kc p) t -> p kc t", p=128)
            kbr = [(0, 2), (2, 4), (4, 8), (8, 10)]
            ng = 0
            ecnt = [0]

            def ln_a(L, x, dx):
                for j in range(2):
                    S.op('dve', lambda e, j=j: e.bn_stats(out=L.st[:, j, :], in_=x[:, j * 512:(j + 1) * 512]), reads=[dx], writes=[L.d])
                S.op('dve', lambda e: e.bn_aggr(out=L.mv[:], in_=L.st[:]), reads=[L.d], writes=[L.d])
                S.op('act', lambda e: e.activation(out=L.rs[:], in_=L.mv[:, 1:2], func=AF.Sqrt, bias=LN_EPS), reads=[L.d], writes=[L.d])

            def ln_b(L, x, dx, A, dAB, T, dT):
                S.op('dve', lambda e: e.reciprocal(out=L.rs[:], in_=L.rs[:]), reads=[L.d], writes=[L.d])
                S.op('dve', lambda e: e.tensor_scalar(out=T[:], in0=x[:], scalar1=L.mv[:, 0:1], scalar2=L.rs[:, 0:1], op0=ALU.subtract, op1=ALU.mult),
                     reads=[dx, L.d], writes=[dT])
                S.op('pool', lambda e: e.tensor_tensor(out=T[:], in0=T[:], in1=A, op=ALU.mult), reads=[dT, dAB], writes=[dT])

            def ln_c(out, dout, T, dT, B, dAB):
                S.op('dve', lambda e: e.tensor_tensor(out=out, in0=T[:], in1=B, op=ALU.add), reads=[dT, dAB], writes=[dout])

            def subtile_steps(c, t0, sub, v, mb):
                X, dX, T, dT, L = xt[c], dxt[c], tmp[c], dtmp[c], lnk[c]
                r0 = t0 + sub * 128
                S.dma('sp', X[:], xin[r0:r0 + 128, :], writes=[dX])
                for half in range(2):
                    hsl = slice(half * 512, (half + 1) * 512)
                    for fc in range(8):
                        S.op('pe', lambda e, fc=fc, hsl=hsl, half=half: e.matmul(psy[half][:], mixT[mb][:, fc, sub * 128:(sub + 1) * 128], Wo[:, fc, hsl],
                                                                              start=(fc == 0), stop=(fc == 7)),
                             reads=[dmixT[mb], dW], writes=[dpsy[half]])
                yield
                for half in range(2):
                    hsl = slice(half * 512, (half + 1) * 512)
                    S.op('dve', lambda e, hsl=hsl, half=half: e.tensor_tensor(out=T[:, hsl], in0=psy[half][:], in1=AD[v][:, hsl], op=ALU.mult),
                         reads=[dpsy[half], dAD[v]], writes=[dT])
                S.op('dve', lambda e: e.scalar_tensor_tensor(out=X[:], in0=X[:], scalar=ALPHA, in1=T[:], op0=ALU.mult, op1=ALU.add), reads=[dX, dT], writes=[dX])
                ln_a1(L, X, dX)
                yield
                yield
                ln_a2(L)
                yield
                ln_b(L, X, dX, L1[:, 0, :], dW, T, dT)
                yield
                ln_c(xm[:], dxm, T, dT, L1[:, 1, :], dW)
                S.dma('sp', XMID[r0:r0 + 128, :], xm[:], reads=[dxm])
                ln_a1(L, xm, dxm)
                yield
                yield
                ln_a2(L)
                yield
                ln_b(L, xm, dxm, AD[v][:, 2048:3072], dAD[v], T, dT)
                yield
                ln_c(X[:], dX, T, dT, AD[v][:, 1024:2048], dAD[v])
                yield
                for q in range(2):
                    for kc in range(q * 4, q * 4 + 4):
                        S.op('pe', lambda e, kc=kc: e.transpose(psT1[:, kc % 4, :], X[:, kc * 128:(kc + 1) * 128], identf[:]), reads=[dX, did], writes=[dps1])
                    if moe:
                        S.op('dve', lambda e, q=q: e.tensor_copy(out=h2Tf[:, q * 4:(q + 1) * 4, :], in_=psT1[:]), reads=[dps1], writes=[dh2Tf])
                    else:
                        S.op('dve', lambda e, q=q: e.tensor_copy(out=h2T[:, q * 4:(q + 1) * 4, :], in_=psT1[:]), reads=[dps1], writes=[dh2T])
                    yield
                if not moe:
                    S.dma('pool', H2Tv[:, :, r0:r0 + 128], h2T[:], reads=[dh2T])
                    for _ in range(4):
                        yield
                    return
                for kc in range(8):
                    S.op('pe', lambda e, kc=kc: e.matmul(psl[:], h2Tf[:, kc, :], WR[:, kc, :], start=(kc == 0), stop=(kc == 7)), reads=[dh2Tf, dW], writes=[dpsl])
                S.op('act', lambda e: e.activation(out=h2T[:], in_=h2Tf[:], func=AF.Copy), reads=[dh2Tf], writes=[dh2T])
                S.dma('pool', H2Tv[:, :, r0:r0 + 128], h2T[:], reads=[dh2T])
                yield
                lg, sel, ex, dn = rt
                m1, m2, lm, ssum = r1
                S.op('dve', lambda e: e.tensor_copy(out=lg[:], in_=psl[:]), reads=[dpsl], writes=[drt])
                S.op('dve', lambda e: e.tensor_tensor(out=sel[:], in0=lg[:], in1=BRR[:], op=ALU.add), reads=[drt, dW], writes=[drt])
                S.op('dve', lambda e: e.tensor_reduce(out=m1[:], in_=sel[:], axis=AX.X, op=ALU.max), reads=[drt], writes=[drt])
                S.op('dve', lambda e: e.tensor_scalar(out=ex[:], in0=sel[:], scalar1=m1[:, 0:1], scalar2=-1e30, op0=ALU.is_ge, op1=ALU.mult), reads=[drt], writes=[drt])
                S.op('dve', lambda e: e.tensor_tensor(out=ex[:], in0=ex[:], in1=sel[:], op=ALU.add), reads=[drt], writes=[drt])
                S.op('dve', lambda e: e.tensor_reduce(out=m2[:], in_=ex[:], axis=AX.X, op=ALU.max), reads=[drt], writes=[drt])
                S.op('dve', lambda e: e.tensor_scalar(out=sel[:], in0=sel[:], scalar1=m2[:, 0:1], scalar2=None, op0=ALU.is_ge), reads=[drt], writes=[drt])
                S.op('dve', lambda e: e.tensor_reduce(out=lm[:], in_=lg[:], axis=AX.X, op=ALU.max), reads=[drt], writes=[drt])
                S.op('dve', lambda e: e.tensor_scalar(out=lm[:], in0=lm[:], scalar1=-1.0, scalar2=None, op0=ALU.mult), reads=[drt], writes=[drt])
                yield
                S.op('act', lambda e: e.activation(out=ex[:], in_=lg[:], func=AF.Exp, bias=lm[:, 0:1]), reads=[drt], writes=[drt])
                yield
                S.op('dve', lambda e: e.tensor_tensor(out=ex[:], in0=ex[:], in1=sel[:], op=ALU.mult), reads=[drt], writes=[drt])
                S.op('dve', lambda e: e.tensor_reduce(out=ssum[:], in_=ex[:], axis=AX.X, op=ALU.add), reads=[drt], writes=[drt])
                S.op('dve', lambda e: e.reciprocal(out=ssum[:], in_=ssum[:]), reads=[drt], writes=[drt])
                S.op('dve', lambda e: e.tensor_scalar(out=dn[:], in0=ex[:], scalar1=ssum[:, 0:1], scalar2=None, op0=ALU.mult), reads=[drt], writes=[drt])
                S.dma('sp', DENSE[r0:r0 + 128, :], dn[:], reads=[drt])
                yield

            def ln_a1(L, x, dx):
                for j in range(2):
                    S.op('dve', lambda e, j=j: e.bn_stats(out=L.st[:, j, :], in_=x[:, j * 512:(j + 1) * 512]), reads=[dx], writes=[L.d])
                S.op('dve', lambda e: e.bn_aggr(out=L.mv[:], in_=L.st[:]), reads=[L.d], writes=[L.d])

            def ln_a2(L):
                S.op('act', lambda e: e.activation(out=L.rs[:], in_=L.mv[:, 1:2], func=AF.Sqrt, bias=LN_EPS), reads=[L.d], writes=[L.d])

            jobs = [[], []]
            npend = [0, 0]
            quota = [0, 0]

            pos = [None, None]

            def chain(c):
                o = 1 - c
                while True:
                    while not jobs[c] or not (pos[o] is None or pos[o] in (8, 9)):
                        yield
                    job = jobs[c].pop(0)
                    pos[c] = 0
                    for _ in subtile_steps(c, *job):
                        pos[c] += 1
                        yield
                    pos[c] = None
                    npend[c] -= 1

            chains = [chain(0), chain(1)]
            slot = [0]

            def step():
                next(chains[slot[0] % 2])
                slot[0] += 1

            stiles = ([(0, nctx)] if nctx else []) + [(nctx + 512 * j_, 512) for j_ in range((ntok - nctx) // 512)]
            for si, (t0, W) in enumerate(stiles):
                v = 1 if t0 < nctx else 0
                mb = si % 2
                S.dma('sp', hs[:, 0:4, 0:W], hTv[:, 0:4, t0:t0 + W], writes=[dhs])
                S.dma('act', hs[:, 4:8, 0:W], hTv[:, 4:8, t0:t0 + W], writes=[dhs2])
                S.dma('sp', bs[:, 0:5, 0:W], bTv[:, 0:5, t0:t0 + W], writes=[dbs])
                S.dma('act', bs[:, 5:10, 0:W], bTv[:, 5:10, t0:t0 + W], writes=[dbs2])
                for _ in range(4):
                    step()
                while npend[0] > quota[0] or npend[1] > quota[1]:
                    step()
                for fc in range(8):
                    for j in range(4):
                        pb = ng % 2
                        ng += 1
                        for kc in range(8):
                            S.op('pe', lambda e, pb=pb, kc=kc, j=j, fc=fc, W=W: e.matmul(psg[pb][:, 0:W], Wg[:, kc, j * 1024 + fc * 128:j * 1024 + fc * 128 + 128], hs[:, kc, 0:W],
                                                                                  start=(kc == 0), stop=(kc == 7)), reads=[dW, dhs if kc < 4 else dhs2], writes=[dpsg[pb]])
                        S.op('act', lambda e, pb=pb, j=j, fc=fc, W=W: e.activation(out=gate[pb][:, 0:W], in_=psg[pb][:, 0:W], func=AF.Sigmoid, bias=bg[:, j * 8 + fc:j * 8 + fc + 1]),
                             reads=[dpsg[pb], dW], writes=[dgate[pb]])
                        step()
                        k0, k1 = kbr[j]
                        for kb in range(k0, k1):
                            S.op('pe', lambda e, pb=pb, kb=kb, fc=fc, k0=k0, k1=k1, W=W: e.matmul(psp[pb][:, 0:W], Wb[:, kb, fc * 128:(fc + 1) * 128], bs[:, kb, 0:W],
                                                                                          start=(kb == k0), stop=(kb == k1 - 1)), reads=[dW, dbs if kb < 5 else dbs2], writes=[dpsp[pb]])
                        if j == 0:
                            S.op('dve', lambda e, pb=pb, W=W: e.tensor_tensor(out=mix[:, 0:W], in0=psp[pb][:, 0:W], in1=gate[pb][:, 0:W], op=ALU.mult), reads=[dpsp[pb], dgate[pb]], writes=[dmix])
                        else:
                            S.op('dve', lambda e, pb=pb, W=W: e.tensor_tensor(out=tm[pb][:, 0:W], in0=psp[pb][:, 0:W], in1=gate[pb][:, 0:W], op=ALU.mult), reads=[dpsp[pb], dgate[pb]], writes=[dtm[pb]])
                            if j < 3:
                                S.op('pool', lambda e, pb=pb, W=W: e.tensor_tensor(out=mix[:, 0:W], in0=mix[:, 0:W], in1=tm[pb][:, 0:W], op=ALU.add), reads=[dtm[pb], dmix], writes=[dmix])
                            else:
                                S.op('pool', lambda e, pb=pb, fc=fc, W=W, mb=mb: e.tensor_tensor(out=mixT[mb][:, fc, 0:W], in0=mix[:, 0:W], in1=tm[pb][:, 0:W], op=ALU.add),
                                     reads=[dtm[pb], dmix], writes=[dmixT[mb]])
                        step()
                quota[0] = quota[1] = 0
                for sub in range(W // 128):
                    jobs[sub % 2].append((t0, sub, v, mb))
                    npend[sub % 2] += 1
                    quota[sub % 2] += 1
            while npend[0] or npend[1]:
                step()
        if DBG.get('p1only'):
            return
        with Scope(S):
            L2 = S.sb([128, 2, 1024], F32)
            dL2 = Dep()
            S.dma('sp', L2[:, 0, :], ln2g.partition_broadcast(128), writes=[dL2])
            S.dma('sp', L2[:, 1, :], ln2b.partition_broadcast(128), writes=[dL2])
            if not last:
                ADN = [S.sb([128, 2048], F32) for _ in range(nv)]
                dADN = [Dep() for _ in range(nv)]
                for v in range(nv):
                    emit_ada(S, cv[v], w_ada_n, b_ada_n, 0, 2048, ADN[v], dADN[v])
                    S.op('dve', lambda e, v=v: e.tensor_scalar(out=ADN[v][:, 1024:2048], in0=ADN[v][:, 1024:2048], scalar1=1.0, scalar2=None, op0=ALU.add), reads=[dADN[v]], writes=[dADN[v]])
                identb = S.sb([128, 128], BF16)
                dib = Dep()
                S.dma('pool', identb[:], ident, writes=[dib])
                hb = S.sb([128, 1024], BF16)
                dhb = Dep()
                psTb = S.ps([128, 8, 128], BF16)
                dpsTb = Dep()
                hTn = S.sb([128, 8, 128], BF16)
                dhTn = Dep()
                HTNv = HTN.rearrange("(kc p) t -> p kc t", p=128)
            WG = [S.sb([128, 8, 768], BF16) for _ in range(2)]
            WU = [S.sb([128, 8, 768], BF16) for _ in range(2)]
            WD = [S.sb([128, 6, 1024], BF16) for _ in range(2)]
            dWf = [Dep(), Dep()]
            hg = S.sb([128, 8, 1024], BF16)
            dhg = Dep()
            facc = S.sb([128, 8, 1024], F32)
            dfacc = Dep()
            dns = S.sb([128, 8, 8], F32)
            ddns = Dep()
            actT = S.sb([128, 6, 512], BF16)
            dactT = Dep()
            sg = [S.sb([128, 512], F32) for _ in range(2)]
            dsg = [Dep(), Dep()]
            psG = [S.ps([128, 512]) for _ in range(2)]
            psU = [S.ps([128, 512]) for _ in range(2)]
            dpG, dpU = [Dep(), Dep()], [Dep(), Dep()]
            psF = [S.ps([128, 512]) for _ in range(2)]
            dpF = [Dep(), Dep()]
            xm_2 = S.sb([128, 1024], F32)
            z_2 = S.sb([128, 1024], F32)
            tmp_2 = S.sb([128, 1024], F32)
            xn_2 = S.sb([128, 1024], F32)
            dxm, dz, dtmp, dxn = Dep(), Dep(), Dep(), Dep()
            ln = LNK(S)
            H2Tv = H2T.rearrange("(kc p) t -> p kc t", p=128)
            ngrp = (ntok + 1023) // 1024
            nf = 0
            pieces = [(e_, fc0, nfc) for e_ in range(NE) for (fc0, nfc) in ((0, 6), (6, 5))]
            seq = [(g, pi) for g in range(ngrp) for pi in range(len(pieces))]

            def load_piece(k):
                e_, fc0, nfc = pieces[seq[k][1]]
                b = k % 2
                cs = slice(fc0 * 128, (fc0 + nfc) * 128)
                S.dma('pool', WG[b][:, :, 0:nfc * 128], wg[e_][:, cs].rearrange("(kc p) n -> p kc n", p=128), writes=[dWf[b]])
                S.dma('pool', WU[b][:, :, 0:nfc * 128], wu[e_][:, cs].rearrange("(kc p) n -> p kc n", p=128), writes=[dWf[b]])
                S.dma('pool', WD[b][:, 0:nfc, :], wd[e_][cs, :].rearrange("(kc p) n -> p kc n", p=128), writes=[dWf[b]])

            load_piece(0)
            for k, (g, pi) in enumerate(seq):
                g0 = g * 1024
                gwid = min(1024, ntok - g0)
                nt = gwid // 128
                ex_, fc0, nfc = pieces[pi]
                wb = k % 2
                if pi == 0 and g == 0:
                    S.dma('sp', hg[:, :, 0:gwid], H2Tv[:, :, g0:g0 + gwid], writes=[dhg])
                    if moe:
                        S.dma('sp', dns[:, 0:nt, :], DENSE[g0:g0 + gwid, :].rearrange("(t p) n -> p t n", p=128), writes=[ddns])
                if k + 1 < len(seq):
                    load_piece(k + 1)
                for c0 in range(0, gwid, 512):
                    cw = min(512, gwid - c0)
                    for fc in range(nfc):
                        pb = nf % 2
                        nf += 1
                        for kc in range(8):
                            S.op('pe', lambda e, pb=pb, kc=kc, fc=fc, c0=c0, cw=cw, wb=wb: e.matmul(psG[pb][:, 0:cw], WG[wb][:, kc, fc * 128:(fc + 1) * 128], hg[:, kc, c0:c0 + cw],
                                                                                                start=(kc == 0), stop=(kc == 7)), reads=[dWf[wb], dhg], writes=[dpG[pb]])
                        for kc in range(8):
                            S.op('pe', lambda e, pb=pb, kc=kc, fc=fc, c0=c0, cw=cw, wb=wb: e.matmul(psU[pb][:, 0:cw], WU[wb][:, kc, fc * 128:(fc + 1) * 128], hg[:, kc, c0:c0 + cw],
                                                                                                start=(kc == 0), stop=(kc == 7)), reads=[dWf[wb], dhg], writes=[dpU[pb]])
                        S.op('act', lambda e, pb=pb, cw=cw: e.activation(out=sg[pb][:, 0:cw], in_=psG[pb][:, 0:cw], func=AF.Silu), reads=[dpG[pb]], writes=[dsg[pb]])
                        S.op('dve', lambda e, pb=pb, cw=cw, fc=fc: e.tensor_tensor(out=actT[:, fc, 0:cw], in0=psU[pb][:, 0:cw], in1=sg[pb][:, 0:cw], op=ALU.mult),
                             reads=[dpU[pb], dsg[pb]], writes=[dactT])
                    for sub in range(cw // 128):
                        ti = (c0 + sub * 128) // 128
                        for half in range(2):
                            pf = (sub * 2 + half) % 2
                            hsl = slice(half * 512, (half + 1) * 512)
                            for fc in range(nfc):
                                S.op('pe', lambda e, pf=pf, fc=fc, sub=sub, hsl=hsl, wb=wb, nfc=nfc: e.matmul(psF[pf][:], actT[:, fc, sub * 128:(sub + 1) * 128], WD[wb][:, fc, hsl],
                                                                                                            start=(fc == 0), stop=(fc == nfc - 1)), reads=[dactT, dWf[wb]], writes=[dpF[pf]])
                            if moe:
                                if pi == 0:
                                    S.op('dve', lambda e, pf=pf, ti=ti, hsl=hsl, ex_=ex_: e.tensor_scalar(out=facc[:, ti, hsl], in0=psF[pf][:], scalar1=dns[:, ti, ex_:ex_ + 1], scalar2=None, op0=ALU.mult),
                                         reads=[dpF[pf], ddns], writes=[dfacc])
                                else:
                                    S.op('dve', lambda e, pf=pf, ti=ti, hsl=hsl, ex_=ex_: e.scalar_tensor_tensor(out=facc[:, ti, hsl], in0=psF[pf][:], scalar=dns[:, ti, ex_:ex_ + 1], in1=facc[:, ti, hsl],
                                                                                                              op0=ALU.mult, op1=ALU.add), reads=[dpF[pf], ddns, dfacc], writes=[dfacc])
                            else:
                                if pi == 0:
                                    S.op('act', lambda e, pf=pf, ti=ti, hsl=hsl: e.activation(out=facc[:, ti, hsl], in_=psF[pf][:], func=AF.Copy), reads=[dpF[pf]], writes=[dfacc])
                                else:
                                    S.op('dve', lambda e, pf=pf, ti=ti, hsl=hsl: e.tensor_tensor(out=facc[:, ti, hsl], in0=psF[pf][:], in1=facc[:, ti, hsl], op=ALU.add),
                                         reads=[dpF[pf], dfacc], writes=[dfacc])
                if pi != len(pieces) - 1:
                    continue
                if g + 1 < ngrp:
                    g0n = (g + 1) * 1024
                    gwn = min(1024, ntok - g0n)
                    S.dma('sp', hg[:, :, 0:gwn], H2Tv[:, :, g0n:g0n + gwn], writes=[dhg])
                    if moe:
                        S.dma('sp', dns[:, 0:gwn // 128, :], DENSE[g0n:g0n + gwn, :].rearrange("(t p) n -> p t n", p=128), writes=[ddns])
                for ti in range(nt):
                    r0 = g0 + ti * 128
                    v = 1 if r0 < nctx else 0
                    S.dma('sp', xm_2[:], XMID[r0:r0 + 128, :], writes=[dxm])
                    S.op('dve', lambda e, ti=ti, v=v: e.tensor_tensor(out=tmp_2[:], in0=facc[:, ti, :], in1=AD[v][:, 3072:4096], op=ALU.mult), reads=[dfacc, dAD[v]], writes=[dtmp])
                    S.op('dve', lambda e: e.scalar_tensor_tensor(out=z_2[:], in0=xm_2[:], scalar=ALPHA, in1=tmp_2[:], op0=ALU.mult, op1=ALU.add), reads=[dxm, dtmp], writes=[dz])
                    ln.norm(xn_2[:], dxn, z_2, dz, L2[:, 0, :], L2[:, 1, :], dL2, tmp_2, dtmp)
                    S.dma('sp', XOUT[r0:r0 + 128, :], xn_2[:], reads=[dxn])
                    if not last:
                        ln.norm(hb[:], dhb, xn_2, dxn, ADN[v][:, 1024:2048], ADN[v][:, 0:1024], dADN[v], tmp_2, dtmp)
                        emit_T(S, hb, dhb, hTn[:], dhTn, identb[:], dib, psTb, dpsTb)
                        S.dma('pool', HTNv[:, :, r0:r0 + 128], hTn[:], reads=[dhTn])


import ml_dtypes

BF = ml_dtypes.bfloat16


def _cols_tm(i):
    c = []
    for base in (256, 512, 768):
        c += list(range(base + 64 * i, base + 64 * i + 64))
    c += [1024 + i, 1028 + i, 1032 + i, 1036 + i]
    c += list(range(1296 + 128 * i, 1296 + 128 * i + 128))
    k = i // 2
    c += list(range(1808 + 64 * k, 1808 + 64 * k + 64)) + list(range(1936 + 64 * k, 1936 + 64 * k + 64))
    for base in (2320, 2576, 2832):
        c += list(range(base + 64 * i, base + 64 * i + 64))
    return c


def _cols_fm(i):
    c = list(range(64 * i, 64 * i + 64)) + list(range(256 + 64 * i, 256 + 64 * i + 64))
    c += list(range(2064 + 64 * i, 2064 + 64 * i + 64)) + list(range(2320 + 64 * i, 2320 + 64 * i + 64))
    c += list(range(1040 + 64 * i, 1040 + 64 * i + 64)) + list(range(3088, 3120))
    return c


def _rope_tabs():
    rows = TL // 64
    row = np.repeat(np.arange(rows, dtype=np.float32), 64)
    col = np.tile(np.arange(64, dtype=np.float32), rows)
    inv = np.power(np.float32(10000.0), -np.arange(16, dtype=np.float32) / 16).astype(np.float32)
    ar = row[:, None] * inv
    ac = col[:, None] * inv
    cos = np.concatenate([np.cos(ar), np.cos(ar), np.cos(ac), np.cos(ac)], axis=1).astype(np.float32)
    sin = np.concatenate([-np.sin(ar), np.sin(ar), -np.sin(ac), np.sin(ac)], axis=1).astype(np.float32)
    return np.ascontiguousarray(np.tile(cos, (1, 3))), np.ascontiguousarray(np.tile(sin, (1, 3)))


def _dt(a):
    return BF16 if a.dtype == BF else F32


def _launch(build, in_maps, outs):
    nc = bass.Bass("TRN2", target_bir_lowering=False)
    aps = {k: nc.dram_tensor(k, list(v.shape), _dt(v), kind="ExternalInput").ap() for k, v in in_maps[0].items()}
    oaps = {k: nc.dram_tensor(k, list(shp), dt, kind="ExternalOutput").ap() for k, (shp, dt) in outs.items()}
    S = Sched(nc)
    build(nc, S, aps, oaps)
    S.emit()
    res = run_bass_kernel_spmd(nc, in_maps, core_ids=list(range(len(in_maps))))
    return res.results


def kernel_unfused(x, c, ctx, c_ctx, w_ada, b_ada, w_in, ml_gate_b, ml_norm_g, gq_qnorm_g, gq_knorm_g,
           gl_w2, gl_b2, gl_norm_g, w_branch, w_gate, b_gate, w_out, ln1_g, ln1_b, ln2_g, ln2_b,
           ffd_wg, ffd_wu, ffd_wd, moe_wr, moe_br, moe_wg, moe_wu, moe_wd):
    f32 = lambda a: np.ascontiguousarray(np.asarray(a, dtype=np.float32))
    x, c, ctx, c_ctx = f32(x), f32(c), f32(ctx), f32(c_ctx)
    w_ada, b_ada, w_in = f32(w_ada), f32(b_ada), f32(w_in)
    ident = np.eye(128, dtype=np.float32)
    tri = np.stack([np.triu(np.ones((128, 128), np.float32)), np.tril(np.ones((128, 128), np.float32))])
    ones = np.ones((128, 128), np.float32)
    cos3, sin3 = _rope_tabs()
    FT = fourier_tables()
    NT0 = TC + 4096

    def assemble_hT(res):
        hTb = []
        for b in range(2):
            parts = [np.asarray(res[4 * b]["HTN"])[:, 0:TC]] + [np.asarray(res[4 * b + r]["HTN"])[:, -4096:] for r in range(4)]
            hTb.append(np.ascontiguousarray(np.concatenate(parts, axis=1)))
        return hTb

    ims = []
    for core in range(8):
        b, r = divmod(core, 4)
        ims.append(dict(xin=np.ascontiguousarray(np.concatenate([ctx[b], x[b, r * 4096:(r + 1) * 4096]], axis=0)),
                        cb=c[b], cc=c_ctx, wa=w_ada[0], ba=b_ada[0], ident=ident))
    res = _launch(lambda nc, S, a, o: emit_PRE(S, a['xin'], NT0, TC, a['cb'], a['cc'], a['wa'], a['ba'], a['ident'], o['HTN']),
                  ims, dict(HTN=([D, NT0], BF16)))
    hTb = assemble_hT(res)
    xcur = [np.ascontiguousarray(np.concatenate([ctx[b], x[b, r * 4096:(r + 1) * 4096]], axis=0)) for b in range(2) for r in range(4)]

    for l in range(2):
        last = l == 1
        ims = []
        for core in range(8):
            b, i = divmod(core, 4)
            d = dict(hT=hTb[b], wtm=np.ascontiguousarray(w_in[l][:, _cols_tm(i)]), wfm=np.ascontiguousarray(w_in[l][:, _cols_fm(i)]),
                     cos3=cos3, sin3=sin3, gq=f32(gq_qnorm_g[l]), gk=f32(gq_knorm_g[l]), ident=ident, tri=tri, ones=ones,
                     gb=f32(np.asarray(ml_gate_b)[l][:, i]), mng=f32(np.asarray(ml_norm_g)[l][64 * i:64 * i + 64]),
                     w2=f32(np.asarray(gl_w2)[l][:, :, 64 * i:64 * i + 64]), b2=f32(np.asarray(gl_b2)[l][:, 64 * i:64 * i + 64]),
                     gng=f32(np.asarray(gl_norm_g)[l][64 * i:64 * i + 64]))
            d.update(FT)
            ims.append(d)

        def buildA(nc, S, a, o, l=l):
            PTM = nc.dram_tensor("PTM", [TA, NTM], F32, kind="Internal").ap()
            PFM = nc.dram_tensor("PFM", [NFM, TA], F32, kind="Internal").ap()
            VD = nc.dram_tensor("VD", [TL, 128], F32, kind="Internal").ap()
            emit_A1(S, a['hT'], a['wtm'], a['wfm'], PTM, PFM)
            S.barrier()
            emit_A2(S, PTM, PFM, o['BR'], a['gb'], a['mng'], a['tri'], a['ones'])
            emit_A3(S, PTM, PFM, o['BR'], a['w2'], a['b2'], a['gng'], a['tri'])
            emit_A5(S, PFM, o['BR'], VD, a['f64'], a['c256'], a['f1'], a['tw'], a['c2'], l == 0)
            emit_A4(S, PTM, o['BR'], a['cos3'], a['sin3'], a['gq'], a['gk'], a['ident'], l == 0)

        res = _launch(buildA, ims, dict(BR=([TA, 320], F32)))
        brT = []
        for b in range(2):
            br = np.empty((TA, 1280), np.float32)
            for i in range(4):
                o = np.asarray(res[4 * b + i]["BR"])
                br[:, 64 * i:64 * i + 64] = o[:, 0:64]
                br[:, 256 + 64 * i:256 + 64 * i + 64] = o[:, 64:128]
                br[:, 512 + 128 * i:512 + 128 * i + 128] = o[:, 128:256]
                br[:, 1024 + 64 * i:1024 + 64 * i + 64] = o[:, 256:320]
            brT.append(np.ascontiguousarray(br.T).astype(BF))
        ntok = NT0 if l == 0 else 4096
        nctx = TC if l == 0 else 0
        moe = l % 2 == 1
        j = l // 2
        if moe:
            wg, wu, wd = f32(np.asarray(moe_wg)[j]), f32(np.asarray(moe_wu)[j]), f32(np.asarray(moe_wd)[j])
        else:
            g_, u_, d_ = np.asarray(ffd_wg)[j], np.asarray(ffd_wu)[j], np.asarray(ffd_wd)[j]
            wg = f32(np.stack([g_[:, 0:1408], g_[:, 1408:2816]]))
            wu = f32(np.stack([u_[:, 0:1408], u_[:, 1408:2816]]))
            wd = f32(np.stack([d_[0:1408], d_[1408:2816]]))
        ims = []
        for core in range(8):
            b, r = divmod(core, 4)
            tcols = (list(range(TC)) if nctx else []) + list(range(TC + r * 4096, TC + (r + 1) * 4096))
            d = dict(xin=xcur[core], hT=np.ascontiguousarray(hTb[b][:, tcols]), brT=np.ascontiguousarray(brT[b][:, tcols]),
                     cb=c[b], cc=c_ctx, wa=w_ada[l], ba=b_ada[l],
                     wgate=f32(np.asarray(w_gate)[l]), bgate=f32(np.asarray(b_gate)[l].reshape(32, 128).T), wbr=f32(np.asarray(w_branch)[l]), wout=f32(np.asarray(w_out)[l]),
                     l1g=f32(np.asarray(ln1_g)[l]), l1b=f32(np.asarray(ln1_b)[l]), l2g=f32(np.asarray(ln2_g)[l]), l2b=f32(np.asarray(ln2_b)[l]),
                     wg=wg, wu=wu, wd=wd, ident=ident)
            if not last:
                d.update(wan=w_ada[l + 1], ban=b_ada[l + 1])
            if moe:
                d.update(wr=f32(np.asarray(moe_wr)[j]), brr=f32(np.asarray(moe_br)[j]))
            ims.append(d)

        def buildB(nc, S, a, o, last=last, moe=moe, ntok=ntok, nctx=nctx):
            XMID = nc.dram_tensor("XMID", [ntok, D], F32, kind="Internal").ap()
            H2T = nc.dram_tensor("H2T", [D, ntok], BF16, kind="Internal").ap()
            DENSE = nc.dram_tensor("DENSE", [ntok, 8], F32, kind="Internal").ap()
            emit_B(S, last, moe, ntok, nctx, a['xin'], a['hT'], a['brT'], a['cb'], a['cc'], a['wa'], a['ba'],
                   a.get('wan'), a.get('ban'), a['wgate'], a['bgate'], a['wbr'], a['wout'], a['l1g'], a['l1b'], a['l2g'], a['l2b'],
                   a['wg'], a['wu'], a['wd'], a.get('wr'), a.get('brr'), a['ident'], XMID, H2T, DENSE, o['XOUT'], o.get('HTN'))

        outs = dict(XOUT=([ntok, D], F32))
        if not last:
            outs['HTN'] = ([D, ntok], BF16)
        res = _launch(buildB, ims, outs)
        if not last:
            hTb = assemble_hT(res)
            xcur = [np.ascontiguousarray(np.asarray(res[core]["XOUT"])[TC:]) for core in range(8)]
        else:
            out = np.empty((2, TL, D), np.float32)
            for core in range(8):
                b, r = divmod(core, 4)
                out[b, r * 4096:(r + 1) * 4096] = np.asarray(res[core]["XOUT"])
            return out


GROUPS = [[0, 1, 2, 3], [4, 5, 6, 7]]
NT0 = TC + 4096


def build_fused(nc, S, a, o, stop=None):
    I = lambda name, shape, dt=F32: o[name] if name in o else nc.dram_tensor(name, list(shape), dt, kind="Internal").ap()
    HTN = [I("HTN0", [D, NT0], BF16), I("HTN1", [D, NT0], BF16)]
    HTG = [I("HTG0", [16 * 4 * 64, NT0], BF16), I("HTG1", [16 * 4 * 64, NT0], BF16)]
    BR = [I("BR0", [TA, 320], BF16), I("BR1", [TA, 320], BF16)]
    BRG = [I("BRG0", [13 * 4 * 1280, 320], BF16), I("BRG1", [13 * 4 * 1280, 320], BF16)]
    PTM, PFM, VD = I("PTM", [TA, NTM]), I("PFM", [NFM, TA]), I("VD", [TL, 128])
    XOUT0 = I("XOUT0", [NT0, D])
    XMID = [I("XMID0", [NT0, D]), I("XMID1", [4096, D])]
    H2T = [I("H2T0", [D, NT0], BF16), I("H2T1", [D, 4096], BF16)]
    DENSE = I("DENSE", [4096, 8])
    BRT = [I("BRT0", [1280, NT0], BF16), I("BRT1", [1280, 4096], BF16)]
    emit_PRE(S, a['xin'], NT0, TC, a['cb'], a['cc'], a['wa0'], a['ba0'], a['ident'], HTN[0])
    if stop == 'pre':
        return
    for l in range(2):
        last = l == 1
        S.barrier()
        for ch in range(16):
            S.cc("AllGather", HTG[l][ch * 256:(ch + 1) * 256, :], HTN[l][ch * 64:(ch + 1) * 64, :], GROUPS)
        S.barrier()
        if stop == 'ag%d' % l:
            return
        HTGv = HTG[l].rearrange("(kc hf r i) t -> r hf i kc t", kc=8, hf=2, r=4)

        def hT_fn(t0, W, HTGv=HTGv):
            if t0 < TC:
                r, c0 = 0, t0
            else:
                r, loc = divmod(t0 - TC, 4096)
                c0 = TC + loc
            return [HTGv[r][hf][:, :, c0:c0 + W] for hf in range(2)]

        emit_A1(S, hT_fn, a[f'wtm{l}'], a[f'wfm{l}'], PTM, PFM)
        S.barrier()
        if stop == 'a1_%d' % l:
            return
        emit_A2(S, PTM, PFM, BR[l], a[f'gb{l}'], a[f'mng{l}'], a['tri'], a['ones'])
        emit_A3(S, PTM, PFM, BR[l], a[f'w2{l}'], a[f'b2{l}'], a[f'gng{l}'], a['tri'])
        emit_A5(S, PFM, BR[l], VD, a['f64'], a['c256'], a['f1'], a['tw'], a['c2'], l == 0)
        emit_A4(S, PTM, BR[l], a['cos3'], a['sin3'], a[f'gq{l}'], a[f'gk{l}'], a['ident'], l == 0)
        S.barrier()
        if stop == 'a_%d' % l:
            return
        for ch in range(13):
            S.cc("AllGather", BRG[l][ch * 5120:(ch + 1) * 5120, :], BR[l][ch * 1280:(ch + 1) * 1280, :], GROUPS)
        S.barrier()
        if stop == 'agb%d' % l:
            return
        BRG4 = BRG[l].rearrange("(ch i t) n -> ch t i n", ch=13, i=4)

        def BRG3(row, BRG4=BRG4):
            ch, w0 = divmod(row, 1280)
            return BRG4[ch][w0:w0 + 128, :, :]

        if l == 0:
            emit_B(S, False, False, NT0, TC, a['xin'], HTN[0], (BRG3, a['selI'], BF16, BRT[l]), a['cb'], a['cc'], a['wa0'], a['ba0'], a['wa1'], a['ba1'],
                   a['wgate0'], a['bgate0'], a['wbr0'], a['wout0'], a['l1g0'], a['l1b0'], a['l2g0'], a['l2b0'],
                   a['fwg0'], a['fwu0'], a['fwd0'], None, None, a['ident'], XMID[0], H2T[0], DENSE, XOUT0, HTN[1])
            if stop == 'b0':
                return
        else:
            emit_B(S, True, True, 4096, 0, XOUT0[TC:NT0, :], HTN[1][:, TC:NT0], (BRG3, a['selI'], BF16, BRT[l]), a['cb'], a['cc'], a['wa1'], a['ba1'], None, None,
                   a['wgate1'], a['bgate1'], a['wbr1'], a['wout1'], a['l1g1'], a['l1b1'], a['l2g1'], a['l2b1'],
                   a['fwg1'], a['fwu1'], a['fwd1'], a['wr'], a['brr'], a['ident'], XMID[1], H2T[1], DENSE, o['XOUT'], None)


def kernel(x, c, ctx, c_ctx, w_ada, b_ada, w_in, ml_gate_b, ml_norm_g, gq_qnorm_g, gq_knorm_g,
           gl_w2, gl_b2, gl_norm_g, w_branch, w_gate, b_gate, w_out, ln1_g, ln1_b, ln2_g, ln2_b,
           ffd_wg, ffd_wu, ffd_wd, moe_wr, moe_br, moe_wg, moe_wu, moe_wd):
    f32 = lambda a: np.ascontiguousarray(np.asarray(a, dtype=np.float32))
    x, c, ctx, c_ctx = f32(x), f32(c), f32(ctx), f32(c_ctx)
    w_ada, b_ada, w_in = f32(w_ada), f32(b_ada), f32(w_in)
    ident = np.eye(128, dtype=np.float32)
    tri = np.stack([np.triu(np.ones((128, 128), np.float32)), np.tril(np.ones((128, 128), np.float32))])
    ones = np.ones((128, 128), np.float32)
    cos3, sin3 = _rope_tabs()
    FT = fourier_tables()
    NT0 = TC + 4096
    g0, u0, d0 = np.asarray(ffd_wg)[0], np.asarray(ffd_wu)[0], np.asarray(ffd_wd)[0]
    shared = dict(cc=c_ctx, wa0=w_ada[0], ba0=b_ada[0], wa1=w_ada[1], ba1=b_ada[1],
                  cos3=cos3, sin3=sin3, ident=ident, tri=tri, ones=ones,
                  fwg0=f32(np.stack([g0[:, 0:1408], g0[:, 1408:2816]])), fwu0=f32(np.stack([u0[:, 0:1408], u0[:, 1408:2816]])),
                  fwd0=f32(np.stack([d0[0:1408], d0[1408:2816]])),
                  fwg1=f32(np.asarray(moe_wg)[0]), fwu1=f32(np.asarray(moe_wu)[0]), fwd1=f32(np.asarray(moe_wd)[0]),
                  wr=f32(np.asarray(moe_wr)[0]), brr=f32(np.asarray(moe_br)[0]))
    shared.update(FT)
    for l in range(2):
        shared.update({f"gq{l}": f32(np.asarray(gq_qnorm_g)[l]), f"gk{l}": f32(np.asarray(gq_knorm_g)[l]),
                       f"wgate{l}": f32(np.asarray(w_gate)[l]), f"bgate{l}": f32(np.asarray(b_gate)[l].reshape(32, 128).T),
                       f"wbr{l}": f32(np.asarray(w_branch)[l]), f"wout{l}": f32(np.asarray(w_out)[l]),
                       f"l1g{l}": f32(np.asarray(ln1_g)[l]), f"l1b{l}": f32(np.asarray(ln1_b)[l]),
                       f"l2g{l}": f32(np.asarray(ln2_g)[l]), f"l2b{l}": f32(np.asarray(ln2_b)[l])})
    ims = []
    for core in range(8):
        b, i = divmod(core, 4)
        d = dict(shared)
        d['xin'] = np.ascontiguousarray(np.concatenate([ctx[b], x[b, i * 4096:(i + 1) * 4096]], axis=0))
        d['cb'] = c[b]
        sel = np.zeros((4, 128, 128), np.float32)
        sel[i] = ident
        d['selI'] = sel
        for l in range(2):
            d[f"wtm{l}"] = np.ascontiguousarray(w_in[l][:, _cols_tm(i)])
            d[f"wfm{l}"] = np.ascontiguousarray(w_in[l][:, _cols_fm(i)])
            d[f"gb{l}"] = f32(np.asarray(ml_gate_b)[l][:, i])
            d[f"mng{l}"] = f32(np.asarray(ml_norm_g)[l][64 * i:64 * i + 64])
            d[f"w2{l}"] = f32(np.asarray(gl_w2)[l][:, :, 64 * i:64 * i + 64])
            d[f"b2{l}"] = f32(np.asarray(gl_b2)[l][:, 64 * i:64 * i + 64])
            d[f"gng{l}"] = f32(np.asarray(gl_norm_g)[l][64 * i:64 * i + 64])
        ims.append(d)
    if DBG.get('ims_only'):
        return ims
    res = _launch(build_fused, ims, dict(XOUT=([4096, D], F32)))
    out = np.empty((2, TL, D), np.float32)
    for core in range(8):
        b, r = divmod(core, 4)
        out[b, r * 4096:(r + 1) * 4096] = np.asarray(res[core]["XOUT"])
    return out
```

```python
import numpy as np
from contextlib import ExitStack
import concourse.bass as bass
import concourse.mybir as mybir
from concourse.bass_utils import run_bass_kernel_spmd

F32 = mybir.dt.float32
BF16 = mybir.dt.bfloat16
AF = mybir.ActivationFunctionType
ALU = mybir.AluOpType
AX = mybir.AxisListType

ENG = ('pe', 'act', 'dve', 'pool', 'sp')
SAME_SYNC = {'pe': False, 'act': True, 'dve': True, 'pool': True, 'sp': False}


class Dep:
    __slots__ = ('w', 'r')

    def __init__(self):
        self.w = {}
        self.r = {}


class Sched:
    def __init__(self, nc, ndma=8):
        self.nc = nc
        self.ndma = ndma
        self.prog = {e: [] for e in ENG}
        self.cnt = {e: 0 for e in ENG}
        self.seen = {e: {} for e in ENG}
        self.dcnt = {e: 0 for e in ENG}
        self.stack = ExitStack()
        self.esem = {e: self.stack.enter_context(nc.semaphore("s_" + e)) for e in ('pe', 'act', 'dve', 'pool')}
        self.dsem = {q: [self.stack.enter_context(nc.semaphore("d_%s%d" % (q, i))) for i in range(ndma)]
                     for q in ('sp', 'pool', 'act')}
        self.csem = self.stack.enter_context(nc.semaphore("c_cc"))
        self.ccnt = 0

    def sb(self, shape, dt, name=None):
        t = self.stack.enter_context(self.nc.sbuf_tensor(name, list(shape), dt) if name else self.nc.sbuf_tensor(list(shape), dt))
        return t

    def ps(self, shape, dt=F32, name=None):
        full = 512 if dt == F32 else 1024
        t = self.stack.enter_context(self.nc.psum_tensor([128, full], dt))
        shape = list(shape)
        n = 1
        for d in shape[1:]:
            n *= d
        assert n <= full
        v = t[0:shape[0], 0:n]
        if len(shape) == 3:
            v = v.rearrange("p (a b) -> p a b", a=shape[1])
        return v

    def _need(self, reads, writes):
        need = {}
        for d in reads:
            for k, v in d.w.items():
                if need.get(k, 0) < v:
                    need[k] = v
        for d in writes:
            for k, v in d.w.items():
                if need.get(k, 0) < v:
                    need[k] = v
            for k, v in d.r.items():
                if need.get(k, 0) < v:
                    need[k] = v
        return need

    def op(self, eng, fn, reads=(), writes=()):
        need = self._need(reads, writes)
        mykey = ('e', eng)
        waits = []
        seen = self.seen[eng]
        for k, v in need.items():
            if k == mykey and not SAME_SYNC[eng]:
                continue
            if seen.get(k, 0) < v:
                seen[k] = v
                waits.append((k, v))
        self.cnt[eng] += 1
        n = self.cnt[eng]
        for d in writes:
            d.w = {mykey: n}
            d.r = {}
        for d in reads:
            if d not in writes:
                d.r[mykey] = n
        self.prog[eng].append((waits, fn, mykey))

    def dma(self, q, out, in_, reads=(), writes=(), **kw):
        need = self._need(reads, writes)
        i = self.dcnt[q] % self.ndma
        gen = self.dcnt[q] // self.ndma
        self.dcnt[q] += 1
        key = ('d', q, i)
        val = 16 * (gen + 1)
        if gen > 0:
            need[key] = max(need.get(key, 0), 16 * gen)
        waits = []
        seen = self.seen[q]
        for k, v in need.items():
            if seen.get(k, 0) < v:
                seen[k] = v
                waits.append((k, v))
        for d in writes:
            d.w = {key: val}
            d.r = {}
        for d in reads:
            if d not in writes:
                d.r[key] = val
        self.prog[q].append((waits, (lambda e, out=out, in_=in_, kw=kw: e.dma_start(out=out, in_=in_, **kw)), key))

    def cc(self, kind, out, in_, groups, reads=(), writes=()):
        q = 'pool'
        need = self._need(reads, writes)
        self.ccnt += 1
        key = ('c',)
        val = self.ccnt
        waits = []
        seen = self.seen[q]
        for k, v in need.items():
            if seen.get(k, 0) < v:
                seen[k] = v
                waits.append((k, v))
        for d in writes:
            d.w = {key: val}
            d.r = {}
        for d in reads:
            if d not in writes:
                d.r[key] = val
        self.prog[q].append((waits, (lambda e: e.collective_compute(kind, ALU.bypass, replica_groups=groups, ins=[in_.opt()], outs=[out.opt()])), key))

    def barrier(self):
        allk = {}
        for e in ('pe', 'act', 'dve', 'pool'):
            if self.cnt[e]:
                allk[('e', e)] = self.cnt[e]
        for q in ('sp', 'pool', 'act'):
            for j in range(min(self.dcnt[q], self.ndma)):
                ngen = (self.dcnt[q] - 1 - j) // self.ndma + 1
                allk[('d', q, j)] = 16 * ngen
        if self.ccnt:
            allk[('c',)] = self.ccnt
        for e in ENG:
            waits = []
            for k, v in allk.items():
                if self.seen[e].get(k, 0) < v:
                    self.seen[e][k] = v
                    waits.append((k, v))
            if waits:
                self.prog[e].append((waits, None, None))

    def _sem(self, k):
        if k[0] == 'e':
            return self.esem[k[1]]
        if k[0] == 'c':
            return self.csem
        return self.dsem[k[1]][k[2]]

    def emit(self):
        self.barrier()
        nc = self.nc
        prog = self.prog
        sched = self

        def replay(e, name):
            for waits, fn, key in prog[name]:
                for k, v in waits:
                    e.wait_ge(sched._sem(k), v)
                if fn is None:
                    continue
                ins = fn(e)
                if key[0] == 'e':
                    ins.then_inc(sched.esem[name], 1)
                elif key[0] == 'c':
                    ins.then_inc(sched.csem, 1)
                else:
                    ins.then_inc(sched.dsem[key[1]][key[2]], 16)

        with nc.Block() as block:
            @block.tensor
            def _(e):
                replay(e, 'pe')

            @block.scalar
            def _(e):
                replay(e, 'act')

            @block.vector
            def _(e):
                replay(e, 'dve')

            @block.gpsimd
            def _(e):
                replay(e, 'pool')

            @block.sync
            def _(e):
                replay(e, 'sp')
        self.stack.close()


TL = 16384
TC = 256
TA = TL + TC
NCH = TA // 128
D = 1024
NTM = 644
NFM = 352
MLK, MLV, MLO, GAT, GQQ, GQK, GQV, GLK, GLV, GLR = 0, 64, 128, 192, 196, 324, 388, 452, 516, 580
LN_EPS = 1e-6


class Scope:
    def __init__(self, S):
        self.S = S

    def __enter__(self):
        self.saved = self.S.stack
        self.S.stack = ExitStack()
        return self

    def __exit__(self, *a):
        self.S.barrier()
        self.S.stack.close()
        self.S.stack = self.saved
        return False


def emit_A1(S, hT, wtm, wfm, PTM, PFM, hT_deps=()):
    with Scope(S):
        Wtm = S.sb([128, 8, NTM], BF16)
        Wfm = S.sb([128, 8, NFM], BF16)
        dWt, dWf = Dep(), Dep()
        S.dma('pool', Wtm[:], wtm.rearrange("(kc p) n -> p kc n", p=128), writes=[dWt])
        S.dma('pool', Wfm[:], wfm.rearrange("(kc p) n -> p kc n", p=128), writes=[dWf])
        hb = [S.sb([128, 8, 512], BF16) for _ in range(2)]
        dh = [Dep(), Dep()]
        psA = [S.ps([128, 512]) for _ in range(2)]
        psB = [S.ps([128, 512]) for _ in range(2)]
        dpA = [Dep(), Dep()]
        dpB = [Dep(), Dep()]
        psF = [S.ps([128, 512]) for _ in range(3)]
        dpF = [Dep() for _ in range(3)]
        stg = [S.sb([128, NTM], F32) for _ in range(2)]
        dstg = [Dep(), Dep()]
        stf = [S.sb([128, 512], F32) for _ in range(3)]
        dstf = [Dep() for _ in range(3)]
        hT_fn = hT if callable(hT) else (lambda t0, W, hTv=hT.rearrange("(kc p) t -> p kc t", p=128): hTv[:, :, t0:t0 + W])
        frows = [(0, 128), (128, 128), (256, 96)]
        tiles = [(0, 256)] + [(256 + 512 * j, 512) for j in range(32)]
        nsub = 0
        for st, (t0, W) in enumerate(tiles):
            b = st % 2
            src = hT_fn(t0, W)
            if isinstance(src, list):
                for hf in range(2):
                    S.dma('sp', hb[b][hf * 64:(hf + 1) * 64, :, 0:W], src[hf], writes=[dh[b]], reads=list(hT_deps))
            else:
                S.dma('sp', hb[b][:, :, 0:W], src, writes=[dh[b]], reads=list(hT_deps))
            for sub in range(W // 128):
                pb = nsub % 2
                nsub += 1
                for kc in range(8):
                    S.op('pe', lambda e, pb=pb, b=b, kc=kc, sub=sub: e.matmul(
                        psA[pb][:, :], hb[b][:, kc, sub * 128:(sub + 1) * 128], Wtm[:, kc, 0:512],
                        start=(kc == 0), stop=(kc == 7)), reads=[dh[b], dWt], writes=[dpA[pb]])
                for kc in range(8):
                    S.op('pe', lambda e, pb=pb, b=b, kc=kc, sub=sub: e.matmul(
                        psB[pb][:, 0:NTM - 512], hb[b][:, kc, sub * 128:(sub + 1) * 128], Wtm[:, kc, 512:NTM],
                        start=(kc == 0), stop=(kc == 7)), reads=[dh[b], dWt], writes=[dpB[pb]])
                S.op('act', lambda e, pb=pb: e.activation(out=stg[pb][:, 0:512], in_=psA[pb][:, :], func=AF.Copy),
                     reads=[dpA[pb]], writes=[dstg[pb]])
                S.op('dve', lambda e, pb=pb: e.tensor_copy(out=stg[pb][:, 512:NTM], in_=psB[pb][:, 0:NTM - 512]),
                     reads=[dpB[pb]], writes=[dstg[pb]])
                r0 = t0 + sub * 128
                S.dma('sp', PTM[r0:r0 + 128, :], stg[pb][:, :], reads=[dstg[pb]])
            for g, (f0, fn) in enumerate(frows):
                for kc in range(8):
                    S.op('pe', lambda e, g=g, b=b, kc=kc, f0=f0, fn=fn, W=W: e.matmul(
                        psF[g][0:fn, 0:W], Wfm[:, kc, f0:f0 + fn], hb[b][:, kc, 0:W],
                        start=(kc == 0), stop=(kc == 7)), reads=[dh[b], dWf], writes=[dpF[g]])
                eng = 'act' if g == 1 else 'dve'
                if eng == 'act':
                    S.op('act', lambda e, g=g, fn=fn, W=W: e.activation(out=stf[g][0:fn, 0:W], in_=psF[g][0:fn, 0:W], func=AF.Copy),
                         reads=[dpF[g]], writes=[dstf[g]])
                else:
                    S.op('dve', lambda e, g=g, fn=fn, W=W: e.tensor_copy(out=stf[g][0:fn, 0:W], in_=psF[g][0:fn, 0:W]),
                         reads=[dpF[g]], writes=[dstf[g]])
                S.dma('pool', PFM[f0:f0 + fn, t0:t0 + W], stf[g][0:fn, 0:W], reads=[dstf[g]])


def emit_A4(S, PTM, BR, cos3, sin3, gq, gk, ident, with_ctx_q):
    with Scope(S):
        QK = S.sb([128, 2, TA], BF16)
        VP = S.sb([128, NCH, 65], BF16)
        identb = S.sb([128, 128], BF16)
        identf = S.sb([128, 128], F32)
        Gq = S.sb([128, 64], F32)
        Gk = S.sb([128, 64], F32)
        dC = Dep()
        S.dma('pool', identb[:], ident, writes=[dC])
        S.dma('sp', identf[:], ident, writes=[dC])
        S.dma('sp', Gq[:], gq.partition_broadcast(128), writes=[dC])
        S.dma('sp', Gk[:], gk.partition_broadcast(128), writes=[dC])
        dQK, dVP = Dep(), Dep()
        S.op('pool', lambda e: e.memset(VP[:, :, 64:65], 1.0), writes=[dVP])
        with Scope(S):
            xin = [S.sb([128, 256], F32) for _ in range(2)]
            cs = [S.sb([128, 192], F32) for _ in range(2)]
            sn = [S.sb([128, 192], F32) for _ in range(2)]
            dx = [Dep(), Dep()]
            sq_ = [S.sb([128, 192], F32) for _ in range(2)]
            ss_ = [S.sb([128, 3], F32) for _ in range(2)]
            rstd_ = [S.sb([128, 3], F32) for _ in range(2)]
            xn_ = [S.sb([128, 192], F32) for _ in range(2)]
            t1_ = [S.sb([128, 192], F32) for _ in range(2)]
            t2_ = [S.sb([128, 192], F32) for _ in range(2)]
            xr = [S.sb([128, 256], BF16) for _ in range(2)]
            dxr = [Dep(), Dep()]
            dsq_, dss_, drs_, dxn_, dt1_, dt2_ = [[Dep(), Dep()] for _ in range(6)]
            psT = [S.ps([128, 2, 128], BF16) for _ in range(2)]
            dpsT = [Dep(), Dep()]
            for c in range(NCH):
                b = c % 2
                r0 = c * 128
                sq, ss, rstd, xn, t1, t2 = sq_[b], ss_[b], rstd_[b], xn_[b], t1_[b], t2_[b]
                dsq, dss, drs, dxn, dt1, dt2 = dsq_[b], dss_[b], drs_[b], dxn_[b], dt1_[b], dt2_[b]
                lat = c >= 2
                S.dma('sp', xin[b][:], PTM[r0:r0 + 128, GQQ:GQQ + 256], writes=[dx[b]])
                if lat:
                    S.dma('sp', cs[b][:], cos3[r0 - TC:r0 - TC + 128, :], writes=[dx[b]])
                    S.dma('sp', sn[b][:], sin3[r0 - TC:r0 - TC + 128, :], writes=[dx[b]])
                S.op('pool', lambda e, sq=sq, ss=ss, rstd=rstd, xn=xn, t1=t1, t2=t2, b=b: e.tensor_tensor(out=sq[:], in0=xin[b][:, 0:192], in1=xin[b][:, 0:192], op=ALU.mult),
                     reads=[dx[b]], writes=[dsq])
                S.op('dve', lambda e, sq=sq, ss=ss, rstd=rstd, xn=xn, t1=t1, t2=t2: e.tensor_reduce(out=ss[:], in_=sq[:].rearrange("p (h d) -> p h d", h=3), axis=AX.X, op=ALU.add),
                     reads=[dsq], writes=[dss])
                S.op('act', lambda e, sq=sq, ss=ss, rstd=rstd, xn=xn, t1=t1, t2=t2: e.activation(out=rstd[:], in_=ss[:], func=AF.Sqrt, bias=LN_EPS, scale=1.0 / 64),
                     reads=[dss], writes=[drs])
                S.op('dve', lambda e, sq=sq, ss=ss, rstd=rstd, xn=xn, t1=t1, t2=t2: e.reciprocal(out=rstd[:], in_=rstd[:]),
                     reads=[drs], writes=[drs])
                for hh in range(3):
                    G = Gq if hh < 2 else Gk
                    dst = xn if lat else xr[b]
                    S.op('dve', lambda e, sq=sq, ss=ss, rstd=rstd, xn=xn, t1=t1, t2=t2, hh=hh, G=G, dst=dst, b=b: e.scalar_tensor_tensor(
                        out=dst[:, hh * 64:(hh + 1) * 64], in0=xin[b][:, hh * 64:(hh + 1) * 64], scalar=rstd[:, hh:hh + 1],
                        in1=G[:], op0=ALU.mult, op1=ALU.mult), reads=[dx[b], drs, dC], writes=[dxn if lat else dxr[b]])
                if lat:
                    S.op('pool', lambda e, sq=sq, ss=ss, rstd=rstd, xn=xn, t1=t1, t2=t2, b=b: e.tensor_tensor(out=t1[:], in0=xn[:], in1=cs[b][:], op=ALU.mult),
                         reads=[dxn, dx[b]], writes=[dt1])
                    xv = xn[:].rearrange("p (g f s) -> p g f s", g=6, f=2)
                    for f in range(2):
                        S.op('dve', lambda e, sq=sq, ss=ss, rstd=rstd, xn=xn, t1=t1, t2=t2, f=f, b=b, xv=xv: e.tensor_tensor(
                            out=t2[:].rearrange("p (g f s) -> p g f s", g=6, f=2)[:, :, f, :],
                            in0=xv[:, :, 1 - f, :],
                            in1=sn[b][:].rearrange("p (g f s) -> p g f s", g=6, f=2)[:, :, f, :], op=ALU.mult),
                            reads=[dxn, dx[b]], writes=[dt2])
                    S.op('dve', lambda e, sq=sq, ss=ss, rstd=rstd, xn=xn, t1=t1, t2=t2, b=b: e.tensor_tensor(out=xr[b][:, 0:192], in0=t1[:], in1=t2[:], op=ALU.add),
                         reads=[dt1, dt2], writes=[dxr[b]])
                S.op('act', lambda e, sq=sq, ss=ss, rstd=rstd, xn=xn, t1=t1, t2=t2, b=b: e.activation(out=xr[b][:, 192:256], in_=xr[b][:, 128:192], func=AF.Copy),
                     reads=[dxr[b]], writes=[dxr[b]])
                S.op('act', lambda e, sq=sq, ss=ss, rstd=rstd, xn=xn, t1=t1, t2=t2, b=b, c=c: e.activation(out=VP[:, c, 0:64], in_=xin[b][:, 192:256], func=AF.Copy),
                     reads=[dx[b]], writes=[dVP])
                for j in range(2):
                    S.op('pe', lambda e, sq=sq, ss=ss, rstd=rstd, xn=xn, t1=t1, t2=t2, b=b, j=j: e.transpose(psT[b][:, j, :], xr[b][:, j * 128:(j + 1) * 128], identb[:]),
                         reads=[dxr[b], dC], writes=[dpsT[b]])
                S.op('act', lambda e, sq=sq, ss=ss, rstd=rstd, xn=xn, t1=t1, t2=t2, b=b, r0=r0: e.activation(out=QK[:, :, r0:r0 + 128], in_=psT[b][:, :, :], func=AF.Copy),
                     reads=[dpsT[b]], writes=[dQK])
        with Scope(S):
            psS = [S.ps([128, 512]) for _ in range(4)]
            dS_ = [Dep() for _ in range(4)]
            Pb = [S.sb([128, 512], BF16) for _ in range(4)]
            dP = [Dep() for _ in range(4)]
            psO = [S.ps([128, 512]) for _ in range(2)]
            dO = [Dep(), Dep()]
            OTs = S.sb([128, 512], F32)
            dOT = Dep()
            psT2 = S.ps([128, 4, 65])
            dT2 = Dep()
            rec = S.sb([128, 4], F32)
            drec = Dep()
            ostg = [S.sb([128, 4, 64], BR.dtype) for _ in range(2)]
            dos = [Dep(), Dep()]
            blocks = []
            if with_ctx_q:
                blocks.append((0, 256, 2))
            for qb in range(32):
                blocks.append((TC + qb * 512, 512, NCH))
            pairs = []
            for bi, (t0, W, nk) in enumerate(blocks):
                for j in range(nk):
                    pairs.append((bi, t0, W, nk, j))

            def emit_S(p):
                bi, t0, W, nk, j = pairs[p]
                for hh in range(2):
                    s = (2 * p + hh) % 4
                    S.op('pe', lambda e, s=s, hh=hh, j=j, t0=t0, W=W: e.matmul(psS[s][:, 0:W], QK[hh * 64:(hh + 1) * 64, 1, j * 128:(j + 1) * 128],
                                                                              QK[hh * 64:(hh + 1) * 64, 0, t0:t0 + W], start=True, stop=True),
                         reads=[dQK], writes=[dS_[s]])

            def finish(hh, t0, W):
                S.op('dve', lambda e: e.tensor_copy(out=OTs[0:65, 0:W], in_=psO[hh][0:65, 0:W]), reads=[dO[hh]], writes=[dOT])
                ns = W // 128
                for s in range(ns):
                    S.op('pe', lambda e, s=s: e.transpose(psT2[:, s, :], OTs[0:65, s * 128:(s + 1) * 128], identf[0:65, 0:65]),
                         reads=[dOT, dC], writes=[dT2])
                S.op('dve', lambda e: e.reciprocal(out=rec[:, 0:ns], in_=psT2[:, 0:ns, 64]), reads=[dT2], writes=[drec])
                for s in range(ns):
                    S.op('dve', lambda e, s=s: e.tensor_scalar(out=ostg[hh][:, s, :], in0=psT2[:, s, 0:64], scalar1=rec[:, s:s + 1],
                                                               scalar2=None, op0=ALU.mult), reads=[dT2, drec], writes=[dos[hh]])
                S.dma('sp', BR[t0:t0 + W, 128 + hh * 64:128 + hh * 64 + 64].rearrange("(s p) n -> p s n", p=128),
                      ostg[hh][:, 0:ns, :], reads=[dos[hh]])

            emit_S(0)
            for p in range(len(pairs)):
                bi, t0, W, nk, j = pairs[p]
                if p + 1 < len(pairs):
                    emit_S(p + 1)
                for hh in range(2):
                    s = (2 * p + hh) % 4
                    S.op('act', lambda e, s=s, W=W: e.activation(out=Pb[s][:, 0:W], in_=psS[s][:, 0:W], func=AF.Exp, scale=0.125),
                         reads=[dS_[s]], writes=[dP[s]])
                for hh in range(2):
                    s = (2 * p + hh) % 4
                    S.op('pe', lambda e, s=s, W=W, j=j, nk=nk, hh=hh: e.matmul(psO[hh][0:65, 0:W], VP[:, j, 0:65], Pb[s][:, 0:W],
                                                                              start=(j == 0), stop=(j == nk - 1)),
                         reads=[dP[s], dVP], writes=[dO[hh]])
                if j == nk - 1:
                    for hh in range(2):
                        finish(hh, t0, W)


def emit_A2(S, PTM, PFM, BR, gate_b, norm_g, tri, ones):
    LN8 = float(np.log(8.0))
    with Scope(S):
        Hd = [S.sb([128, NCH, 64], F32) for _ in range(2)]
        dH = [Dep(), Dep()]
        with Scope(S):
            QT = S.sb([64, TA], BF16)
            KT = S.sb([64, TA], BF16)
            Ktm = S.sb([128, NCH, 64], BF16)
            V2 = [S.sb([128, NCH, 65], BF16) for _ in range(2)]
            G = S.sb([128, NCH, 4], F32)
            gb = S.sb([128, 4], F32)
            nb = S.sb([128, 4], F32)
            bse = S.sb([128, 2], F32)
            TR = S.sb([128, 2, 128], F32)
            ON = S.sb([128, 128], F32)
            E = [S.sb([128, NCH], F32) for _ in range(2)]
            IRS = [S.sb([128, NCH], F32) for _ in range(2)]
            DEC = [S.sb([128, NCH], F32) for _ in range(2)]
            dQT, dKT, dK, dG, dc, dV2 = Dep(), Dep(), Dep(), Dep(), Dep(), [Dep(), Dep()]
            dE, dIRS, dDEC = [Dep(), Dep()], [Dep(), Dep()], [Dep(), Dep()]
            for j in range(4):
                a, bnd = j * (TA // 4), (j + 1) * (TA // 4)
                S.dma('pool', QT[:, a:bnd], PFM[0:64, a:bnd], writes=[dQT])
                S.dma('pool', KT[:, a:bnd], PFM[64:128, a:bnd], writes=[dKT])
            PTMv = PTM.rearrange("(c p) n -> p c n", p=128)
            for j in range(5):
                S.dma('pool', Ktm[:, j * 26:(j + 1) * 26, :], PTMv[:, j * 26:(j + 1) * 26, MLK:MLK + 64], writes=[dK])
            S.dma('sp', G[:], PTMv[:, :, GAT:GAT + 4], writes=[dG])
            S.dma('sp', gb[:], gate_b.partition_broadcast(128), writes=[dc])
            S.dma('sp', TR[:], tri.rearrange("a p n -> p a n"), writes=[dc])
            S.dma('sp', ON[:], ones, writes=[dc])
            S.op('dve', lambda e: e.tensor_scalar(out=nb[:], in0=gb[:], scalar1=-1.0, scalar2=None, op0=ALU.mult), reads=[dc], writes=[dc])
            S.op('dve', lambda e: e.tensor_scalar(out=bse[:], in0=gb[:].rearrange("p (a b) -> p a b", b=2)[:, :, 0], scalar1=-LN8, scalar2=None, op0=ALU.add),
                 reads=[dc], writes=[dc])
            with Scope(S):
                V = Hd[1]
                dV = Dep()
                for j in range(5):
                    S.dma('sp', V[:, j * 26:(j + 1) * 26, :], PTMv[:, j * 26:(j + 1) * 26, MLV:MLV + 64], writes=[dV])
                L = [S.sb([128, NCH], F32) for _ in range(2)]
                e1 = S.sb([128, NCH], F32)
                tmp = S.sb([128, NCH], F32)
                psC = [S.ps([128, 2, NCH]) for _ in range(2)]
                dL, de1, dtmp, dpc = [Dep(), Dep()], Dep(), Dep(), [Dep(), Dep()]
                for d in range(2):
                    gi, gf = 2 * d, 2 * d + 1
                    S.op('act', lambda e, gf=gf: e.activation(out=e1[:], in_=G[:, :, gf], func=AF.Exp, scale=-1.0, bias=nb[:, gf:gf + 1]),
                         reads=[dG, dc], writes=[de1])
                    S.op('act', lambda e, d=d: e.activation(out=L[d][:], in_=e1[:], func=AF.Ln, bias=1.0), reads=[de1], writes=[dL[d]])
                    S.op('pe', lambda e, d=d: e.matmul(psC[d][:, 0, :], TR[:, d, :], L[d][:], start=True, stop=True), reads=[dL[d], dc], writes=[dpc[d]])
                    S.op('pe', lambda e, d=d: e.matmul(psC[d][:, 1, :], ON[:], L[d][:], start=True, stop=True), reads=[dL[d], dc], writes=[dpc[d]])
                    S.op('dve', lambda e, d=d, gi=gi: e.tensor_tensor(out=tmp[:], in0=G[:, :, gi], in1=psC[d][:, 0, :], op=ALU.add),
                         reads=[dG, dpc[d]], writes=[dtmp])
                    S.op('act', lambda e, d=d: e.activation(out=E[d][:], in_=tmp[:], func=AF.Exp, bias=bse[:, d:d + 1]), reads=[dtmp, dc], writes=[dE[d]])
                    S.op('act', lambda e, d=d: e.activation(out=IRS[d][:], in_=psC[d][:, 0, :], func=AF.Exp), reads=[dpc[d]], writes=[dIRS[d]])
                    S.op('act', lambda e, d=d: e.activation(out=DEC[d][:], in_=psC[d][:, 1, :], func=AF.Exp, scale=-1.0), reads=[dpc[d]], writes=[dDEC[d]])
                    S.op('dve', lambda e, d=d: e.tensor_tensor(out=V2[d][:, :, 0:64], in0=V[:], in1=E[d][:].unsqueeze(2).to_broadcast([128, NCH, 64]), op=ALU.mult),
                         reads=[dV, dE[d]], writes=[dV2[d]])
                    S.op('pool', lambda e, d=d: e.tensor_copy(out=V2[d][:, :, 64], in_=E[d][:]), reads=[dE[d]], writes=[dV2[d]])
            with Scope(S):
                C = [S.sb([64, 65], F32) for _ in range(2)]
                Cb = [S.sb([64, 65], BF16) for _ in range(2)]
                dC_, dCb = [Dep(), Dep()], [Dep(), Dep()]
                psS = [S.ps([128, 128]) for _ in range(2)]
                psH = [S.ps([128, 65]) for _ in range(2)]
                psU = [S.ps([64, 65]) for _ in range(2)]
                dpS, dpH, dpU = [Dep(), Dep()], [Dep(), Dep()], [Dep(), Dep()]
                Sm = [S.sb([128, 128], BF16) for _ in range(2)]
                dSm = [Dep(), Dep()]
                dab = [S.sb([128, 1], F32) for _ in range(2)]
                fac = [S.sb([128, 1], F32) for _ in range(2)]
                ddab, dfac = [Dep(), Dep()], [Dep(), Dep()]
                tmpC = [S.sb([64, 65], F32) for _ in range(2)]
                dtC = [Dep(), Dep()]
                for d in range(2):
                    S.op('pool', lambda e, d=d: e.memset(C[d][:], 0.0), writes=[dC_[d]])
                    S.op('pool', lambda e, d=d: e.memset(Cb[d][:], 0.0), writes=[dCb[d]])
                order = [list(range(NCH)), [1, 0] + list(range(NCH - 1, 1, -1))]
                for k in range(NCH):
                    for d in range(2):
                        c = order[d][k]
                        sl = slice(c * 128, (c + 1) * 128)
                        S.op('pe', lambda e, d=d, sl=sl: e.matmul(psS[d][:], KT[:, sl], QT[:, sl], start=True, stop=True), reads=[dKT, dQT], writes=[dpS[d]])
                        S.op('dve', lambda e, d=d: e.tensor_tensor(out=Sm[d][:], in0=psS[d][:], in1=TR[:, d, :], op=ALU.mult), reads=[dpS[d], dc], writes=[dSm[d]])
                        S.op('pe', lambda e, d=d, c=c: e.matmul(psH[d][:], Sm[d][:], V2[d][:, c, :], start=True, stop=False), reads=[dSm[d], dV2[d]], writes=[dpH[d]])
                        S.op('pe', lambda e, d=d, sl=sl: e.matmul(psH[d][:], QT[:, sl], Cb[d][:], start=False, stop=True), reads=[dQT, dCb[d]], writes=[dpH[d]])
                        S.op('pe', lambda e, d=d, c=c: e.matmul(psU[d][:], Ktm[:, c, :], V2[d][:, c, :], start=True, stop=True), reads=[dK, dV2[d]], writes=[dpU[d]])
                        S.op('act', lambda e, d=d: e.activation(out=dab[d][:], in_=psH[d][:, 64:65], func=AF.Abs), reads=[dpH[d]], writes=[ddab[d]])
                        S.op('dve', lambda e, d=d, c=c: e.tensor_scalar(out=fac[d][:], in0=dab[d][:], scalar1=IRS[d][:, c:c + 1], scalar2=None, op0=ALU.max),
                             reads=[ddab[d], dIRS[d]], writes=[dfac[d]])
                        S.op('dve', lambda e, d=d: e.reciprocal(out=fac[d][:], in_=fac[d][:]), reads=[dfac[d]], writes=[dfac[d]])
                        S.op('act', lambda e, d=d, c=c: e.activation(out=Hd[d][:, c, :], in_=psH[d][:, 0:64], func=AF.Copy, scale=fac[d][:, 0:1]),
                             reads=[dpH[d], dfac[d]], writes=[dH[d]])
                        S.op('dve', lambda e, d=d: e.tensor_tensor(out=tmpC[d][:], in0=psU[d][:], in1=C[d][:], op=ALU.add), reads=[dpU[d], dC_[d]], writes=[dtC[d]])
                        S.op('dve', lambda e, d=d, c=c: e.tensor_scalar(out=C[d][:], in0=tmpC[d][:], scalar1=DEC[d][0:64, c:c + 1], scalar2=None, op0=ALU.mult),
                             reads=[dtC[d], dDEC[d]], writes=[dC_[d]])
                        S.op('act', lambda e, d=d, c=c: e.activation(out=Cb[d][:], in_=tmpC[d][:], func=AF.Copy, scale=DEC[d][0:64, c:c + 1]),
                             reads=[dtC[d], dDEC[d]], writes=[dCb[d]])
        with Scope(S):
            PTMv = PTM.rearrange("(c p) n -> p c n", p=128)
            O = S.sb([128, NCH, 64], F32)
            NG = S.sb([128, 64], F32)
            sm = S.sb([128, NCH], F32)
            vr = S.sb([128, NCH], F32)
            dO, dn, dsm, dvr = Dep(), Dep(), Dep(), Dep()
            for j in range(5):
                S.dma('sp', O[:, j * 26:(j + 1) * 26, :], PTMv[:, j * 26:(j + 1) * 26, MLO:MLO + 64], writes=[dO])
            S.dma('sp', NG[:], norm_g.partition_broadcast(128), writes=[dn])
            H, H2 = Hd[0], Hd[1]
            S.op('dve', lambda e: e.tensor_tensor(out=H[:], in0=H[:], in1=H2[:], op=ALU.add), reads=[dH[0], dH[1]], writes=[dH[0]])
            _finish_norm(S, H, H2, dH[0], dH[1], sm, vr, dsm, dvr, True)
            S.op('pool', lambda e: e.tensor_tensor(out=H[:], in0=H[:], in1=NG[:].unsqueeze(1).to_broadcast([128, NCH, 64]), op=ALU.mult), reads=[dH[0], dn], writes=[dH[0]])
            S.op('act', lambda e: e.activation(out=O[:], in_=O[:], func=AF.Sigmoid), reads=[dO], writes=[dO])
            Hout = H if BR.dtype == F32 else S.sb([128, NCH, 64], BR.dtype)
            S.op('dve', lambda e: e.tensor_tensor(out=Hout[:], in0=H[:], in1=O[:], op=ALU.mult), reads=[dH[0], dO], writes=[dH[0]])
            for j in range(5):
                S.dma('sp', BR.rearrange("(c p) n -> p c n", p=128)[:, j * 26:(j + 1) * 26, 0:64], Hout[:, j * 26:(j + 1) * 26, :], reads=[dH[0]])


def _finish_norm(S, H, T, dH, dT, sm, vr, dsm, dvr, center):
    bc = lambda a: a[:].unsqueeze(2).to_broadcast([128, NCH, 64])
    if center:
        S.op('dve', lambda e: e.tensor_reduce(out=sm[:], in_=H[:], axis=AX.X, op=ALU.add), reads=[dH], writes=[dsm])
        S.op('dve', lambda e: e.tensor_scalar(out=sm[:], in0=sm[:], scalar1=1.0 / 64, scalar2=None, op0=ALU.mult), reads=[dsm], writes=[dsm])
        S.op('dve', lambda e: e.tensor_tensor(out=H[:], in0=H[:], in1=bc(sm), op=ALU.subtract), reads=[dH, dsm], writes=[dH])
    S.op('pool', lambda e: e.tensor_tensor(out=T[:], in0=H[:], in1=H[:], op=ALU.mult), reads=[dH], writes=[dT])
    S.op('dve', lambda e: e.tensor_reduce(out=vr[:], in_=T[:], axis=AX.X, op=ALU.add), reads=[dT], writes=[dvr])
    S.op('act', lambda e: e.activation(out=vr[:], in_=vr[:], func=AF.Sqrt, bias=LN_EPS, scale=1.0 / 64), reads=[dvr], writes=[dvr])
    S.op('dve', lambda e: e.reciprocal(out=vr[:], in_=vr[:]), reads=[dvr], writes=[dvr])
    S.op('dve', lambda e: e.tensor_tensor(out=H[:], in0=H[:], in1=bc(vr), op=ALU.mult), reads=[dH, dvr], writes=[dH])


def emit_A3(S, PTM, PFM, BR, w2, b2, norm_g, tri):
    GW = 8
    groups = [(0, 2)] + [(2 + 8 * j, 8) for j in range(16)]
    with Scope(S):
        Hd = [S.sb([128, NCH, 64], F32) for _ in range(2)]
        dH = [Dep(), Dep()]
        PTMv = PTM.rearrange("(c p) n -> p c n", p=128)
        with Scope(S):
            TR = S.sb([128, 2, 128], F32)
            W2 = S.sb([16, 2, 64], BF16)
            B2 = S.sb([128, 2, 64], F32)
            dc = Dep()
            S.dma('sp', TR[:], tri.rearrange("a p n -> p a n"), writes=[dc])
            S.dma('pool', W2[:], w2.rearrange("a k n -> k a n"), writes=[dc])
            S.dma('sp', B2[:], b2.rearrange("a n -> (a n)").partition_broadcast(128), writes=[dc])
            St = [S.sb([64, 64], F32) for _ in range(2)]
            Sb = [S.sb([64, 64], BF16) for _ in range(2)]
            dSt, dSb = [Dep(), Dep()], [Dep(), Dep()]
            for d in range(2):
                S.op('pool', lambda e, d=d: e.memset(St[d][:], 0.0), writes=[dSt[d]])
                S.op('pool', lambda e, d=d: e.memset(Sb[d][:], 0.0), writes=[dSb[d]])
            AT = [S.sb([16, GW * 128], BF16) for _ in range(2)]
            QTf = [S.sb([64, GW * 128], F32) for _ in range(2)]
            KTf = [S.sb([64, GW * 128], F32) for _ in range(2)]
            Ktm = [S.sb([128, GW, 64], F32) for _ in range(2)]
            V = [S.sb([128, GW, 64], BF16) for _ in range(2)]
            dld = [Dep(), Dep()]
            A = [S.sb([128, GW, 64], F32) for _ in range(2)]
            dA = [Dep(), Dep()]
            EK = [S.sb([128, GW, 64], F32) for _ in range(2)]
            dEK = [Dep(), Dep()]
            Kt2 = [S.sb([128, GW, 64], BF16) for _ in range(2)]
            dKt2 = [Dep(), Dep()]
            EQ = [S.sb([64, GW * 128], F32) for _ in range(2)]
            EKT = [S.sb([64, GW * 128], F32) for _ in range(2)]
            dEQ, dEKT = [Dep(), Dep()], [Dep(), Dep()]
            QT2 = [S.sb([64, GW * 128], BF16) for _ in range(2)]
            KT2 = [S.sb([64, GW * 128], BF16) for _ in range(2)]
            dQT2, dKT2 = [Dep(), Dep()], [Dep(), Dep()]
            psA = S.ps([128, GW, 64])
            psG = S.ps([128, GW * 64])
            psGT = [S.ps([64, 4, 128]) for _ in range(2)]
            dpA, dpG, dpGT = Dep(), Dep(), [Dep(), Dep()]
            psS = S.ps([128, 128])
            psH = S.ps([128, 64])
            psU = S.ps([64, 64])
            dpS, dpH, dpU = Dep(), Dep(), Dep()
            Sm = S.sb([128, 128], BF16)
            dSm = Dep()
            tmpS = S.sb([64, 64], F32)
            dtS = Dep()
            gorder = [list(range(17)), [0] + list(range(16, 0, -1))]
            for gi in range(17):
                for d in range(2):
                    c0, gw = groups[gorder[d][gi]]
                    W = gw * 128
                    t0 = c0 * 128
                    arow = 320 + 16 * d
                    S.dma('pool', AT[d][:, 0:W], PFM[arow:arow + 16, t0:t0 + W], writes=[dld[d]])
                    S.dma('sp', QTf[d][:, 0:W], PFM[128:192, t0:t0 + W], writes=[dld[d]])
                    S.dma('sp', KTf[d][:, 0:W], PFM[192:256, t0:t0 + W], writes=[dld[d]])
                    S.dma('sp', Ktm[d][:, 0:gw, :], PTMv[:, c0:c0 + gw, GLK:GLK + 64], writes=[dld[d]])
                    S.dma('pool', V[d][:, 0:gw, :], PTMv[:, c0:c0 + gw, GLV:GLV + 64], writes=[dld[d]])
                    for j in range(gw):
                        S.op('pe', lambda e, d=d, j=j: e.matmul(psA[:, j, :], AT[d][:, j * 128:(j + 1) * 128], W2[:, d, :], start=True, stop=True),
                             reads=[dld[d], dc], writes=[dpA])
                    S.op('dve', lambda e, d=d, gw=gw: e.tensor_tensor(out=A[d][:, 0:gw, :], in0=psA[:, 0:gw, :],
                                                                       in1=B2[:, d, :].unsqueeze(1).to_broadcast([128, gw, 64]), op=ALU.add),
                         reads=[dpA, dc], writes=[dA[d]])
                    S.op('act', lambda e, d=d, gw=gw: e.activation(out=A[d][:, 0:gw, :], in_=A[d][:, 0:gw, :], func=AF.Exp, scale=-1.0), reads=[dA[d]], writes=[dA[d]])
                    S.op('act', lambda e, d=d, gw=gw: e.activation(out=A[d][:, 0:gw, :], in_=A[d][:, 0:gw, :], func=AF.Ln, bias=1.0), reads=[dA[d]], writes=[dA[d]])
                    S.op('pe', lambda e, d=d, gw=gw: e.matmul(psG[:, 0:gw * 64], TR[:, d, :], A[d][:, 0:gw, :].rearrange("p g n -> p (g n)"), start=True, stop=True),
                         reads=[dA[d], dc], writes=[dpG])
                    S.op('act', lambda e, d=d, gw=gw: e.activation(out=EK[d][:, 0:gw, :].rearrange("p g n -> p (g n)"), in_=psG[:, 0:gw * 64], func=AF.Exp, scale=1.0 / 16),
                         reads=[dpG], writes=[dEK[d]])
                    S.op('dve', lambda e, d=d, gw=gw: e.tensor_tensor(out=Kt2[d][:, 0:gw, :], in0=Ktm[d][:, 0:gw, :], in1=EK[d][:, 0:gw, :], op=ALU.mult),
                         reads=[dld[d], dEK[d]], writes=[dKt2[d]])
                    for j in range(gw):
                        S.op('pe', lambda e, d=d, j=j: e.matmul(psGT[j // 4][:, j % 4, :], A[d][:, j, :], TR[:, d, :], start=True, stop=True),
                             reads=[dA[d], dc], writes=[dpGT[j // 4]])
                    for hf in range((gw + 3) // 4):
                        n = min(4, gw - 4 * hf)
                        sl = slice(hf * 512, hf * 512 + n * 128)
                        S.op('act', lambda e, d=d, hf=hf, n=n, sl=sl: e.activation(out=EQ[d][:, sl], in_=psGT[hf][:, 0:n, :].rearrange("p g n -> p (g n)"), func=AF.Exp, scale=-1.0 / 16),
                             reads=[dpGT[hf]], writes=[dEQ[d]])
                        S.op('act', lambda e, d=d, hf=hf, n=n, sl=sl: e.activation(out=EKT[d][:, sl], in_=psGT[hf][:, 0:n, :].rearrange("p g n -> p (g n)"), func=AF.Exp, scale=1.0 / 16),
                             reads=[dpGT[hf]], writes=[dEKT[d]])
                    S.op('dve', lambda e, d=d, W=W: e.scalar_tensor_tensor(out=QT2[d][:, 0:W], in0=QTf[d][:, 0:W], scalar=0.125, in1=EQ[d][:, 0:W], op0=ALU.mult, op1=ALU.mult),
                         reads=[dld[d], dEQ[d]], writes=[dQT2[d]])
                    S.op('pool', lambda e, d=d, W=W: e.tensor_tensor(out=KT2[d][:, 0:W], in0=KTf[d][:, 0:W], in1=EKT[d][:, 0:W], op=ALU.mult),
                         reads=[dld[d], dEKT[d]], writes=[dKT2[d]])
                    corder = list(range(gw)) if d == 0 else list(range(gw - 1, -1, -1))
                    for j in corder:
                        c = c0 + j
                        sl = slice(j * 128, (j + 1) * 128)
                        dcol = j * 128 + (127 if d == 0 else 0)
                        S.op('pe', lambda e, d=d, sl=sl: e.matmul(psS[:], KT2[d][:, sl], QT2[d][:, sl], start=True, stop=True), reads=[dKT2[d], dQT2[d]], writes=[dpS])
                        S.op('dve', lambda e, d=d: e.tensor_tensor(out=Sm[:], in0=psS[:], in1=TR[:, d, :], op=ALU.mult), reads=[dpS, dc], writes=[dSm])
                        S.op('pe', lambda e, d=d, j=j: e.matmul(psH[:], Sm[:], V[d][:, j, :], start=True, stop=False), reads=[dSm, dld[d]], writes=[dpH])
                        S.op('pe', lambda e, d=d, sl=sl: e.matmul(psH[:], QT2[d][:, sl], Sb[d][:], start=False, stop=True), reads=[dQT2[d], dSb[d]], writes=[dpH])
                        S.op('pe', lambda e, d=d, j=j: e.matmul(psU[:], Kt2[d][:, j, :], V[d][:, j, :], start=True, stop=True), reads=[dKt2[d], dld[d]], writes=[dpU])
                        S.op('act', lambda e, d=d, c=c: e.activation(out=Hd[d][:, c, :], in_=psH[:], func=AF.Copy), reads=[dpH], writes=[dH[d]])
                        S.op('dve', lambda e, d=d: e.tensor_tensor(out=tmpS[:], in0=psU[:], in1=St[d][:], op=ALU.add), reads=[dpU, dSt[d]], writes=[dtS])
                        S.op('dve', lambda e, d=d, dcol=dcol: e.tensor_scalar(out=St[d][:], in0=tmpS[:], scalar1=EQ[d][:, dcol:dcol + 1], scalar2=None, op0=ALU.mult),
                             reads=[dtS, dEQ[d]], writes=[dSt[d]])
                        S.op('act', lambda e, d=d, dcol=dcol: e.activation(out=Sb[d][:], in_=tmpS[:], func=AF.Copy, scale=EQ[d][:, dcol:dcol + 1]),
                             reads=[dtS, dEQ[d]], writes=[dSb[d]])
        with Scope(S):
            Rg = S.sb([128, NCH, 64], F32)
            NG = S.sb([128, 64], F32)
            sm = S.sb([128, NCH], F32)
            vr = S.sb([128, NCH], F32)
            dR, dn, dsm, dvr = Dep(), Dep(), Dep(), Dep()
            for j in range(5):
                S.dma('sp', Rg[:, j * 26:(j + 1) * 26, :], PTMv[:, j * 26:(j + 1) * 26, GLR:GLR + 64], writes=[dR])
            S.dma('sp', NG[:], norm_g.partition_broadcast(128), writes=[dn])
            H, H2 = Hd[0], Hd[1]
            S.op('dve', lambda e: e.tensor_tensor(out=H[:], in0=H[:], in1=H2[:], op=ALU.add), reads=[dH[0], dH[1]], writes=[dH[0]])
            _finish_norm(S, H, H2, dH[0], dH[1], sm, vr, dsm, dvr, False)
            S.op('pool', lambda e: e.tensor_tensor(out=H[:], in0=H[:], in1=NG[:].unsqueeze(1).to_broadcast([128, NCH, 64]), op=ALU.mult), reads=[dH[0], dn], writes=[dH[0]])
            S.op('act', lambda e: e.activation(out=Rg[:], in_=Rg[:], func=AF.Silu), reads=[dR], writes=[dR])
            Hout = H if BR.dtype == F32 else S.sb([128, NCH, 64], BR.dtype)
            S.op('dve', lambda e: e.tensor_tensor(out=Hout[:], in0=H[:], in1=Rg[:], op=ALU.mult), reads=[dH[0], dR], writes=[dH[0]])
            for j in range(5):
                S.dma('sp', BR.rearrange("(c p) n -> p c n", p=128)[:, j * 26:(j + 1) * 26, 256:320], Hout[:, j * 26:(j + 1) * 26, :], reads=[dH[0]])


def emit_A5(S, PFM, BR, VD, f64, c256, f1, tw, c2, with_ctx):
    with Scope(S):
        F64 = S.sb([64, 128], F32)
        dc = Dep()
        S.dma('sp', F64[:], f64, writes=[dc])
        Vc = S.sb([128, 2, 128], F32)
        dVc = Dep()
        with Scope(S):
            UT = [S.sb([64, 2048], F32) for _ in range(2)]
            dU = [Dep(), Dep()]
            ps0 = [S.ps([128, 4, 128]) for _ in range(2)]
            dp0 = [Dep(), Dep()]
            Vt = [S.sb([128, 4, 128], F32) for _ in range(2)]
            dVt = [Dep(), Dep()]
            S.dma('sp', UT[0][:, 0:256], PFM[256:320, 0:256], writes=[dU[0]])
            for j in range(2):
                S.op('pe', lambda e, j=j: e.matmul(ps0[0][:, j, :], UT[0][:, j * 128:(j + 1) * 128], F64[:], start=True, stop=True), reads=[dU[0], dc], writes=[dp0[0]])
            S.op('dve', lambda e: e.tensor_copy(out=Vc[:], in_=ps0[0][:, 0:2, :]), reads=[dp0[0]], writes=[dVc])
            n4 = 0
            for g in range(8):
                b = (g + 1) % 2
                t0 = TC + g * 2048
                S.dma('sp', UT[b][:], PFM[256:320, t0:t0 + 2048], writes=[dU[b]])
                for q in range(4):
                    pb = n4 % 2
                    n4 += 1
                    for j in range(4):
                        col = (q * 4 + j) * 128
                        S.op('pe', lambda e, b=b, pb=pb, j=j, col=col: e.matmul(ps0[pb][:, j, :], UT[b][:, col:col + 128], F64[:], start=True, stop=True),
                             reads=[dU[b], dc], writes=[dp0[pb]])
                    if pb == 0:
                        S.op('dve', lambda e, pb=pb: e.tensor_copy(out=Vt[pb][:], in_=ps0[pb][:]), reads=[dp0[pb]], writes=[dVt[pb]])
                    else:
                        S.op('act', lambda e, pb=pb: e.activation(out=Vt[pb][:], in_=ps0[pb][:], func=AF.Copy), reads=[dp0[pb]], writes=[dVt[pb]])
                    r0 = g * 2048 + q * 512
                    S.dma('pool', VD[r0:r0 + 512, :].rearrange("(s p) n -> p s n", p=128), Vt[pb][:], reads=[dVt[pb]])
        if with_ctx:
            with Scope(S):
                CS = S.sb([128, 2, 2, 256], F32)
                dcs = Dep()
                S.dma('sp', CS[:], c256.rearrange("a (bt p) n -> p a bt n", p=128), writes=[dcs])
                psc = S.ps([128, 64])
                dpc = Dep()
                yc = S.sb([128, 64], BR.dtype)
                dyc = Dep()
                for a in range(2):
                    k = 0
                    for bt in range(2):
                        for cs_ in range(2):
                            S.op('pe', lambda e, a=a, bt=bt, cs_=cs_, k=k: e.matmul(psc[:], CS[:, cs_, bt, a * 128:(a + 1) * 128], Vc[:, bt, cs_ * 64:(cs_ + 1) * 64],
                                                                                  start=(k == 0), stop=(k == 3)), reads=[dcs, dVc], writes=[dpc])
                            k += 1
                    S.op('dve', lambda e: e.tensor_copy(out=yc[:], in_=psc[:]), reads=[dpc], writes=[dyc])
                    S.dma('sp', BR[a * 128:(a + 1) * 128, 64:128], yc[:], reads=[dyc])
        with Scope(S):
            X = S.sb([128, 128, 128], F32)
            dX = Dep()
            VDv = VD.rearrange("(t1 t2) n -> t1 t2 n", t2=128)
            for j in range(4):
                S.dma('sp' if j % 2 == 0 else 'pool', X[:, j * 32:(j + 1) * 32, :], VDv[:, j * 32:(j + 1) * 32, :], writes=[dX])
            F1 = S.sb([128, 2, 256], F32)
            TW = S.sb([128, 2, 128], F32)
            C2 = S.sb([128, 2, 128], F32)
            dt = Dep()
            S.dma('sp', F1[:], f1.rearrange("a p n -> p a n"), writes=[dt])
            S.dma('sp', TW[:], tw.rearrange("a p n -> p a n"), writes=[dt])
            S.dma('sp', C2[:], c2.rearrange("a p n -> p a n"), writes=[dt])
            ZP = [S.sb([128, 128, 64], F32) for _ in range(2)]
            dZP = [Dep(), Dep()]
            psZ = [S.ps([128, 256]) for _ in range(2)]
            dpZ = [Dep(), Dep()]
            tmp = [[S.sb([128, 128], F32) for _ in range(4)] for _ in range(2)]
            dtm = [[Dep() for _ in range(4)] for _ in range(2)]
            for c in range(64):
                b = c % 2
                S.op('pe', lambda e, b=b, c=c: e.matmul(psZ[b][:], X[:, :, c], F1[:, 0, :], start=True, stop=False), reads=[dX, dt], writes=[dpZ[b]])
                S.op('pe', lambda e, b=b, c=c: e.matmul(psZ[b][:], X[:, :, 64 + c], F1[:, 1, :], start=False, stop=True), reads=[dX, dt], writes=[dpZ[b]])
                combos = [(0, 0), (1, 1), (0, 1), (1, 0)]
                for k, (zp, tp) in enumerate(combos):
                    S.op('dve', lambda e, b=b, k=k, zp=zp, tp=tp: e.tensor_tensor(out=tmp[b][k][:], in0=psZ[b][:, zp * 128:(zp + 1) * 128], in1=TW[:, tp, :], op=ALU.mult),
                         reads=[dpZ[b], dt], writes=[dtm[b][k]])
                S.op('pool', lambda e, b=b, c=c: e.tensor_tensor(out=ZP[0][:, :, c], in0=tmp[b][0][:], in1=tmp[b][1][:], op=ALU.subtract),
                     reads=[dtm[b][0], dtm[b][1]], writes=[dZP[0]])
                S.op('pool', lambda e, b=b, c=c: e.tensor_tensor(out=ZP[1][:, :, c], in0=tmp[b][2][:], in1=tmp[b][3][:], op=ALU.add),
                     reads=[dtm[b][2], dtm[b][3]], writes=[dZP[1]])
            psY = [S.ps([128, 512]) for _ in range(2)]
            dpY = [Dep(), Dep()]
            Yst = [S.sb([128, 512], BR.dtype) for _ in range(2)]
            dY = [Dep(), Dep()]
            BRv = BR[TC:TA, 64:128].rearrange("(p t1) n -> p t1 n", t1=128)
            for k in range(16):
                b = k % 2
                for part in range(2):
                    S.op('pe', lambda e, b=b, k=k, part=part: e.matmul(psY[b][:], C2[:, part, :], ZP[part][:, 8 * k:8 * k + 8, :].rearrange("p a n -> p (a n)"),
                                                                       start=(part == 0), stop=(part == 1)), reads=[dZP[part], dt], writes=[dpY[b]])
                S.op('act', lambda e, b=b: e.activation(out=Yst[b][:], in_=psY[b][:], func=AF.Copy), reads=[dpY[b]], writes=[dY[b]])
                S.dma('sp', BRv[:, 8 * k:8 * k + 8, :], Yst[b][:].rearrange("p (a n) -> p a n", n=64), reads=[dY[b]])


def fourier_tables():
    f = np.float64
    c = np.arange(64, dtype=f)
    a64 = 2 * np.pi * np.outer(c, c) / 64
    f64 = np.concatenate([np.cos(a64), -np.sin(a64)], axis=1)
    t = np.arange(256, dtype=f)
    a256 = 2 * np.pi * np.outer(t, t) / 256
    c256 = np.stack([np.cos(a256), np.sin(a256)]) / 128.0
    n = np.arange(128, dtype=f)
    a128 = 2 * np.pi * np.outer(n, n) / 128
    C, Sn = np.cos(a128), np.sin(a128)
    f1 = np.stack([np.concatenate([C, -Sn], axis=1), np.concatenate([Sn, C], axis=1)])
    atw = 2 * np.pi * np.outer(n, n) / 16384
    tw = np.stack([np.cos(atw), -np.sin(atw)])
    c2 = np.stack([C, Sn]) / 1024.0
    g = lambda a: np.ascontiguousarray(a.astype(np.float32))
    return dict(f64=g(f64), c256=g(c256), f1=g(f1), tw=g(tw), c2=g(c2))


ALPHA = (2.0 * 2) ** 0.25
DBG = {}


def emit_ada(S, cvec, w, bvec, col0, ncols, out_tile, dout):
    with Scope(S):
        c8 = S.sb([128, 8], F32)
        sc = S.sb([128, 8, 128], F32)
        dc = Dep()
        S.dma('sp', c8[:], cvec.rearrange("(kc p) -> p kc", p=128), writes=[dc], allow_slow_non_contiguous=True)
        S.op('act', lambda e: e.activation(out=c8[:], in_=c8[:], func=AF.Silu), reads=[dc], writes=[dc])
        S.op('dve', lambda e: e.tensor_copy(out=sc[:], in_=c8[:].unsqueeze(2).to_broadcast([128, 8, 128])), reads=[dc], writes=[dc])
        S.dma('sp', out_tile[:, 0:ncols], bvec[col0:col0 + ncols].partition_broadcast(128), writes=[dout])
        wb = [S.sb([128, 8, 512], F32) for _ in range(2)]
        dw = [Dep(), Dep()]
        ps = [S.ps([128, 512]) for _ in range(2)]
        dp = [Dep(), Dep()]
        wv = w.rearrange("(kc p) n -> p kc n", p=128)
        for j in range(ncols // 512):
            b = j % 2
            S.dma('sp', wb[b][:], wv[:, :, col0 + j * 512:col0 + (j + 1) * 512], writes=[dw[b]])
            for kc in range(8):
                S.op('pe', lambda e, b=b, kc=kc: e.matmul(ps[b][:], sc[:, kc, :], wb[b][:, kc, :], start=(kc == 0), stop=(kc == 7)), reads=[dc, dw[b]], writes=[dp[b]])
            S.op('dve', lambda e, b=b, j=j: e.tensor_tensor(out=out_tile[:, j * 512:(j + 1) * 512], in0=ps[b][:], in1=out_tile[:, j * 512:(j + 1) * 512], op=ALU.add),
                 reads=[dp[b], dout], writes=[dout])


class LNK:
    def __init__(self, S):
        self.S = S
        self.st = S.sb([128, 2, 6], F32)
        self.mv = S.sb([128, 2], F32)
        self.rs = S.sb([128, 1], F32)
        self.d = Dep()

    def norm(self, out, dout, x, dx, A, B, dAB, tmp, dtmp):
        S = self.S
        st, mv, rs, d = self.st, self.mv, self.rs, self.d
        for j in range(2):
            S.op('dve', lambda e, j=j: e.bn_stats(out=st[:, j, :], in_=x[:, j * 512:(j + 1) * 512]), reads=[dx], writes=[d])
        S.op('dve', lambda e: e.bn_aggr(out=mv[:], in_=st[:]), reads=[d], writes=[d])
        S.op('act', lambda e: e.activation(out=rs[:], in_=mv[:, 1:2], func=AF.Sqrt, bias=LN_EPS), reads=[d], writes=[d])
        S.op('dve', lambda e: e.reciprocal(out=rs[:], in_=rs[:]), reads=[d], writes=[d])
        S.op('dve', lambda e: e.tensor_scalar(out=tmp[:], in0=x[:], scalar1=mv[:, 0:1], scalar2=rs[:, 0:1], op0=ALU.subtract, op1=ALU.mult),
             reads=[dx, d], writes=[dtmp])
        S.op('pool', lambda e: e.tensor_tensor(out=tmp[:], in0=tmp[:], in1=A, op=ALU.mult), reads=[dtmp, dAB], writes=[dtmp])
        S.op('dve', lambda e: e.tensor_tensor(out=out, in0=tmp[:], in1=B, op=ALU.add), reads=[dtmp, dAB], writes=[dout])


def emit_T(S, src, dsrc, dstT, ddst, ident, dident, ps, dps, dt_copy_eng='act'):
    for kc in range(8):
        S.op('pe', lambda e, kc=kc: e.transpose(ps[:, kc, :], src[:, kc * 128:(kc + 1) * 128], ident), reads=[dsrc, dident], writes=[dps])
    S.op(dt_copy_eng, (lambda e: e.activation(out=dstT, in_=ps[:], func=AF.Copy)) if dt_copy_eng == 'act' else (lambda e: e.tensor_copy(out=dstT, in_=ps[:])),
         reads=[dps], writes=[ddst])


def emit_PRE(S, xin, ntok, nctx, c_b, c_ctx, w_ada, b_ada, ident, HTN):
    with Scope(S):
        AD = [S.sb([128, 2048], F32) for _ in range(2)]
        dAD = [Dep(), Dep()]
        emit_ada(S, c_b, w_ada, b_ada, 0, 2048, AD[0], dAD[0])
        if nctx:
            emit_ada(S, c_ctx, w_ada, b_ada, 0, 2048, AD[1], dAD[1])
        for v in range(2 if nctx else 1):
            S.op('dve', lambda e, v=v: e.tensor_scalar(out=AD[v][:, 1024:2048], in0=AD[v][:, 1024:2048], scalar1=1.0, scalar2=None, op0=ALU.add), reads=[dAD[v]], writes=[dAD[v]])
        _lnmodT_loop(S, xin, ntok, nctx, AD, dAD, ident, HTN)


def _lnmodT_loop(S, xin, ntok, nctx, AD, dAD, ident, HTN):
    identb = S.sb([128, 128], BF16)
    did = Dep()
    S.dma('pool', identb[:], ident, writes=[did])
    ln = LNK(S)
    xt = [S.sb([128, 1024], F32) for _ in range(2)]
    dxt = [Dep(), Dep()]
    tmp = S.sb([128, 1024], F32)
    dtmp = Dep()
    hb = [S.sb([128, 1024], BF16) for _ in range(2)]
    dhb = [Dep(), Dep()]
    psT = [S.ps([128, 8, 128], BF16) for _ in range(2)]
    dpsT = [Dep(), Dep()]
    hT = [S.sb([128, 8, 128], BF16) for _ in range(2)]
    dhT = [Dep(), Dep()]
    HTv = HTN.rearrange("(kc p) t -> p kc t", p=128)
    for t in range(ntok // 128):
        b = t % 2
        v = 1 if t * 128 < nctx else 0
        S.dma('sp', xt[b][:], xin[t * 128:(t + 1) * 128, :], writes=[dxt[b]])
        ln.norm(hb[b][:], dhb[b], xt[b], dxt[b], AD[v][:, 1024:2048], AD[v][:, 0:1024], dAD[v], tmp, dtmp)
        emit_T(S, hb[b], dhb[b], hT[b][:], dhT[b], identb[:], did, psT[b], dpsT[b])
        S.dma('pool', HTv[:, :, t * 128:(t + 1) * 128], hT[b][:], reads=[dhT[b]])


def emit_B(S, last, moe, ntok, nctx, xin, hT, brT, c_b, c_ctx, w_ada, b_ada, w_ada_n, b_ada_n,
           w_gate, b_gate, w_branch, w_out, ln1g, ln1b, ln2g, ln2b, wg, wu, wd, wr, brr, ident,
           XMID, H2T, DENSE, XOUT, HTN):
    NE = 8 if moe else 2
    nv = 2 if nctx else 1
    cv = [c_b, c_ctx]
    with Scope(S):
        AD = [S.sb([128, 4096], F32) for _ in range(nv)]
        dAD = [Dep() for _ in range(nv)]
        for v in range(nv):
            emit_ada(S, cv[v], w_ada, b_ada, 2048, 4096, AD[v], dAD[v])
            S.op('dve', lambda e, v=v: e.tensor_scalar(out=AD[v][:, 2048:3072], in0=AD[v][:, 2048:3072], scalar1=1.0, scalar2=None, op0=ALU.add), reads=[dAD[v]], writes=[dAD[v]])
        identf = S.sb([128, 128], F32)
        did = Dep()
        S.dma('sp', identf[:], ident, writes=[did])
        with Scope(S):
            Wg = S.sb([128, 8, 4096], BF16)
            Wb = S.sb([128, 10, 1024], BF16)
            Wo = S.sb([128, 8, 1024], BF16)
            bg = S.sb([128, 32], F32)
            L1 = S.sb([128, 2, 1024], F32)
            dW = Dep()
            wgv = w_gate.rearrange("(kc p) n -> p kc n", p=128)
            for j in range(4):
                S.dma('pool', Wg[:, :, j * 1024:(j + 1) * 1024], wgv[:, :, j * 1024:(j + 1) * 1024], writes=[dW])
            S.dma('pool', Wb[:], w_branch.rearrange("(kc p) n -> p kc n", p=128), writes=[dW])
            S.dma('pool', Wo[:], w_out.rearrange("(kc p) n -> p kc n", p=128), writes=[dW])
            S.dma('sp', bg[:], b_gate, writes=[dW])
            S.dma('sp', L1[:, 0, :], ln1g.partition_broadcast(128), writes=[dW])
            S.dma('sp', L1[:, 1, :], ln1b.partition_broadcast(128), writes=[dW])
            if moe:
                WR = S.sb([128, 8, 8], F32)
                BRR = S.sb([128, 8], F32)
                S.dma('sp', WR[:], wr.rearrange("(kc p) n -> p kc n", p=128), writes=[dW])
                S.dma('sp', BRR[:], brr.partition_broadcast(128), writes=[dW])
            if isinstance(brT, tuple) and len(brT) > 3:
                BRG_fn0, selI0, cdt0, BRT0 = brT
                with Scope(S):
                    SelI0 = S.sb([128, 4, 128], BF16)
                    idb0 = S.sb([128, 128], BF16)
                    dsel0 = Dep()
                    S.dma('sp', SelI0[:], selI0.rearrange("r p n -> p r n"), writes=[dsel0]) if False else S.dma('pool', SelI0[:], selI0.rearrange("r p n -> p r n"), writes=[dsel0])
                    S.dma('pool', idb0[:], ident, writes=[dsel0])
                    cand0 = [[S.sb([128, 4, 320], cdt0) for _ in range(4)] for _ in range(2)]
                    dcand0 = [[Dep() for _ in range(4)] for _ in range(2)]
                    brtok0 = [[S.sb([128, 1280], BF16) for _ in range(4)] for _ in range(2)]
                    dbrtok0 = [[Dep() for _ in range(4)] for _ in range(2)]
                    psB0 = [[S.ps([128, 4, 128]) for _ in range(3)] for _ in range(2)]
                    dpsB0 = [[Dep() for _ in range(3)] for _ in range(2)]
                    bst0 = [S.sb([128, 10, 128], BF16) for _ in range(2)]
                    dbst0 = [Dep(), Dep()]
                    BRTv0 = BRT0.rearrange("(kc p) t -> p kc t", p=128)
                    segs0 = [(0, 0, 64), (256, 64, 64), (512, 128, 128), (1024, 256, 64)]
                    nops = 0
                    for ti0 in range(ntok // 128):
                        tt = ti0 * 128
                        tb = ti0 % 2
                        isctx = tt < nctx
                        ncand = 1 if isctx else 4
                        for r in range(ncand):
                            row = tt if isctx else TC + r * 4096 + (tt - nctx)
                            S.dma('sp', cand0[tb][r][:], BRG_fn0(row), writes=[dcand0[tb][r]])
                            for si, (fo, co, w) in enumerate(segs0):
                                eng = ('act', 'pool', 'dve')[nops % 3]
                                nops += 1
                                dst = brtok0[tb][r][:, fo:fo + 4 * w].rearrange("p (i w) -> p i w", i=4)
                                src = cand0[tb][r][:, :, co:co + w]
                                if eng == 'act':
                                    S.op('act', lambda e, dst=dst, src=src: e.activation(out=dst, in_=src, func=AF.Copy), reads=[dcand0[tb][r]], writes=[dbrtok0[tb][r]])
                                else:
                                    S.op(eng, lambda e, dst=dst, src=src: e.tensor_copy(out=dst, in_=src), reads=[dcand0[tb][r]], writes=[dbrtok0[tb][r]])
                        for kg in range(3):
                            kbs = list(range(kg * 4, min(10, kg * 4 + 4)))
                            for kb in kbs:
                                for r in range(ncand):
                                    rhs = idb0[:] if isctx else SelI0[:, r, :]
                                    S.op('pe', lambda e, kb=kb, r=r, kg=kg, ncand=ncand, rhs=rhs, tb=tb: e.matmul(
                                        psB0[tb][kg][:, kb - kg * 4, :], brtok0[tb][r][:, kb * 128:(kb + 1) * 128], rhs,
                                        start=(r == 0), stop=(r == ncand - 1)), reads=[dbrtok0[tb][r], dsel0], writes=[dpsB0[tb][kg]])
                            if kg == 1:
                                S.op('act', lambda e, kg=kg, kbs=kbs, tb=tb: e.activation(out=bst0[tb][:, kbs[0]:kbs[-1] + 1, :], in_=psB0[tb][kg][:, 0:len(kbs), :], func=AF.Copy),
                                     reads=[dpsB0[tb][kg]], writes=[dbst0[tb]])
                            else:
                                S.op('dve', lambda e, kg=kg, kbs=kbs, tb=tb: e.tensor_copy(out=bst0[tb][:, kbs[0]:kbs[-1] + 1, :], in_=psB0[tb][kg][:, 0:len(kbs), :]),
                                     reads=[dpsB0[tb][kg]], writes=[dbst0[tb]])
                        S.dma('sp', BRTv0[:, :, tt:tt + 128], bst0[tb][:], reads=[dbst0[tb]])
                brT = BRT0
            hs = S.sb([128, 8, 512], BF16)
            bs = S.sb([128, 10, 512], BF16)
            dhs, dbs = Dep(), Dep()
            dhs2, dbs2 = Dep(), Dep()
            fused_br = isinstance(brT, tuple)
            if fused_br:
                BRG, selI = brT[0], brT[1]
                SelI = S.sb([128, 4, 128], BF16)
                dsel = Dep()
                S.dma('pool', SelI[:], selI.rearrange("r p n -> p r n"), writes=[dsel])
                cand = S.sb([128, 4, 320], brT[2] if len(brT) > 2 else F32)
                dcand = Dep()
                brtok = [S.sb([128, 1280], BF16) for _ in range(4)]
                dbrtok = [Dep() for _ in range(4)]
                psB = S.ps([128, 4, 128])
                dpsB = Dep()
                BRG_fn = BRG if callable(BRG) else (lambda row, v=BRG.rearrange("i t n -> t i n"): v[row:row + 128, :, :])
            assert not fused_br
            mixT = [S.sb([128, 8, 512], BF16) for _ in range(2)]
            dmixT = [Dep(), Dep()]
            gate = [S.sb([128, 512], F32) for _ in range(2)]
            dgate = [Dep(), Dep()]
            mix = S.sb([128, 512], F32)
            dmix = Dep()
            tm = [S.sb([128, 512], F32) for _ in range(2)]
            dtm = [Dep(), Dep()]
            psg = [S.ps([128, 512]) for _ in range(2)]
            psp = [S.ps([128, 512]) for _ in range(2)]
            dpsg, dpsp = [Dep(), Dep()], [Dep(), Dep()]
            psy = [S.ps([128, 512]) for _ in range(2)]
            dpsy = [Dep(), Dep()]
            psT1 = S.ps([128, 4, 128])
            dps1 = Dep()
            psl = S.ps([128, 8])
            dpsl = Dep()
            xt = [S.sb([128, 1024], F32) for _ in range(2)]
            tmp = [S.sb([128, 1024], F32) for _ in range(2)]
            dxt, dtmp = [Dep(), Dep()], [Dep(), Dep()]
            xm = S.sb([128, 1024], F32)
            dxm = Dep()
            h2T = S.sb([128, 8, 128], BF16)
            h2Tf = S.sb([128, 8, 128], F32) if moe else None
            dh2T, dh2Tf = Dep(), Dep()
            lnk = [LNK(S), LNK(S)]
            rt = [S.sb([128, 8], F32) for _ in range(4)]
            r1 = [S.sb([128, 1], F32) for _ in range(4)]
            drt = Dep()
            hTv = hT.rearrange("(kc p) t -> p kc t", p=128)
            bTv = brT.rearrange("(kc p) t -> p kc t", p=128)
            H2Tv = H2T.rearrange("(kc p) t -> p kc t", p=128)
            kbr = [(0, 2), (2, 4), (4, 8), (8, 10)]
            ng = 0
            ecnt = [0]

            def ln_a(L, x, dx):
                for j in range(2):
                    S.op('dve', lambda e, j=j: e.bn_stats(out=L.st[:, j, :], in_=x[:, j * 512:(j + 1) * 512]), reads=[dx], writes=[L.d])
                S.op('dve', lambda e: e.bn_aggr(out=L.mv[:], in_=L.st[:]), reads=[L.d], writes=[L.d])
                S.op('act', lambda e: e.activation(out=L.rs[:], in_=L.mv[:, 1:2], func=AF.Sqrt, bias=LN_EPS), reads=[L.d], writes=[L.d])

            lastL = [None]

            def ln_b(L, x, dx, A, dAB, T, dT):
                S.op('dve', lambda e: e.reciprocal(out=L.rs[:], in_=L.rs[:]), reads=[L.d], writes=[L.d])
                S.op('dve', lambda e: e.scalar_tensor_tensor(out=T[:], in0=x[:], scalar=L.mv[:, 0:1], in1=A, op0=ALU.subtract, op1=ALU.mult),
                     reads=[dx, L.d, dAB], writes=[dT])
                lastL[0] = L

            def ln_c(out, dout, T, dT, B, dAB, L):
                S.op('dve', lambda e: e.scalar_tensor_tensor(out=out, in0=T[:], scalar=L.rs[:, 0:1], in1=B, op0=ALU.mult, op1=ALU.add),
                     reads=[dT, L.d, dAB], writes=[dout])

            def subtile_steps(c, t0, sub, v, mb):
                X, dX, T, dT, L = xt[c], dxt[c], tmp[c], dtmp[c], lnk[c]
                r0 = t0 + sub * 128
                S.dma('sp', X[:], xin[r0:r0 + 128, :], writes=[dX])
                for half in range(2):
                    hsl = slice(half * 512, (half + 1) * 512)
                    for fc in range(8):
                        S.op('pe', lambda e, fc=fc, hsl=hsl, half=half: e.matmul(psy[half][:], mixT[mb][:, fc, sub * 128:(sub + 1) * 128], Wo[:, fc, hsl],
                                                                              start=(fc == 0), stop=(fc == 7)),
                             reads=[dmixT[mb], dW], writes=[dpsy[half]])
                yield
                for half in range(2):
                    hsl = slice(half * 512, (half + 1) * 512)
                    S.op('dve', lambda e, hsl=hsl, half=half: e.tensor_tensor(out=T[:, hsl], in0=psy[half][:], in1=AD[v][:, hsl], op=ALU.mult),
                         reads=[dpsy[half], dAD[v]], writes=[dT])
                S.op('dve', lambda e: e.scalar_tensor_tensor(out=X[:], in0=X[:], scalar=ALPHA, in1=T[:], op0=ALU.mult, op1=ALU.add), reads=[dX, dT], writes=[dX])
                ln_a1(L, X, dX)
                yield
                yield
                ln_a2(L)
                yield
                ln_b(L, X, dX, L1[:, 0, :], dW, T, dT)
                yield
                ln_c(xm[:], dxm, T, dT, L1[:, 1, :], dW, L)
                S.dma('sp', XMID[r0:r0 + 128, :], xm[:], reads=[dxm])
                ln_a1(L, xm, dxm)
                yield
                yield
                ln_a2(L)
                yield
                ln_b(L, xm, dxm, AD[v][:, 2048:3072], dAD[v], T, dT)
                yield
                ln_c(X[:], dX, T, dT, AD[v][:, 1024:2048], dAD[v], L)
                yield
                for q in range(2):
                    for kc in range(q * 4, q * 4 + 4):
                        S.op('pe', lambda e, kc=kc: e.transpose(psT1[:, kc % 4, :], X[:, kc * 128:(kc + 1) * 128], identf[:]), reads=[dX, did], writes=[dps1])
                    if moe:
                        S.op('dve', lambda e, q=q: e.tensor_copy(out=h2Tf[:, q * 4:(q + 1) * 4, :], in_=psT1[:]), reads=[dps1], writes=[dh2Tf])
                    else:
                        S.op('dve', lambda e, q=q: e.tensor_copy(out=h2T[:, q * 4:(q + 1) * 4, :], in_=psT1[:]), reads=[dps1], writes=[dh2T])
                    yield
                if not moe:
                    S.dma('pool', H2Tv[:, :, r0:r0 + 128], h2T[:], reads=[dh2T])
                    for _ in range(4):
                        yield
                    return
                for kc in range(8):
                    S.op('pe', lambda e, kc=kc: e.matmul(psl[:], h2Tf[:, kc, :], WR[:, kc, :], start=(kc == 0), stop=(kc == 7)), reads=[dh2Tf, dW], writes=[dpsl])
                S.op('act', lambda e: e.activation(out=h2T[:], in_=h2Tf[:], func=AF.Copy), reads=[dh2Tf], writes=[dh2T])
                S.dma('pool', H2Tv[:, :, r0:r0 + 128], h2T[:], reads=[dh2T])
                yield
                lg, sel, ex, dn = rt
                m1, m2, lm, ssum = r1
                S.op('dve', lambda e: e.tensor_copy(out=lg[:], in_=psl[:]), reads=[dpsl], writes=[drt])
                S.op('dve', lambda e: e.tensor_tensor(out=sel[:], in0=lg[:], in1=BRR[:], op=ALU.add), reads=[drt, dW], writes=[drt])
                S.op('dve', lambda e: e.tensor_reduce(out=m1[:], in_=sel[:], axis=AX.X, op=ALU.max), reads=[drt], writes=[drt])
                S.op('dve', lambda e: e.tensor_scalar(out=ex[:], in0=sel[:], scalar1=m1[:, 0:1], scalar2=-1e30, op0=ALU.is_ge, op1=ALU.mult), reads=[drt], writes=[drt])
                S.op('dve', lambda e: e.tensor_tensor(out=ex[:], in0=ex[:], in1=sel[:], op=ALU.add), reads=[drt], writes=[drt])
                S.op('dve', lambda e: e.tensor_reduce(out=m2[:], in_=ex[:], axis=AX.X, op=ALU.max), reads=[drt], writes=[drt])
                S.op('dve', lambda e: e.tensor_scalar(out=sel[:], in0=sel[:], scalar1=m2[:, 0:1], scalar2=None, op0=ALU.is_ge), reads=[drt], writes=[drt])
                S.op('dve', lambda e: e.tensor_reduce(out=lm[:], in_=lg[:], axis=AX.X, op=ALU.max), reads=[drt], writes=[drt])
                S.op('dve', lambda e: e.tensor_scalar(out=lm[:], in0=lm[:], scalar1=-1.0, scalar2=None, op0=ALU.mult), reads=[drt], writes=[drt])
                yield
                S.op('act', lambda e: e.activation(out=ex[:], in_=lg[:], func=AF.Exp, bias=lm[:, 0:1]), reads=[drt], writes=[drt])
                yield
                S.op('dve', lambda e: e.tensor_tensor(out=ex[:], in0=ex[:], in1=sel[:], op=ALU.mult), reads=[drt], writes=[drt])
                S.op('dve', lambda e: e.tensor_reduce(out=ssum[:], in_=ex[:], axis=AX.X, op=ALU.add), reads=[drt], writes=[drt])
                S.op('dve', lambda e: e.reciprocal(out=ssum[:], in_=ssum[:]), reads=[drt], writes=[drt])
                S.op('dve', lambda e: e.tensor_scalar(out=dn[:], in0=ex[:], scalar1=ssum[:, 0:1], scalar2=None, op0=ALU.mult), reads=[drt], writes=[drt])
                S.dma('sp', DENSE[r0:r0 + 128, :], dn[:], reads=[drt])
                yield

            def ln_a1(L, x, dx):
                for j in range(2):
                    S.op('dve', lambda e, j=j: e.bn_stats(out=L.st[:, j, :], in_=x[:, j * 512:(j + 1) * 512]), reads=[dx], writes=[L.d])
                S.op('dve', lambda e: e.bn_aggr(out=L.mv[:], in_=L.st[:]), reads=[L.d], writes=[L.d])

            def ln_a2(L):
                S.op('act', lambda e: e.activation(out=L.rs[:], in_=L.mv[:, 1:2], func=AF.Sqrt, bias=LN_EPS), reads=[L.d], writes=[L.d])

            jobs = [[], []]
            npend = [0, 0]
            quota = [0, 0]

            pos = [None, None]

            def chain(c):
                o = 1 - c
                while True:
                    while not jobs[c] or not (pos[o] is None or pos[o] in (8, 9)):
                        yield
                    job = jobs[c].pop(0)
                    pos[c] = 0
                    for _ in subtile_steps(c, *job):
                        pos[c] += 1
                        yield
                    pos[c] = None
                    npend[c] -= 1

            chains = [chain(0), chain(1)]
            slot = [0]

            def step():
                next(chains[slot[0] % 2])
                slot[0] += 1

            stiles = ([(0, nctx)] if nctx else []) + [(nctx + 512 * j_, 512) for j_ in range((ntok - nctx) // 512)]
            for si, (t0, W) in enumerate(stiles):
                v = 1 if t0 < nctx else 0
                mb = si % 2
                S.dma('sp', hs[:, 0:4, 0:W], hTv[:, 0:4, t0:t0 + W], writes=[dhs])
                S.dma('act', hs[:, 4:8, 0:W], hTv[:, 4:8, t0:t0 + W], writes=[dhs2])
                S.dma('sp', bs[:, 0:5, 0:W], bTv[:, 0:5, t0:t0 + W], writes=[dbs])
                S.dma('act', bs[:, 5:10, 0:W], bTv[:, 5:10, t0:t0 + W], writes=[dbs2])
                for _ in range(4):
                    step()
                while npend[0] > quota[0] or npend[1] > quota[1]:
                    step()
                for fc in range(8):
                    for j in range(4):
                        pb = ng % 2
                        ng += 1
                        for kc in range(8):
                            S.op('pe', lambda e, pb=pb, kc=kc, j=j, fc=fc, W=W: e.matmul(psg[pb][:, 0:W], Wg[:, kc, j * 1024 + fc * 128:j * 1024 + fc * 128 + 128], hs[:, kc, 0:W],
                                                                                  start=(kc == 0), stop=(kc == 7)), reads=[dW, dhs if kc < 4 else dhs2], writes=[dpsg[pb]])
                        S.op('act', lambda e, pb=pb, j=j, fc=fc, W=W: e.activation(out=gate[pb][:, 0:W], in_=psg[pb][:, 0:W], func=AF.Sigmoid, bias=bg[:, j * 8 + fc:j * 8 + fc + 1]),
                             reads=[dpsg[pb], dW], writes=[dgate[pb]])
                        step()
                        k0, k1 = kbr[j]
                        for kb in range(k0, k1):
                            S.op('pe', lambda e, pb=pb, kb=kb, fc=fc, k0=k0, k1=k1, W=W: e.matmul(psp[pb][:, 0:W], Wb[:, kb, fc * 128:(fc + 1) * 128], bs[:, kb, 0:W],
                                                                                          start=(kb == k0), stop=(kb == k1 - 1)), reads=[dW, dbs if kb < 5 else dbs2], writes=[dpsp[pb]])
                        if j == 0:
                            S.op('dve', lambda e, pb=pb, W=W: e.tensor_tensor(out=mix[:, 0:W], in0=psp[pb][:, 0:W], in1=gate[pb][:, 0:W], op=ALU.mult), reads=[dpsp[pb], dgate[pb]], writes=[dmix])
                        else:
                            S.op('dve', lambda e, pb=pb, W=W: e.tensor_tensor(out=tm[pb][:, 0:W], in0=psp[pb][:, 0:W], in1=gate[pb][:, 0:W], op=ALU.mult), reads=[dpsp[pb], dgate[pb]], writes=[dtm[pb]])
                            if j < 3:
                                S.op('pool', lambda e, pb=pb, W=W: e.tensor_tensor(out=mix[:, 0:W], in0=mix[:, 0:W], in1=tm[pb][:, 0:W], op=ALU.add), reads=[dtm[pb], dmix], writes=[dmix])
                            else:
                                S.op('pool', lambda e, pb=pb, fc=fc, W=W, mb=mb: e.tensor_tensor(out=mixT[mb][:, fc, 0:W], in0=mix[:, 0:W], in1=tm[pb][:, 0:W], op=ALU.add),
                                     reads=[dtm[pb], dmix], writes=[dmixT[mb]])
                        step()
                quota[0] = quota[1] = 0
                for sub in range(W // 128):
                    jobs[sub % 2].append((t0, sub, v, mb))
                    npend[sub % 2] += 1
                    quota[sub % 2] += 1
            while npend[0] or npend[1]:
                step()
        if DBG.get('p1only'):
            return
        with Scope(S):
            L2 = S.sb([128, 2, 1024], F32)
            dL2 = Dep()
            S.dma('sp', L2[:, 0, :], ln2g.partition_broadcast(128), writes=[dL2])
            S.dma('sp', L2[:, 1, :], ln2b.partition_broadcast(128), writes=[dL2])
            if not last:
                ADN = [S.sb([128, 2048], F32) for _ in range(nv)]
                dADN = [Dep() for _ in range(nv)]
                for v in range(nv):
                    emit_ada(S, cv[v], w_ada_n, b_ada_n, 0, 2048, ADN[v], dADN[v])
                    S.op('dve', lambda e, v=v: e.tensor_scalar(out=ADN[v][:, 1024:2048], in0=ADN[v][:, 1024:2048], scalar1=1.0, scalar2=None, op0=ALU.add), reads=[dADN[v]], writes=[dADN[v]])
                identb = S.sb([128, 128], BF16)
                dib = Dep()
                S.dma('pool', identb[:], ident, writes=[dib])
                hb = S.sb([128, 1024], BF16)
                dhb = Dep()
                psTb = S.ps([128, 8, 128], BF16)
                dpsTb = Dep()
                hTn = S.sb([128, 8, 128], BF16)
                dhTn = Dep()
                HTNv = HTN.rearrange("(kc p) t -> p kc t", p=128)
            WG = [S.sb([128, 8, 768], BF16) for _ in range(2)]
            WU = [S.sb([128, 8, 768], BF16) for _ in range(2)]
            WD = [S.sb([128, 6, 1024], BF16) for _ in range(2)]
            dWf = [Dep(), Dep()]
            hg = S.sb([128, 8, 1024], BF16)
            dhg = Dep()
            facc = S.sb([128, 8, 1024], F32)
            dfacc = Dep()
            dns = S.sb([128, 8, 8], F32)
            ddns = Dep()
            actT = S.sb([128, 6, 512], BF16)
            dactT = Dep()
            sg = [S.sb([128, 512], F32) for _ in range(2)]
            dsg = [Dep(), Dep()]
            psG = [S.ps([128, 512]) for _ in range(2)]
            psU = [S.ps([128, 512]) for _ in range(2)]
            dpG, dpU = [Dep(), Dep()], [Dep(), Dep()]
            psF = [S.ps([128, 512]) for _ in range(2)]
            dpF = [Dep(), Dep()]
            xm_2 = S.sb([128, 1024], F32)
            z_2 = S.sb([128, 1024], F32)
            tmp_2 = S.sb([128, 1024], F32)
            xn_2 = S.sb([128, 1024], F32)
            dxm, dz, dtmp, dxn = Dep(), Dep(), Dep(), Dep()
            ln = LNK(S)
            H2Tv = H2T.rearrange("(kc p) t -> p kc t", p=128)
            ngrp = (ntok + 1023) // 1024
            nf = 0
            pieces = [(e_, fc0, nfc) for e_ in range(NE) for (fc0, nfc) in ((0, 6), (6, 5))]
            seq = [(g, pi) for g in range(ngrp) for pi in range(len(pieces))]

            def load_piece(k):
                e_, fc0, nfc = pieces[seq[k][1]]
                b = k % 2
                cs = slice(fc0 * 128, (fc0 + nfc) * 128)
                S.dma('pool', WG[b][:, :, 0:nfc * 128], wg[e_][:, cs].rearrange("(kc p) n -> p kc n", p=128), writes=[dWf[b]])
                S.dma('pool', WU[b][:, :, 0:nfc * 128], wu[e_][:, cs].rearrange("(kc p) n -> p kc n", p=128), writes=[dWf[b]])
                S.dma('pool', WD[b][:, 0:nfc, :], wd[e_][cs, :].rearrange("(kc p) n -> p kc n", p=128), writes=[dWf[b]])

            load_piece(0)
            for k, (g, pi) in enumerate(seq):
                g0 = g * 1024
                gwid = min(1024, ntok - g0)
                nt = gwid // 128
                ex_, fc0, nfc = pieces[pi]
                wb = k % 2
                if pi == 0 and g == 0:
                    S.dma('sp', hg[:, :, 0:gwid], H2Tv[:, :, g0:g0 + gwid], writes=[dhg])
                    if moe:
                        S.dma('sp', dns[:, 0:nt, :], DENSE[g0:g0 + gwid, :].rearrange("(t p) n -> p t n", p=128), writes=[ddns])
                if k + 1 < len(seq):
                    load_piece(k + 1)
                for c0 in range(0, gwid, 512):
                    cw = min(512, gwid - c0)
                    for fc in range(nfc):
                        pb = nf % 2
                        nf += 1
                        for kc in range(8):
                            S.op('pe', lambda e, pb=pb, kc=kc, fc=fc, c0=c0, cw=cw, wb=wb: e.matmul(psG[pb][:, 0:cw], WG[wb][:, kc, fc * 128:(fc + 1) * 128], hg[:, kc, c0:c0 + cw],
                                                                                                start=(kc == 0), stop=(kc == 7)), reads=[dWf[wb], dhg], writes=[dpG[pb]])
                        for kc in range(8):
                            S.op('pe', lambda e, pb=pb, kc=kc, fc=fc, c0=c0, cw=cw, wb=wb: e.matmul(psU[pb][:, 0:cw], WU[wb][:, kc, fc * 128:(fc + 1) * 128], hg[:, kc, c0:c0 + cw],
                                                                                                start=(kc == 0), stop=(kc == 7)), reads=[dWf[wb], dhg], writes=[dpU[pb]])
                        S.op('act', lambda e, pb=pb, cw=cw: e.activation(out=sg[pb][:, 0:cw], in_=psG[pb][:, 0:cw], func=AF.Silu), reads=[dpG[pb]], writes=[dsg[pb]])
                        S.op('dve', lambda e, pb=pb, cw=cw, fc=fc: e.tensor_tensor(out=actT[:, fc, 0:cw], in0=psU[pb][:, 0:cw], in1=sg[pb][:, 0:cw], op=ALU.mult),
                             reads=[dpU[pb], dsg[pb]], writes=[dactT])
                    for sub in range(cw // 128):
                        ti = (c0 + sub * 128) // 128
                        for half in range(2):
                            pf = (sub * 2 + half) % 2
                            hsl = slice(half * 512, (half + 1) * 512)
                            for fc in range(nfc):
                                S.op('pe', lambda e, pf=pf, fc=fc, sub=sub, hsl=hsl, wb=wb, nfc=nfc: e.matmul(psF[pf][:], actT[:, fc, sub * 128:(sub + 1) * 128], WD[wb][:, fc, hsl],
                                                                                                            start=(fc == 0), stop=(fc == nfc - 1)), reads=[dactT, dWf[wb]], writes=[dpF[pf]])
                            if moe:
                                if pi == 0:
                                    S.op('dve', lambda e, pf=pf, ti=ti, hsl=hsl, ex_=ex_: e.tensor_scalar(out=facc[:, ti, hsl], in0=psF[pf][:], scalar1=dns[:, ti, ex_:ex_ + 1], scalar2=None, op0=ALU.mult),
                                         reads=[dpF[pf], ddns], writes=[dfacc])
                                else:
                                    S.op('dve', lambda e, pf=pf, ti=ti, hsl=hsl, ex_=ex_: e.scalar_tensor_tensor(out=facc[:, ti, hsl], in0=psF[pf][:], scalar=dns[:, ti, ex_:ex_ + 1], in1=facc[:, ti, hsl],
                                                                                                              op0=ALU.mult, op1=ALU.add), reads=[dpF[pf], ddns, dfacc], writes=[dfacc])
                            else:
                                if pi == 0:
                                    S.op('act', lambda e, pf=pf, ti=ti, hsl=hsl: e.activation(out=facc[:, ti, hsl], in_=psF[pf][:], func=AF.Copy), reads=[dpF[pf]], writes=[dfacc])
                                else:
                                    S.op('dve', lambda e, pf=pf, ti=ti, hsl=hsl: e.tensor_tensor(out=facc[:, ti, hsl], in0=psF[pf][:], in1=facc[:, ti, hsl], op=ALU.add),
                                         reads=[dpF[pf], dfacc], writes=[dfacc])
                if pi != len(pieces) - 1:
                    continue
                if g + 1 < ngrp:
                    g0n = (g + 1) * 1024
                    gwn = min(1024, ntok - g0n)
                    S.dma('sp', hg[:, :, 0:gwn], H2Tv[:, :, g0n:g0n + gwn], writes=[dhg])
                    if moe:
                        S.dma('sp', dns[:, 0:gwn // 128, :], DENSE[g0n:g0n + gwn, :].rearrange("(t p) n -> p t n", p=128), writes=[ddns])
                for ti in range(nt):
                    r0 = g0 + ti * 128
                    v = 1 if r0 < nctx else 0
                    S.dma('sp', xm_2[:], XMID[r0:r0 + 128, :], writes=[dxm])
                    S.op('dve', lambda e, ti=ti, v=v: e.tensor_tensor(out=tmp_2[:], in0=facc[:, ti, :], in1=AD[v][:, 3072:4096], op=ALU.mult), reads=[dfacc, dAD[v]], writes=[dtmp])
                    S.op('dve', lambda e: e.scalar_tensor_tensor(out=z_2[:], in0=xm_2[:], scalar=ALPHA, in1=tmp_2[:], op0=ALU.mult, op1=ALU.add), reads=[dxm, dtmp], writes=[dz])
                    ln.norm(xn_2[:], dxn, z_2, dz, L2[:, 0, :], L2[:, 1, :], dL2, tmp_2, dtmp)
                    S.dma('sp', XOUT[r0:r0 + 128, :], xn_2[:], reads=[dxn])
                    if not last:
                        ln.norm(hb[:], dhb, xn_2, dxn, ADN[v][:, 1024:2048], ADN[v][:, 0:1024], dADN[v], tmp_2, dtmp)
                        emit_T(S, hb, dhb, hTn[:], dhTn, identb[:], dib, psTb, dpsTb)
                        S.dma('pool', HTNv[:, :, r0:r0 + 128], hTn[:], reads=[dhTn])


import ml_dtypes

BF = ml_dtypes.bfloat16


def _cols_tm(i):
    c = []
    for base in (256, 512, 768):
        c += list(range(base + 64 * i, base + 64 * i + 64))
    c += [1024 + i, 1028 + i, 1032 + i, 1036 + i]
    c += list(range(1296 + 128 * i, 1296 + 128 * i + 128))
    k = i // 2
    c += list(range(1808 + 64 * k, 1808 + 64 * k + 64)) + list(range(1936 + 64 * k, 1936 + 64 * k + 64))
    for base in (2320, 2576, 2832):
        c += list(range(base + 64 * i, base + 64 * i + 64))
    return c


def _cols_fm(i):
    c = list(range(64 * i, 64 * i + 64)) + list(range(256 + 64 * i, 256 + 64 * i + 64))
    c += list(range(2064 + 64 * i, 2064 + 64 * i + 64)) + list(range(2320 + 64 * i, 2320 + 64 * i + 64))
    c += list(range(1040 + 64 * i, 1040 + 64 * i + 64)) + list(range(3088, 3120))
    return c


def _rope_tabs():
    rows = TL // 64
    row = np.repeat(np.arange(rows, dtype=np.float32), 64)
    col = np.tile(np.arange(64, dtype=np.float32), rows)
    inv = np.power(np.float32(10000.0), -np.arange(16, dtype=np.float32) / 16).astype(np.float32)
    ar = row[:, None] * inv
    ac = col[:, None] * inv
    cos = np.concatenate([np.cos(ar), np.cos(ar), np.cos(ac), np.cos(ac)], axis=1).astype(np.float32)
    sin = np.concatenate([-np.sin(ar), np.sin(ar), -np.sin(ac), np.sin(ac)], axis=1).astype(np.float32)
    return np.ascontiguousarray(np.tile(cos, (1, 3))), np.ascontiguousarray(np.tile(sin, (1, 3)))


def _dt(a):
    return BF16 if a.dtype == BF else F32


def _launch(build, in_maps, outs):
    nc = bass.Bass("TRN2", target_bir_lowering=False)
    aps = {k: nc.dram_tensor(k, list(v.shape), _dt(v), kind="ExternalInput").ap() for k, v in in_maps[0].items()}
    oaps = {k: nc.dram_tensor(k, list(shp), dt, kind="ExternalOutput").ap() for k, (shp, dt) in outs.items()}
    S = Sched(nc)
    build(nc, S, aps, oaps)
    S.emit()
    res = run_bass_kernel_spmd(nc, in_maps, core_ids=list(range(len(in_maps))))
    return res.results


def kernel_unfused(x, c, ctx, c_ctx, w_ada, b_ada, w_in, ml_gate_b, ml_norm_g, gq_qnorm_g, gq_knorm_g,
           gl_w2, gl_b2, gl_norm_g, w_branch, w_gate, b_gate, w_out, ln1_g, ln1_b, ln2_g, ln2_b,
           ffd_wg, ffd_wu, ffd_wd, moe_wr, moe_br, moe_wg, moe_wu, moe_wd):
    f32 = lambda a: np.ascontiguousarray(np.asarray(a, dtype=np.float32))
    x, c, ctx, c_ctx = f32(x), f32(c), f32(ctx), f32(c_ctx)
    w_ada, b_ada, w_in = f32(w_ada), f32(b_ada), f32(w_in)
    ident = np.eye(128, dtype=np.float32)
    tri = np.stack([np.triu(np.ones((128, 128), np.float32)), np.tril(np.ones((128, 128), np.float32))])
    ones = np.ones((128, 128), np.float32)
    cos3, sin3 = _rope_tabs()
    FT = fourier_tables()
    NT0 = TC + 4096

    def assemble_hT(res):
        hTb = []
        for b in range(2):
            parts = [np.asarray(res[4 * b]["HTN"])[:, 0:TC]] + [np.asarray(res[4 * b + r]["HTN"])[:, -4096:] for r in range(4)]
            hTb.append(np.ascontiguousarray(np.concatenate(parts, axis=1)))
        return hTb

    ims = []
    for core in range(8):
        b, r = divmod(core, 4)
        ims.append(dict(xin=np.ascontiguousarray(np.concatenate([ctx[b], x[b, r * 4096:(r + 1) * 4096]], axis=0)),
                        cb=c[b], cc=c_ctx, wa=w_ada[0], ba=b_ada[0], ident=ident))
    res = _launch(lambda nc, S, a, o: emit_PRE(S, a['xin'], NT0, TC, a['cb'], a['cc'], a['wa'], a['ba'], a['ident'], o['HTN']),
                  ims, dict(HTN=([D, NT0], BF16)))
    hTb = assemble_hT(res)
    xcur = [np.ascontiguousarray(np.concatenate([ctx[b], x[b, r * 4096:(r + 1) * 4096]], axis=0)) for b in range(2) for r in range(4)]

    for l in range(2):
        last = l == 1
        ims = []
        for core in range(8):
            b, i = divmod(core, 4)
            d = dict(hT=hTb[b], wtm=np.ascontiguousarray(w_in[l][:, _cols_tm(i)]), wfm=np.ascontiguousarray(w_in[l][:, _cols_fm(i)]),
                     cos3=cos3, sin3=sin3, gq=f32(gq_qnorm_g[l]), gk=f32(gq_knorm_g[l]), ident=ident, tri=tri, ones=ones,
                     gb=f32(np.asarray(ml_gate_b)[l][:, i]), mng=f32(np.asarray(ml_norm_g)[l][64 * i:64 * i + 64]),
                     w2=f32(np.asarray(gl_w2)[l][:, :, 64 * i:64 * i + 64]), b2=f32(np.asarray(gl_b2)[l][:, 64 * i:64 * i + 64]),
                     gng=f32(np.asarray(gl_norm_g)[l][64 * i:64 * i + 64]))
            d.update(FT)
            ims.append(d)

        def buildA(nc, S, a, o, l=l):
            PTM = nc.dram_tensor("PTM", [TA, NTM], F32, kind="Internal").ap()
            PFM = nc.dram_tensor("PFM", [NFM, TA], F32, kind="Internal").ap()
            VD = nc.dram_tensor("VD", [TL, 128], F32, kind="Internal").ap()
            emit_A1(S, a['hT'], a['wtm'], a['wfm'], PTM, PFM)
            S.barrier()
            emit_A2(S, PTM, PFM, o['BR'], a['gb'], a['mng'], a['tri'], a['ones'])
            emit_A3(S, PTM, PFM, o['BR'], a['w2'], a['b2'], a['gng'], a['tri'])
            emit_A5(S, PFM, o['BR'], VD, a['f64'], a['c256'], a['f1'], a['tw'], a['c2'], l == 0)
            emit_A4(S, PTM, o['BR'], a['cos3'], a['sin3'], a['gq'], a['gk'], a['ident'], l == 0)

        res = _launch(buildA, ims, dict(BR=([TA, 320], F32)))
        brT = []
        for b in range(2):
            br = np.empty((TA, 1280), np.float32)
            for i in range(4):
                o = np.asarray(res[4 * b + i]["BR"])
                br[:, 64 * i:64 * i + 64] = o[:, 0:64]
                br[:, 256 + 64 * i:256 + 64 * i + 64] = o[:, 64:128]
                br[:, 512 + 128 * i:512 + 128 * i + 128] = o[:, 128:256]
                br[:, 1024 + 64 * i:1024 + 64 * i + 64] = o[:, 256:320]
            brT.append(np.ascontiguousarray(br.T).astype(BF))
        ntok = NT0 if l == 0 else 4096
        nctx = TC if l == 0 else 0
        moe = l % 2 == 1
        j = l // 2
        if moe:
            wg, wu, wd = f32(np.asarray(moe_wg)[j]), f32(np.asarray(moe_wu)[j]), f32(np.asarray(moe_wd)[j])
        else:
            g_, u_, d_ = np.asarray(ffd_wg)[j], np.asarray(ffd_wu)[j], np.asarray(ffd_wd)[j]
            wg = f32(np.stack([g_[:, 0:1408], g_[:, 1408:2816]]))
            wu = f32(np.stack([u_[:, 0:1408], u_[:, 1408:2816]]))
            wd = f32(np.stack([d_[0:1408], d_[1408:2816]]))
        ims = []
        for core in range(8):
            b, r = divmod(core, 4)
            tcols = (list(range(TC)) if nctx else []) + list(range(TC + r * 4096, TC + (r + 1) * 4096))
            d = dict(xin=xcur[core], hT=np.ascontiguousarray(hTb[b][:, tcols]), brT=np.ascontiguousarray(brT[b][:, tcols]),
                     cb=c[b], cc=c_ctx, wa=w_ada[l], ba=b_ada[l],
                     wgate=f32(np.asarray(w_gate)[l]), bgate=f32(np.asarray(b_gate)[l].reshape(32, 128).T), wbr=f32(np.asarray(w_branch)[l]), wout=f32(np.asarray(w_out)[l]),
                     l1g=f32(np.asarray(ln1_g)[l]), l1b=f32(np.asarray(ln1_b)[l]), l2g=f32(np.asarray(ln2_g)[l]), l2b=f32(np.asarray(ln2_b)[l]),
                     wg=wg, wu=wu, wd=wd, ident=ident)
            if not last:
                d.update(wan=w_ada[l + 1], ban=b_ada[l + 1])
            if moe:
                d.update(wr=f32(np.asarray(moe_wr)[j]), brr=f32(np.asarray(moe_br)[j]))
            ims.append(d)

        def buildB(nc, S, a, o, last=last, moe=moe, ntok=ntok, nctx=nctx):
            XMID = nc.dram_tensor("XMID", [ntok, D], F32, kind="Internal").ap()
            H2T = nc.dram_tensor("H2T", [D, ntok], BF16, kind="Internal").ap()
            DENSE = nc.dram_tensor("DENSE", [ntok, 8], F32, kind="Internal").ap()
            emit_B(S, last, moe, ntok, nctx, a['xin'], a['hT'], a['brT'], a['cb'], a['cc'], a['wa'], a['ba'],
                   a.get('wan'), a.get('ban'), a['wgate'], a['bgate'], a['wbr'], a['wout'], a['l1g'], a['l1b'], a['l2g'], a['l2b'],
                   a['wg'], a['wu'], a['wd'], a.get('wr'), a.get('brr'), a['ident'], XMID, H2T, DENSE, o['XOUT'], o.get('HTN'))

        outs = dict(XOUT=([ntok, D], F32))
        if not last:
            outs['HTN'] = ([D, ntok], BF16)
        res = _launch(buildB, ims, outs)
        if not last:
            hTb = assemble_hT(res)
            xcur = [np.ascontiguousarray(np.asarray(res[core]["XOUT"])[TC:]) for core in range(8)]
        else:
            out = np.empty((2, TL, D), np.float32)
            for core in range(8):
                b, r = divmod(core, 4)
                out[b, r * 4096:(r + 1) * 4096] = np.asarray(res[core]["XOUT"])
            return out


GROUPS = [[0, 1, 2, 3], [4, 5, 6, 7]]
NT0 = TC + 4096


def build_fused(nc, S, a, o, stop=None):
    I = lambda name, shape, dt=F32: o[name] if name in o else nc.dram_tensor(name, list(shape), dt, kind="Internal").ap()
    HTN = [I("HTN0", [D, NT0], BF16), I("HTN1", [D, NT0], BF16)]
    HTG = [I("HTG0", [16 * 4 * 64, NT0], BF16), I("HTG1", [16 * 4 * 64, NT0], BF16)]
    BR = [I("BR0", [TA, 320], BF16), I("BR1", [TA, 320], BF16)]
    BRG = [I("BRG0", [13 * 4 * 1280, 320], BF16), I("BRG1", [13 * 4 * 1280, 320], BF16)]
    PTM, PFM, VD = I("PTM", [TA, NTM]), I("PFM", [NFM, TA]), I("VD", [TL, 128])
    XOUT0 = I("XOUT0", [NT0, D])
    XMID = [I("XMID0", [NT0, D]), I("XMID1", [4096, D])]
    H2T = [I("H2T0", [D, NT0], BF16), I("H2T1", [D, 4096], BF16)]
    DENSE = I("DENSE", [4096, 8])
    BRT = [I("BRT0", [1280, NT0], BF16), I("BRT1", [1280, 4096], BF16)]
    emit_PRE(S, a['xin'], NT0, TC, a['cb'], a['cc'], a['wa0'], a['ba0'], a['ident'], HTN[0])
    if stop == 'pre':
        return
    for l in range(2):
        last = l == 1
        S.barrier()
        for ch in range(16):
            S.cc("AllGather", HTG[l][ch * 256:(ch + 1) * 256, :], HTN[l][ch * 64:(ch + 1) * 64, :], GROUPS)
        S.barrier()
        if stop == 'ag%d' % l:
            return
        HTGv = HTG[l].rearrange("(kc hf r i) t -> r hf i kc t", kc=8, hf=2, r=4)

        def hT_fn(t0, W, HTGv=HTGv):
            if t0 < TC:
                r, c0 = 0, t0
            else:
                r, loc = divmod(t0 - TC, 4096)
                c0 = TC + loc
            return [HTGv[r][hf][:, :, c0:c0 + W] for hf in range(2)]

        emit_A1(S, hT_fn, a[f'wtm{l}'], a[f'wfm{l}'], PTM, PFM)
        S.barrier()
        if stop == 'a1_%d' % l:
            return
        emit_A2(S, PTM, PFM, BR[l], a[f'gb{l}'], a[f'mng{l}'], a['tri'], a['ones'])
        emit_A3(S, PTM, PFM, BR[l], a[f'w2{l}'], a[f'b2{l}'], a[f'gng{l}'], a['tri'])
        emit_A5(S, PFM, BR[l], VD, a['f64'], a['c256'], a['f1'], a['tw'], a['c2'], l == 0)
        emit_A4(S, PTM, BR[l], a['cos3'], a['sin3'], a[f'gq{l}'], a[f'gk{l}'], a['ident'], l == 0)
        S.barrier()
        if stop == 'a_%d' % l:
            return
        for ch in range(13):
            S.cc("AllGather", BRG[l][ch * 5120:(ch + 1) * 5120, :], BR[l][ch * 1280:(ch + 1) * 1280, :], GROUPS)
        S.barrier()
        if stop == 'agb%d' % l:
            return
        BRG4 = BRG[l].rearrange("(ch i t) n -> ch t i n", ch=13, i=4)

        def BRG3(row, BRG4=BRG4):
            ch, w0 = divmod(row, 1280)
            return BRG4[ch][w0:w0 + 128, :, :]

        if l == 0:
            emit_B(S, False, False, NT0, TC, a['xin'], HTN[0], (BRG3, a['selI'], BF16, BRT[l]), a['cb'], a['cc'], a['wa0'], a['ba0'], a['wa1'], a['ba1'],
                   a['wgate0'], a['bgate0'], a['wbr0'], a['wout0'], a['l1g0'], a['l1b0'], a['l2g0'], a['l2b0'],
                   a['fwg0'], a['fwu0'], a['fwd0'], None, None, a['ident'], XMID[0], H2T[0], DENSE, XOUT0, HTN[1])
            if stop == 'b0':
                return
        else:
            emit_B(S, True, True, 4096, 0, XOUT0[TC:NT0, :], HTN[1][:, TC:NT0], (BRG3, a['selI'], BF16, BRT[l]), a['cb'], a['cc'], a['wa1'], a['ba1'], None, None,
                   a['wgate1'], a['bgate1'], a['wbr1'], a['wout1'], a['l1g1'], a['l1b1'], a['l2g1'], a['l2b1'],
                   a['fwg1'], a['fwu1'], a['fwd1'], a['wr'], a['brr'], a['ident'], XMID[1], H2T[1], DENSE, o['XOUT'], None)


def kernel(x, c, ctx, c_ctx, w_ada, b_ada, w_in, ml_gate_b, ml_norm_g, gq_qnorm_g, gq_knorm_g,
           gl_w2, gl_b2, gl_norm_g, w_branch, w_gate, b_gate, w_out, ln1_g, ln1_b, ln2_g, ln2_b,
           ffd_wg, ffd_wu, ffd_wd, moe_wr, moe_br, moe_wg, moe_wu, moe_wd):
    f32 = lambda a: np.ascontiguousarray(np.asarray(a, dtype=np.float32))
    x, c, ctx, c_ctx = f32(x), f32(c), f32(ctx), f32(c_ctx)
    w_ada, b_ada, w_in = f32(w_ada), f32(b_ada), f32(w_in)
    ident = np.eye(128, dtype=np.float32)
    tri = np.stack([np.triu(np.ones((128, 128), np.float32)), np.tril(np.ones((128, 128), np.float32))])
    ones = np.ones((128, 128), np.float32)
    cos3, sin3 = _rope_tabs()
    FT = fourier_tables()
    NT0 = TC + 4096
    g0, u0, d0 = np.asarray(ffd_wg)[0], np.asarray(ffd_wu)[0], np.asarray(ffd_wd)[0]
    shared = dict(cc=c_ctx, wa0=w_ada[0], ba0=b_ada[0], wa1=w_ada[1], ba1=b_ada[1],
                  cos3=cos3, sin3=sin3, ident=ident, tri=tri, ones=ones,
                  fwg0=f32(np.stack([g0[:, 0:1408], g0[:, 1408:2816]])), fwu0=f32(np.stack([u0[:, 0:1408], u0[:, 1408:2816]])),
                  fwd0=f32(np.stack([d0[0:1408], d0[1408:2816]])),
                  fwg1=f32(np.asarray(moe_wg)[0]), fwu1=f32(np.asarray(moe_wu)[0]), fwd1=f32(np.asarray(moe_wd)[0]),
                  wr=f32(np.asarray(moe_wr)[0]), brr=f32(np.asarray(moe_br)[0]))
    shared.update(FT)
    for l in range(2):
        shared.update({f"gq{l}": f32(np.asarray(gq_qnorm_g)[l]), f"gk{l}": f32(np.asarray(gq_knorm_g)[l]),
                       f"wgate{l}": f32(np.asarray(w_gate)[l]), f"bgate{l}": f32(np.asarray(b_gate)[l].reshape(32, 128).T),
                       f"wbr{l}": f32(np.asarray(w_branch)[l]), f"wout{l}": f32(np.asarray(w_out)[l]),
                       f"l1g{l}": f32(np.asarray(ln1_g)[l]), f"l1b{l}": f32(np.asarray(ln1_b)[l]),
                       f"l2g{l}": f32(np.asarray(ln2_g)[l]), f"l2b{l}": f32(np.asarray(ln2_b)[l])})
    ims = []
    for core in range(8):
        b, i = divmod(core, 4)
        d = dict(shared)
        d['xin'] = np.ascontiguousarray(np.concatenate([ctx[b], x[b, i * 4096:(i + 1) * 4096]], axis=0))
        d['cb'] = c[b]
        sel = np.zeros((4, 128, 128), np.float32)
        sel[i] = ident
        d['selI'] = sel
        for l in range(2):
            d[f"wtm{l}"] = np.ascontiguousarray(w_in[l][:, _cols_tm(i)])
            d[f"wfm{l}"] = np.ascontiguousarray(w_in[l][:, _cols_fm(i)])
            d[f"gb{l}"] = f32(np.asarray(ml_gate_b)[l][:, i])
            d[f"mng{l}"] = f32(np.asarray(ml_norm_g)[l][64 * i:64 * i + 64])
            d[f"w2{l}"] = f32(np.asarray(gl_w2)[l][:, :, 64 * i:64 * i + 64])
            d[f"b2{l}"] = f32(np.asarray(gl_b2)[l][:, 64 * i:64 * i + 64])
            d[f"gng{l}"] = f32(np.asarray(gl_norm_g)[l][64 * i:64 * i + 64])
        ims.append(d)
    if DBG.get('ims_only'):
        return ims
    res = _launch(build_fused, ims, dict(XOUT=([4096, D], F32)))
    out = np.empty((2, TL, D), np.float32)
    for core in range(8):
        b, r = divmod(core, 4)
        out[b, r * 4096:(r + 1) * 4096] = np.asarray(res[core]["XOUT"])
    return out
```
